# Optimizing a Trainium2 kernel written in Bass

```python
import math
import jax, jax.numpy as jnp
from jax import lax
import numpy as np

D_MODEL = 1024
BATCH = 8
SEQ = 4096
DEPTH = 2

N_MIXERS = 2
N_POOL_LAYERS = (DEPTH + 1) // 2
N_DN_LAYERS = DEPTH // 2

RMS_EPS = 1e-6

POOL_WINDOWS = (2, 4, 8, 16)
POOL_GROUPS = len(POOL_WINDOWS)
POOL_GROUP_DIM = D_MODEL // POOL_GROUPS

DN_HEAD_DIM = 128
DN_HEADS = D_MODEL // DN_HEAD_DIM
DN_KEY_DIM = DN_HEADS * DN_HEAD_DIM
DN_CONV = 4
DN_CHUNK = 64
DN_IN_DIM = 4 * DN_KEY_DIM + 2 * DN_HEADS

DENSE_FF = 2816
N_EXPERTS = 8
TOP_K = 2
EXPERT_FF = 3584

kernel_name = "hybrid_pool_gdn_moe_trunk"


def rmsnorm(x, g):
    x32 = x.astype(jnp.float32)
    y = x32 * lax.rsqrt(jnp.mean(x32 * x32, axis=-1, keepdims=True) + RMS_EPS)
    return (y * g.astype(jnp.float32)).astype(x.dtype)


def l2norm(x):
    return x * lax.rsqrt(jnp.sum(x * x, axis=-1, keepdims=True) + RMS_EPS)


def swiglu(x, w_gate, w_up, w_down):
    return (jax.nn.silu(x @ w_gate) * (x @ w_up)) @ w_down


def pool_mixer(h, w_in, w_group, scale):
    b, s, d = h.shape
    u = (h @ w_in).reshape(b, s, POOL_GROUPS, POOL_GROUP_DIM).astype(jnp.float32)
    csum = jnp.cumsum(u, axis=1)
    pos = jnp.arange(1, s + 1, dtype=jnp.float32)[:, None]
    outs = []
    for gi, w in enumerate(POOL_WINDOWS):
        c = csum[:, :, gi]
        c_prev = jnp.pad(c, ((0, 0), (w, 0), (0, 0)))[:, :s]
        cnt = jnp.minimum(pos, float(w))
        outs.append((c - c_prev) / cnt - u[:, :, gi])
    m = jnp.stack(outs, axis=2).astype(h.dtype)
    y = jnp.einsum('bsgc,gce->bsge', m, w_group).reshape(b, s, d)
    return y * scale


def causal_depthwise_conv(x, w):
    k, c = w.shape
    return lax.conv_general_dilated(
        x, w[:, None, :], window_strides=(1,), padding=[(k - 1, 0)],
        dimension_numbers=('NWC', 'WIO', 'NWC'), feature_group_count=c)


def gated_delta_rule_chunked(q, k, v, beta, g):
    b, s, h, dk = q.shape
    dv = v.shape[-1]
    c = DN_CHUNK
    n = s // c

    def to_chunks(t):
        t = t.reshape((b, n, c, h) + t.shape[3:])
        return jnp.moveaxis(t, 3, 1)

    q, k, v, beta, g = (to_chunks(t) for t in (q, k, v, beta, g))
    gc = jnp.cumsum(g, axis=-1)
    causal = jnp.tril(jnp.ones((c, c), dtype=bool))
    strict = jnp.tril(jnp.ones((c, c), dtype=bool), -1)
    decay = jnp.exp(jnp.where(causal, gc[..., :, None] - gc[..., None, :], -jnp.inf))

    k_beta = k * beta[..., None]
    v_beta = v * beta[..., None]
    a = jnp.einsum('bhnid,bhnjd->bhnij', k_beta, k) * decay
    a = jnp.where(strict, a, 0.0) + jnp.eye(c, dtype=a.dtype)
    u = lax.linalg.triangular_solve(a, v_beta, left_side=True, lower=True, unit_diagonal=True)
    w = lax.linalg.triangular_solve(a, k_beta * jnp.exp(gc)[..., None],
                                    left_side=True, lower=True, unit_diagonal=True)
    qk = jnp.einsum('bhnid,bhnjd->bhnij', q, k) * decay

    xs = tuple(jnp.moveaxis(t, 2, 0) for t in (q, k, u, w, qk, gc))

    def step(state, inp):
        q_c, k_c, u_c, w_c, qk_c, gc_c = inp
        v_new = u_c - jnp.einsum('bhck,bhkv->bhcv', w_c, state)
        o = (jnp.einsum('bhck,bhkv->bhcv', q_c * jnp.exp(gc_c)[..., None], state)
             + jnp.einsum('bhij,bhjv->bhiv', qk_c, v_new))
        g_last = gc_c[..., -1]
        k_dec = k_c * jnp.exp(g_last[..., None] - gc_c)[..., None]
        state = state * jnp.exp(g_last)[..., None, None] + jnp.einsum('bhck,bhcv->bhkv', k_dec, v_new)
        return state, o

    s0 = jnp.zeros((b, h, dk, dv), jnp.float32)
    _, o = lax.scan(step, s0, xs)
    return jnp.transpose(o, (1, 0, 3, 2, 4)).reshape(b, s, h, dv)


def deltanet_mixer(h, w_in, conv_w, a_log, dt_bias, norm_g, w_out):
    b, s, _ = h.shape
    kd = DN_KEY_DIM
    proj = h @ w_in
    qkv = proj[..., :3 * kd]
    z = proj[..., 3 * kd:4 * kd]
    b_logit = proj[..., 4 * kd:4 * kd + DN_HEADS].astype(jnp.float32)
    a_in = proj[..., 4 * kd + DN_HEADS:].astype(jnp.float32)
    qkv = jax.nn.silu(causal_depthwise_conv(qkv, conv_w)).astype(jnp.float32)
    q, k, v = jnp.split(qkv, 3, axis=-1)
    q = l2norm(q.reshape(b, s, DN_HEADS, DN_HEAD_DIM)) * (DN_HEAD_DIM ** -0.5)
    k = l2norm(k.reshape(b, s, DN_HEADS, DN_HEAD_DIM))
    v = v.reshape(b, s, DN_HEADS, DN_HEAD_DIM)
    beta = jax.nn.sigmoid(b_logit)
    g = -jnp.exp(a_log.astype(jnp.float32)) * jax.nn.softplus(a_in + dt_bias.astype(jnp.float32))
    o = gated_delta_rule_chunked(q, k, v, beta, g)
    o = o * lax.rsqrt(jnp.mean(o * o, axis=-1, keepdims=True) + RMS_EPS) * norm_g.astype(jnp.float32)
    o = o * jax.nn.silu(z.astype(jnp.float32)).reshape(b, s, DN_HEADS, DN_HEAD_DIM)
    return o.reshape(b, s, kd).astype(h.dtype) @ w_out


def moe_ffn(h, router_w, router_b, w_gate, w_up, w_down):
    b, s, d = h.shape
    t = h.reshape(b * s, d)
    logits = (t @ router_w).astype(jnp.float32) + router_b.astype(jnp.float32)
    top_vals, top_idx = lax.top_k(logits, TOP_K)
    top_w = jax.nn.softmax(top_vals, axis=-1)
    gates = jnp.sum(jax.nn.one_hot(top_idx, N_EXPERTS, dtype=jnp.float32) * top_w[..., None], axis=1)
    out = jnp.zeros((b * s, d), jnp.float32)
    for e in range(N_EXPERTS):
        y = swiglu(t, w_gate[e], w_up[e], w_down[e]).astype(jnp.float32)
        out = out + gates[:, e:e + 1] * y
    return out.astype(h.dtype).reshape(b, s, d)


def setup_inputs(seed: int = 0) -> dict:
    key = jax.random.key(seed)
    ks = jax.random.split(key, 24)
    f32 = jnp.float32
    D = D_MODEL
    NP, ND = N_POOL_LAYERS, N_DN_LAYERS

    def nrm(k, shape, fan_in):
        return jax.random.normal(k, shape, f32) * (fan_in ** -0.5)

    def gain(k, shape):
        return 1.0 + 0.05 * jax.random.normal(k, shape, f32)

    dt = jnp.exp(jax.random.uniform(ks[10], (ND, DN_HEADS), f32,
                                    minval=math.log(1e-3), maxval=math.log(1e-1)))
    return {
        "x": jax.random.normal(ks[0], (BATCH, SEQ, D), f32),
        "norm_mix_g": gain(ks[1], (DEPTH, D)),
        "norm_ffn_g": gain(ks[2], (DEPTH, D)),
        "pool_w_in": nrm(ks[3], (NP, D, D), D),
        "pool_w_group": nrm(ks[4], (NP, POOL_GROUPS, POOL_GROUP_DIM, POOL_GROUP_DIM), POOL_GROUP_DIM),
        "pool_scale": gain(ks[5], (NP, D)),
        "dn_w_in": nrm(ks[6], (ND, D, DN_IN_DIM), D),
        "dn_conv_w": nrm(ks[7], (ND, DN_CONV, 3 * DN_KEY_DIM), DN_CONV),
        "dn_a_log": jnp.log(jax.random.uniform(ks[8], (ND, DN_HEADS), f32, minval=1.0, maxval=16.0)),
        "dn_dt_bias": dt + jnp.log(-jnp.expm1(-dt)),
        "dn_norm_g": gain(ks[9], (ND, DN_HEAD_DIM)),
        "dn_w_out": nrm(ks[11], (ND, DN_KEY_DIM, D), DN_KEY_DIM),
        "ffn_w_gate": nrm(ks[12], (NP, D, DENSE_FF), D),
        "ffn_w_up": nrm(ks[13], (NP, D, DENSE_FF), D),
        "ffn_w_down": nrm(ks[14], (NP, DENSE_FF, D), DENSE_FF),
        "moe_router_w": nrm(ks[15], (ND, D, N_EXPERTS), D),
        "moe_router_b": 0.01 * jax.random.normal(ks[16], (ND, N_EXPERTS), f32),
        "moe_w_gate": nrm(ks[17], (ND, N_EXPERTS, D, EXPERT_FF), D),
        "moe_w_up": nrm(ks[18], (ND, N_EXPERTS, D, EXPERT_FF), D),
        "moe_w_down": nrm(ks[19], (ND, N_EXPERTS, EXPERT_FF, D), EXPERT_FF),
        "final_norm_g": gain(ks[20], (D,)),
    }


def reference(x, norm_mix_g, norm_ffn_g, pool_w_in, pool_w_group, pool_scale,
              dn_w_in, dn_conv_w, dn_a_log, dn_dt_bias, dn_norm_g, dn_w_out,
              ffn_w_gate, ffn_w_up, ffn_w_down,
              moe_router_w, moe_router_b, moe_w_gate, moe_w_up, moe_w_down,
              final_norm_g):
    h = x
    for i in range(DEPTH):
        j = i // N_MIXERS
        hn = rmsnorm(h, norm_mix_g[i])
        if i % N_MIXERS == 0:
            h = h + pool_mixer(hn, pool_w_in[j], pool_w_group[j], pool_scale[j])
        else:
            h = h + deltanet_mixer(hn, dn_w_in[j], dn_conv_w[j], dn_a_log[j], dn_dt_bias[j],
                                   dn_norm_g[j], dn_w_out[j])
        hn = rmsnorm(h, norm_ffn_g[i])
        if i % 2 == 0:
            h = h + swiglu(hn, ffn_w_gate[j], ffn_w_up[j], ffn_w_down[j])
        else:
            h = h + moe_ffn(hn, moe_router_w[j], moe_router_b[j], moe_w_gate[j], moe_w_up[j], moe_w_down[j])
    return rmsnorm(h, final_norm_g)
```

```python
import numpy as np
from contextlib import ExitStack
import concourse.bass as bass
import concourse.mybir as mybir
from concourse.bass_utils import run_bass_kernel_spmd

F32 = mybir.dt.float32
BF16 = mybir.dt.bfloat16
I32 = mybir.dt.int32
ALU = mybir.AluOpType
AF = mybir.ActivationFunctionType
AX = mybir.AxisListType


class Buf:
    __slots__ = ("name", "lw", "rd", "sem", "excl")

    def __init__(self, name):
        self.name = name
        self.excl = False
        self.lw = None
        self.rd = []
        self.sem = None


class Op:
    __slots__ = ("eng", "fn", "deps", "sig", "dma", "semname", "used")

    def __init__(self, eng, fn, dma, semname):
        self.eng = eng
        self.fn = fn
        self.deps = []
        self.sig = None
        self.dma = dma
        self.semname = semname
        self.used = False


class KB:
    ENGS = ("pe", "act", "dve", "pool", "sp")

    def __init__(self, nc):
        self.nc = nc
        self.stack = ExitStack()
        self.sems = {}
        self.semval = {}
        self.free_dma_sems = {"hw": [], "sw": []}
        self.n_dma_sems = 0
        self.n_logical = 0
        for e in ("pe", "act", "dve", "pool"):
            self._mksem("c_" + e)
        self.ops = []
        self.last_dma_on_sem = {}
        self.phase_stack = None
        self.n_emitted = 0

    def _mksem(self, name):
        h = self.stack.enter_context(self.nc.semaphore(name))
        self.sems[name] = h
        self.semval[name] = 0
        return h

    def dma_sem(self, kind):
        if self.free_dma_sems[kind]:
            return self.free_dma_sems[kind].pop()
        name = "d%s%d" % (kind, self.n_dma_sems)
        self.n_dma_sems += 1
        self._mksem(name)
        return name

    def begin_phase(self):
        self.ops = []
        self.last_dma_on_sem = {}
        self.phase_stack = ExitStack()
        self.phase_sems = []
        self.sem_map = {}
        self.bufs = []

    def pbuf(self):
        b = self.buf()
        b.excl = True
        return b

    def buf(self, name="b"):
        b = Buf(name)
        self.bufs.append(b)
        return b

    def sb(self, name, shape, dtype):
        self.uid = getattr(self, "uid", 0) + 1
        return self.phase_stack.enter_context(self.nc.sbuf_tensor("sb%d_%s" % (self.uid, name), list(shape), dtype))

    def ps(self, name, shape, dtype=F32):
        self.uid = getattr(self, "uid", 0) + 1
        return self.phase_stack.enter_context(self.nc.psum_tensor("ps%d_%s" % (self.uid, name), list(shape), dtype))

    def phase_dma_sem(self):
        self.n_logical += 1
        return "L%d" % self.n_logical

    def op(self, eng, fn, reads=(), writes=(), dma_sem=None):
        if dma_sem is not None:
            kind = "sw" if eng == "pool" else "hw"
            key = (dma_sem, kind)
            if key not in self.sem_map:
                ph = self.dma_sem(kind)
                self.sem_map[key] = ph
                self.phase_sems.append((kind, ph))
            dma_sem = self.sem_map[key]
        o = Op(eng, fn, dma_sem is not None, dma_sem)
        deps = []
        for b in reads:
            if b.lw is not None:
                deps.append(b.lw)
            if b.excl:
                deps.extend(r for r in b.rd if r.eng != eng)
        for b in writes:
            if b.lw is not None:
                deps.append(b.lw)
            deps.extend(b.rd)
        if dma_sem is not None:
            p = self.last_dma_on_sem.get(dma_sem)
            if p is not None:
                deps.append(p)
            self.last_dma_on_sem[dma_sem] = o
        seen = set()
        for d in deps:
            if id(d) in seen or d is o:
                continue
            seen.add(id(d))
            if eng == "pe" and d.eng == "pe" and not d.dma and not o.dma:
                continue
            o.deps.append(d)
            d.used = True
        for b in reads:
            b.rd.append(o)
        for b in writes:
            b.lw = o
            b.rd = []
        self.ops.append(o)
        return o

    def end_phase(self, final_wait=True):
        nc = self.nc
        for o in self.ops:
            if o.dma:
                self.semval[o.semname] += 16
                o.sig = (o.semname, self.semval[o.semname])
            elif o.used:
                s = "c_" + o.eng
                self.semval[s] += 1
                o.sig = (s, self.semval[s])
        per = {e: [] for e in self.ENGS}
        for o in self.ops:
            per[o.eng].append(o)
        final = [(s, self.semval[s]) for s in set(o.semname for o in self.ops if o.dma)]
        sems = self.sems
        self.n_emitted += len(self.ops)

        def emit(engname, handle):
            waited = {}
            for o in per[engname]:
                for d in o.deps:
                    s, v = d.sig
                    if waited.get(s, 0) < v:
                        handle.wait_ge(sems[s], v)
                        waited[s] = v
                ins = o.fn(handle)
                if o.sig is not None:
                    ins.then_inc(sems[o.sig[0]], 16 if o.dma else 1)
            if engname == "sp" and final_wait:
                for s, v in final:
                    if waited.get(s, 0) < v:
                        handle.wait_ge(sems[s], v)

        with nc.Block() as block:
            @block.sync
            def _(e):
                emit("sp", e)

            @block.tensor
            def _(e):
                emit("pe", e)

            @block.scalar
            def _(e):
                emit("act", e)

            @block.vector
            def _(e):
                emit("dve", e)

            @block.gpsimd
            def _(e):
                emit("pool", e)
        for kind, s in self.phase_sems:
            self.free_dma_sems[kind].append(s)
        self.phase_stack.close()
        self.phase_stack = None
        self.ops = []

    def close(self):
        self.stack.close()


EPS = 1e-6
S = 4096
D = 1024


def norm_tiles(kb, nt, src, b_src, gbc, b_gbc, hn, b_hn, ss, b_ss, rstd, b_rstd, junk, b_junk):
    for j in range(nt):
        kb.op("act", lambda e, j=j: e.activation(out=junk[:], in_=src[:, j, :], func=AF.Square, accum_out=ss[:, j:j + 1]),
              reads=[b_src[j]], writes=[b_junk, b_ss])
    kb.op("dve", lambda e: e.tensor_scalar(out=rstd[:, 0:nt], in0=ss[:, 0:nt], scalar1=1.0 / D, scalar2=EPS, op0=ALU.mult, op1=ALU.add),
          reads=[b_ss], writes=[b_rstd])
    kb.op("act", lambda e: e.activation(out=rstd[:, 0:nt], in_=rstd[:, 0:nt], func=AF.Sqrt), reads=[b_rstd], writes=[b_rstd])
    kb.op("dve", lambda e: e.reciprocal(out=rstd[:, 0:nt], in_=rstd[:, 0:nt]), reads=[b_rstd], writes=[b_rstd])
    for j in range(nt):
        kb.op("dve", lambda e, j=j: e.scalar_tensor_tensor(out=hn[:, j, :], in0=src[:, j, :], scalar=rstd[:, j:j + 1], in1=gbc[:],
                                                             op0=ALU.mult, op1=ALU.mult),
              reads=[b_src[j], b_rstd, b_gbc], writes=[b_hn[j]])


def phase_pool(kb, x_d, h1_d, W):
    nc = kb.nc
    kb.begin_phase()
    NB = S // 512
    win = kb.sb("win", [128, 8, 1024], BF16); b_win = kb.buf()
    wgs = kb.sb("wgs", [128, 4, 2, 256], F32); b_wgs = kb.buf()
    wg = kb.sb("wg", [128, 4, 2, 256], BF16); b_wg = kb.buf()
    scbc = kb.sb("scbc", [128, 1024], F32); b_scbc = kb.buf()
    gbc = kb.sb("gbc", [128, 1024], F32); b_gbc = kb.buf()
    idf = kb.sb("idf", [128, 128], F32); b_idf = kb.buf()
    invc = kb.sb("invc", [128, 4, 16], F32); b_invc = kb.buf()
    sq = kb.phase_dma_sem
    kb.op("pool", lambda e: e.dma_start(out=win[:], in_=W["pool_w_in"][0].rearrange("(k p) n -> p k n", p=128)), writes=[b_win], dma_sem=sq())
    kb.op("sp", lambda e: e.dma_start(out=wgs[:], in_=W["pool_w_group"][0].rearrange("g (k p) e -> p g k e", p=128)), writes=[b_wgs], dma_sem=sq())
    kb.op("sp", lambda e: e.dma_start(out=scbc[:], in_=W["pool_scale_bc"]), writes=[b_scbc], dma_sem=sq())
    kb.op("sp", lambda e: e.dma_start(out=gbc[:], in_=W["g_mix0_bc"]), writes=[b_gbc], dma_sem=sq())
    kb.op("sp", lambda e: e.dma_start(out=idf[:], in_=W["ident"]), writes=[b_idf], dma_sem=sq())
    kb.op("sp", lambda e: e.dma_start(out=invc[:], in_=W["invc"]), writes=[b_invc], dma_sem=sq())
    for kk in range(2):
        kb.op("dve", lambda e, kk=kk: e.tensor_tensor(out=wg[:, :, kk, :], in0=wgs[:, :, kk, :],
                                                       in1=scbc[:].rearrange("p (g e) -> p g e", g=4), op=ALU.mult),
              reads=[b_wgs, b_scbc], writes=[b_wg])

    NBUF = 3
    xt = [kb.sb("xt%d" % i, [128, 4, 1024], F32) for i in range(NBUF)]
    b_xt = [[kb.buf() for _ in range(4)] for i in range(NBUF)]
    s_xt = [sq() for i in range(NBUF)]
    s_st = [sq() for i in range(NBUF)]
    hns = [kb.sb("hn%d" % i, [128, 4, 1024], F32) for i in range(2)]; b_hns = [[kb.buf() for _ in range(4)] for _ in range(2)]
    hnTs = [kb.sb("hnT%d" % i, [128, 8, 512], BF16) for i in range(2)]; b_hnTs = [kb.buf() for _ in range(2)]
    ss = kb.sb("ss", [128, 4], F32); b_ss = kb.buf()
    rstd = kb.sb("rstd", [128, 4], F32); b_rstd = kb.buf()
    junk = kb.sb("junk", [128, 1024], BF16); b_junk = kb.buf()
    U = [kb.sb("U%d" % c, [128, 528], F32) for c in range(8)]; b_U = [kb.buf() for _ in range(8)]
    NAB = 3
    A = [kb.sb("A%d" % i, [128, 528], F32) for i in range(NAB)]; b_A = [kb.buf() for _ in range(NAB)]
    B = [kb.sb("B%d" % i, [128, 528], F32) for i in range(NAB)]; b_B = [kb.buf() for _ in range(NAB)]
    mTs = [kb.sb("mT%d" % i, [128, 8, 512], BF16) for i in range(2)]; b_mTs = [[kb.buf() for _ in range(8)] for _ in range(2)]
    tmp16 = kb.sb("tmp16", [128, 16], F32); b_tmp16 = kb.buf()
    pT = [kb.ps("pT%d" % i, [128, 2, 512], F32) for i in range(2)]; b_pT = [kb.pbuf() for _ in range(2)]
    pU = [kb.ps("pU%d" % i, [128, 512], F32) for i in range(2)]; b_pU = [kb.pbuf() for _ in range(2)]
    pY = kb.ps("pY", [128, 2, 512], F32); b_pY = kb.pbuf()

    for c in range(8):
        kb.op("pool", lambda e, c=c: e.memset(U[c][:, 0:16], 0.0), writes=[b_U[c]])

    def stA(blk):
        xb = xt[blk % NBUF]; bx = b_xt[blk % NBUF]
        hn = hns[blk % 2]; b_hn = b_hns[blk % 2]; hnT = hnTs[blk % 2]; b_hnT = b_hnTs[blk % 2]
        r0 = blk * 512
        kb.op("sp", lambda e: e.dma_start(out=xb[:], in_=x_d[r0:r0 + 512, :].rearrange("(j p) d -> p j d", p=128)),
              writes=bx, dma_sem=s_xt[blk % NBUF])
        yield
        for j in range(4):
            kb.op("act", lambda e, j=j: e.activation(out=junk[:], in_=xb[:, j, :], func=AF.Square, accum_out=ss[:, j:j + 1]),
                  reads=[bx[j]], writes=[b_junk, b_ss])
        yield
        kb.op("dve", lambda e: e.tensor_scalar(out=rstd[:], in0=ss[:], scalar1=1.0 / D, scalar2=EPS, op0=ALU.mult, op1=ALU.add), reads=[b_ss], writes=[b_rstd])
        yield
        kb.op("act", lambda e: e.activation(out=rstd[:], in_=rstd[:], func=AF.Sqrt), reads=[b_rstd], writes=[b_rstd])
        yield
        kb.op("dve", lambda e: e.reciprocal(out=rstd[:], in_=rstd[:]), reads=[b_rstd], writes=[b_rstd])
        yield
        for j in range(4):
            kb.op("dve", lambda e, j=j: e.scalar_tensor_tensor(out=hn[:, j, :], in0=xb[:, j, :], scalar=rstd[:, j:j + 1], in1=gbc[:], op0=ALU.mult, op1=ALU.mult),
                  reads=[bx[j], b_rstd, b_gbc], writes=[b_hn[j]])
            yield
        for j in range(4):
            pt = pT[j % 2]; bp = b_pT[j % 2]
            for k in range(8):
                kb.op("pe", lambda e, j=j, k=k, pt=pt: e.transpose(pt[:, k // 4, (k % 4) * 128:(k % 4 + 1) * 128], hn[:, j, k * 128:(k + 1) * 128], idf[:]),
                      reads=[b_hn[j], b_idf], writes=[bp])
            yield
            kb.op("act", lambda e, j=j, pt=pt: e.copy(out=hnT[:, :, j * 128:(j + 1) * 128], in_=pt[:].rearrange("p a (b t) -> p (a b) t", t=128)),
                  reads=[bp], writes=[b_hnT])
            yield

    def stB(blk):
        hnT = hnTs[blk % 2]; b_hnT = b_hnTs[blk % 2]; mT = mTs[blk % 2]; b_mT = b_mTs[blk % 2]
        state = {}
        for step in range(8 + 2):
            c = step
            if c < 8:
                pu = pU[c % 2]; bpu = b_pU[c % 2]
                for k in range(8):
                    kb.op("pe", lambda e, c=c, k=k, pu=pu: e.matmul(pu[:], win[:, k, c * 128:(c + 1) * 128], hnT[:, k, :], start=(k == 0), stop=(k == 7)),
                          reads=[b_hnT, b_win], writes=[bpu])
                kb.op("act", lambda e, c=c, pu=pu: e.copy(out=U[c][:, 16:528], in_=pu[:]), reads=[bpu], writes=[b_U[c]])
            c = step - 1
            if 0 <= c < 8:
                g = c // 2
                w = 2 ** (g + 1)
                a = A[c % NAB]; ba = b_A[c % NAB]; b = B[c % NAB]; bb = b_B[c % NAB]
                kb.op("pool", lambda e, c=c, a=a: e.tensor_tensor(out=a[:, 1:528], in0=U[c][:, 1:528], in1=U[c][:, 0:527], op=ALU.add),
                      reads=[b_U[c]], writes=[ba])
                cur, bcur = a, ba
                if w >= 4:
                    kb.op("pool", lambda e, a=a, b=b: e.tensor_tensor(out=b[:, 3:528], in0=a[:, 3:528], in1=a[:, 1:526], op=ALU.add),
                          reads=[ba], writes=[bb])
                    cur, bcur = b, bb
                if w >= 8:
                    kb.op("pool", lambda e, a=a, b=b: e.tensor_tensor(out=a[:, 7:528], in0=b[:, 7:528], in1=b[:, 3:524], op=ALU.add),
                          reads=[bb], writes=[ba])
                    cur, bcur = a, ba
                if w >= 16:
                    kb.op("pool", lambda e, a=a, b=b: e.tensor_tensor(out=b[:, 15:528], in0=a[:, 15:528], in1=a[:, 7:520], op=ALU.add),
                          reads=[ba], writes=[bb])
                    cur, bcur = b, bb
                state[c] = (cur, bcur, w, g)
            c = step - 2
            if 0 <= c < 8:
                cur, bcur, w, g = state[c]
                kb.op("dve", lambda e, c=c, cur=cur, w=w: e.scalar_tensor_tensor(out=mT[:, c, :], in0=cur[:, 16:528], scalar=1.0 / w, in1=U[c][:, 16:528],
                                                                                op0=ALU.mult, op1=ALU.subtract),
                      reads=[bcur, b_U[c]], writes=[b_mT[c]])
                if blk == 0:
                    kb.op("dve", lambda e, cur=cur, g=g: e.tensor_tensor(out=tmp16[:], in0=cur[:, 16:32], in1=invc[:, g, :], op=ALU.mult),
                          reads=[bcur, b_invc], writes=[b_tmp16])
                    kb.op("dve", lambda e, c=c: e.tensor_tensor(out=mT[:, c, 0:16], in0=tmp16[:], in1=U[c][:, 16:32], op=ALU.subtract),
                          reads=[b_tmp16, b_U[c]], writes=[b_mT[c]])
                kb.op("pool", lambda e, c=c: e.tensor_copy(out=U[c][:, 0:16], in_=U[c][:, 512:528]), reads=[b_U[c]], writes=[b_U[c]])
            yield

    def stC(blk):
        xb = xt[blk % NBUF]; bx = b_xt[blk % NBUF]
        mT = mTs[blk % 2]; b_mT = b_mTs[blk % 2]
        r0 = blk * 512
        for j in range(4):
            for g in range(4):
                for kk in range(2):
                    kb.op("pe", lambda e, j=j, g=g, kk=kk: e.matmul(pY[:, g // 2, (g % 2) * 256:(g % 2 + 1) * 256],
                                                                   mT[:, 2 * g + kk, j * 128:(j + 1) * 128], wg[:, g, kk, :],
                                                                   start=(kk == 0), stop=(kk == 1)),
                          reads=[b_mT[2 * g + kk], b_wg], writes=[b_pY])
            yield
            for hh in range(2):
                kb.op("dve", lambda e, j=j, hh=hh: e.tensor_tensor(out=xb[:, j, hh * 512:(hh + 1) * 512], in0=xb[:, j, hh * 512:(hh + 1) * 512],
                                                                   in1=pY[:, hh, :], op=ALU.add),
                      reads=[b_pY, bx[j]], writes=[bx[j]])
            yield
        kb.op("sp", lambda e: e.dma_start(out=h1_d[r0:r0 + 512, :].rearrange("(j p) d -> p j d", p=128), in_=xb[:]),
              reads=bx, dma_sem=s_st[blk % NBUF])
        yield

    for it in range(-1, NB + 1):
        gens = []
        if 0 <= it < NB:
            gens.append(stB(it))
        if 0 <= it - 1 < NB:
            gens.append(stC(it - 1))
        if 0 <= it + 1 < NB:
            gens.append(stA(it + 1))
        while gens:
            for g_ in list(gens):
                try:
                    next(g_)
                except StopIteration:
                    gens.remove(g_)
    kb.end_phase()


def phase_ffn(kb, hin_d, hout_d, W, moe, final_norm=False):
    nc = kb.nc
    if moe:
        NE, FF = 8, 3584
        wg_d, wu_d, wd_d = W["moe_w_gate"][0], W["moe_w_up"][0], W["moe_w_down"][0]
        gname = "g_ffn1_bc"
    else:
        NE, FF = 1, 2816
        wg_d, wu_d, wd_d = W["ffn_w_gate"], W["ffn_w_up"], W["ffn_w_down"]
        gname = "g_ffn0_bc"
    T = 2048
    NT = T // 128
    FB = 512
    blocks = []
    f0 = 0
    while f0 < FF:
        blocks.append((f0, min(FB, FF - f0)))
        f0 += FB
    kb.begin_phase()
    sq = kb.phase_dma_sem
    gbc = kb.sb("gbc", [128, 1024], F32); b_gbc = kb.buf()
    idf = kb.sb("idf", [128, 128], F32); b_idf = kb.buf()
    kb.op("sp", lambda e: e.dma_start(out=gbc[:], in_=W[gname]), writes=[b_gbc], dma_sem=sq())
    kb.op("sp", lambda e: e.dma_start(out=idf[:], in_=W["ident"]), writes=[b_idf], dma_sem=sq())
    if final_norm:
        gfin = kb.sb("gfin", [128, 1024], F32); b_gfin = kb.buf()
        kb.op("sp", lambda e: e.dma_start(out=gfin[:], in_=W["g_final_bc"]), writes=[b_gfin], dma_sem=sq())
    if moe:
        wr = kb.sb("wr", [128, 8, 8], F32); b_wr = kb.buf()
        rb = kb.sb("rb", [128, 8], F32); b_rb = kb.buf()
        kb.op("sp", lambda e: e.dma_start(out=wr[:], in_=W["router_w_l"]), writes=[b_wr], dma_sem=sq())
        kb.op("sp", lambda e: e.dma_start(out=rb[:], in_=W["router_b_bc"]), writes=[b_rb], dma_sem=sq())
        xf = kb.sb("xf", [128, 8, 128], F32); b_xf = kb.buf()
        lg = kb.sb("lg", [128, NT, 8], F32); b_lg = kb.buf()
        gates = kb.sb("gates", [128, NT, 8], F32); b_gates = kb.buf()
        m1 = kb.sb("m1", [128, NT], F32); m2 = kb.sb("m2", [128, NT], F32)
        mk1 = kb.sb("mk1", [128, NT, 8], F32); mk2 = kb.sb("mk2", [128, NT, 8], F32); l2 = kb.sb("l2", [128, NT, 8], F32)
        w1 = kb.sb("w1", [128, NT], F32); w2 = kb.sb("w2", [128, NT], F32)
        b_gt = kb.buf()
    acc = kb.sb("acc", [128, NT, 1024], F32); b_acc = [kb.buf() for _ in range(NT)]
    xnT = kb.sb("xnT", [128, 8, T], BF16); b_xnT = [kb.buf() for _ in range(T // 512)]
    NIF = 3
    hns = [kb.sb("hn%d" % i, [128, 1024], F32) for i in range(NIF)]; b_hns = [kb.buf() for _ in range(NIF)]
    sss = [kb.sb("ss%d" % i, [128, 1], F32) for i in range(NIF)]; b_sss = [kb.buf() for _ in range(NIF)]
    rstds = [kb.sb("rstd%d" % i, [128, 1], F32) for i in range(NIF)]; b_rstds = [kb.buf() for _ in range(NIF)]
    junks = [kb.sb("junk%d" % i, [128, 1024], BF16) for i in range(NIF)]; b_junks = [kb.buf() for _ in range(NIF)]
    if moe:
        xfs = [xf] + [kb.sb("xf%d" % i, [128, 8, 128], F32) for i in range(1, NIF)]; b_xfs = [b_xf] + [kb.buf() for _ in range(1, NIF)]
    NW = 2
    wgb = [kb.sb("wgb%d" % i, [128, 8, FB], BF16) for i in range(NW)]
    wub = [kb.sb("wub%d" % i, [128, 8, FB], BF16) for i in range(NW)]
    wdb = [kb.sb("wdb%d" % i, [128, FB // 128, 1024], BF16) for i in range(NW)]
    b_wgb = [kb.buf() for _ in range(NW)]; b_wub = [kb.buf() for _ in range(NW)]; b_wdb = [kb.buf() for _ in range(NW)]
    s_wg = [sq() for _ in range(NW)]; s_wu = [sq() for _ in range(NW)]; s_wd = [sq() for _ in range(NW)]
    sg = [kb.sb("sg%d" % i, [128, 512], BF16) for i in range(2)]; b_sg = [kb.buf() for _ in range(2)]
    hm = [kb.sb("hm%d" % i, [128, FB // 128, 512], BF16) for i in range(2)]; b_hm = [[kb.buf() for _ in range(FB // 128)] for _ in range(2)]
    s_ld = [sq() for _ in range(4)]; s_st = [sq() for _ in range(4)]
    pG = [kb.ps("pG%d" % i, [128, 512], F32) for i in range(2)]; b_pG = [kb.pbuf() for _ in range(2)]
    pU = [kb.ps("pU%d" % i, [128, 512], F32) for i in range(2)]; b_pU = [kb.pbuf() for _ in range(2)]
    pY = [kb.ps("pY%d" % i, [128, 512], F32) for i in range(2)]; b_pY = [kb.pbuf() for _ in range(2)]
    pTa = kb.ps("pTa", [128, 512], F32); b_pTa = kb.pbuf()
    pTb = kb.ps("pTb", [128, 512], F32); b_pTb = kb.pbuf()
    SLOT_T = [((pTa, b_pTa), (pTb, b_pTb)), ((pG[0], b_pG[0]), (pG[1], b_pG[1])), ((pU[0], b_pU[0]), (pU[1], b_pU[1]))]
    SLOT_L = [(pY[0][:, 0:8], b_pY[0]), (pY[1][:, 0:8], b_pY[1]), (pY[0][:, 8:16], b_pY[0])]

    def run_lockstep(gens):
        while gens:
            for g_ in list(gens):
                try:
                    next(g_)
                except StopIteration:
                    gens.remove(g_)

    def norm_gen(src, b_src, gain, b_gain, i):
        hn_, ss_, rstd_, junk_ = hns[i], sss[i], rstds[i], junks[i]
        kb.op("act", lambda e: e.activation(out=junk_[:], in_=src, func=AF.Square, accum_out=ss_[:, 0:1]), reads=[b_src], writes=[b_junks[i], b_sss[i]])
        yield
        kb.op("dve", lambda e: e.tensor_scalar(out=rstd_[:], in0=ss_[:], scalar1=1.0 / D, scalar2=EPS, op0=ALU.mult, op1=ALU.add), reads=[b_sss[i]], writes=[b_rstds[i]])
        yield
        kb.op("act", lambda e: e.activation(out=rstd_[:], in_=rstd_[:], func=AF.Sqrt), reads=[b_rstds[i]], writes=[b_rstds[i]])
        yield
        kb.op("dve", lambda e: e.reciprocal(out=rstd_[:], in_=rstd_[:]), reads=[b_rstds[i]], writes=[b_rstds[i]])
        yield
        kb.op("dve", lambda e: e.scalar_tensor_tensor(out=hn_[:], in0=src, scalar=rstd_[:, 0:1], in1=gain[:], op0=ALU.mult, op1=ALU.mult),
              reads=[b_src, b_rstds[i], b_gain], writes=[b_hns[i]])
        yield

    def pro_gen(t, i, t0):
        r0 = t0 + t * 128
        kb.op("sp", lambda e: e.dma_start(out=acc[:, t, :], in_=hin_d[r0:r0 + 128, :]), writes=[b_acc[t]], dma_sem=s_ld[i])
        yield
        for _ in norm_gen(acc[:, t, :], b_acc[t], gbc, b_gbc, i):
            yield
        hn_ = hns[i]
        for half in range(2):
            pt_, bpt_ = SLOT_T[i][half]
            for k4 in range(4):
                k = half * 4 + k4
                kb.op("pe", lambda e, k=k, k4=k4, pt_=pt_: e.transpose(pt_[:, k4 * 128:(k4 + 1) * 128], hn_[:, k * 128:(k + 1) * 128], idf[:]),
                      reads=[b_hns[i], b_idf], writes=[bpt_])
        yield
        for half in range(2):
            pt_, bpt_ = SLOT_T[i][half]
            kb.op("act", lambda e, half=half, pt_=pt_: e.copy(out=xnT[:, half * 4:(half + 1) * 4, t * 128:(t + 1) * 128], in_=pt_[:].rearrange("p (b t) -> p b t", t=128)),
                  reads=[bpt_], writes=[b_xnT[t // 4]])
            if moe:
                kb.op("dve", lambda e, half=half, pt_=pt_: e.tensor_copy(out=xfs[i][:, half * 4:(half + 1) * 4, :], in_=pt_[:].rearrange("p (b t) -> p b t", t=128)),
                      reads=[bpt_], writes=[b_xfs[i]])
        yield
        if moe:
            pL_, bpL_ = SLOT_L[i]
            for k in range(8):
                kb.op("pe", lambda e, k=k: e.matmul(pL_, xfs[i][:, k, :], wr[:, k, :], start=(k == 0), stop=(k == 7)),
                      reads=[b_xfs[i], b_wr], writes=[bpL_])
            yield
            kb.op("dve", lambda e: e.tensor_tensor(out=lg[:, t, :], in0=pL_, in1=rb[:], op=ALU.add), reads=[bpL_, b_rb], writes=[b_lg])
            yield

    def epi_gen(t, i, t0):
        r0 = t0 + t * 128
        for _ in norm_gen(acc[:, t, :], b_acc[t], gfin, b_gfin, i):
            yield
        kb.op("sp", lambda e: e.dma_start(out=hout_d[r0:r0 + 128, :], in_=hns[i][:]), reads=[b_hns[i]], dma_sem=s_st[i])
        yield

    wcount = 0
    for half in range(S // T):
        t0 = half * T
        for base in range(0, NT, NIF):
            run_lockstep([pro_gen(t, t - base, t0) for t in range(base, min(base + NIF, NT))])
        if moe:
            R = [b_lg, b_gt]
            bc = lambda a: a[:].unsqueeze(2).to_broadcast([128, NT, 8])
            kb.op("dve", lambda e: e.tensor_reduce(out=m1[:], in_=lg[:], axis=AX.X, op=ALU.max), reads=R, writes=[b_gt])
            kb.op("dve", lambda e: e.tensor_tensor(out=mk1[:], in0=lg[:], in1=bc(m1), op=ALU.is_equal), reads=R, writes=[b_gt])
            kb.op("dve", lambda e: e.scalar_tensor_tensor(out=l2[:], in0=mk1[:], scalar=-1e30, in1=lg[:], op0=ALU.mult, op1=ALU.add), reads=R, writes=[b_gt])
            kb.op("dve", lambda e: e.tensor_reduce(out=m2[:], in_=l2[:], axis=AX.X, op=ALU.max), reads=R, writes=[b_gt])
            kb.op("dve", lambda e: e.tensor_tensor(out=mk2[:], in0=l2[:], in1=bc(m2), op=ALU.is_equal), reads=R, writes=[b_gt])
            kb.op("dve", lambda e: e.tensor_tensor(out=w2[:], in0=m2[:], in1=m1[:], op=ALU.subtract), reads=R, writes=[b_gt])
            kb.op("act", lambda e: e.activation(out=w2[:], in_=w2[:], func=AF.Exp), reads=R, writes=[b_gt])
            kb.op("dve", lambda e: e.tensor_scalar(out=w1[:], in0=w2[:], scalar1=1.0, scalar2=None, op0=ALU.add), reads=R, writes=[b_gt])
            kb.op("dve", lambda e: e.reciprocal(out=w1[:], in_=w1[:]), reads=R, writes=[b_gt])
            kb.op("dve", lambda e: e.tensor_tensor(out=w2[:], in0=w2[:], in1=w1[:], op=ALU.mult), reads=R, writes=[b_gt])
            kb.op("dve", lambda e: e.tensor_tensor(out=mk1[:], in0=mk1[:], in1=bc(w1), op=ALU.mult), reads=R, writes=[b_gt])
            kb.op("dve", lambda e: e.tensor_tensor(out=mk2[:], in0=mk2[:], in1=bc(w2), op=ALU.mult), reads=R, writes=[b_gt])
            kb.op("dve", lambda e: e.tensor_tensor(out=gates[:], in0=mk1[:], in1=mk2[:], op=ALU.add), reads=R, writes=[b_gates])
        for ex in range(NE):
            for (f0, fsz) in blocks:
                nch = fsz // 128
                wi = wcount % NW
                wcount += 1
                gsrc = wg_d[ex][:, f0:f0 + fsz].rearrange("(k p) f -> p k f", p=128)
                usrc = wu_d[ex][:, f0:f0 + fsz].rearrange("(k p) f -> p k f", p=128)
                dsrc = wd_d[ex][f0:f0 + fsz, :].rearrange("(c p) d -> p c d", p=128)
                kb.op("pool", lambda e, wi=wi, gsrc=gsrc, fsz=fsz: e.dma_start(out=wgb[wi][:, :, 0:fsz], in_=gsrc), writes=[b_wgb[wi]], dma_sem=s_wg[wi])
                kb.op("pool", lambda e, wi=wi, usrc=usrc, fsz=fsz: e.dma_start(out=wub[wi][:, :, 0:fsz], in_=usrc), writes=[b_wub[wi]], dma_sem=s_wu[wi])
                kb.op("pool", lambda e, wi=wi, dsrc=dsrc, nch=nch: e.dma_start(out=wdb[wi][:, 0:nch, :], in_=dsrc), writes=[b_wdb[wi]], dma_sem=s_wd[wi])
                for tb in range(T // 512):
                    hi = tb % 2
                    for c in range(nch):
                        pi = c % 2
                        for k in range(8):
                            kb.op("pe", lambda e, wi=wi, c=c, k=k, pi=pi, tb=tb: e.matmul(pG[pi][:], wgb[wi][:, k, c * 128:(c + 1) * 128],
                                                                                          xnT[:, k, tb * 512:(tb + 1) * 512], start=(k == 0), stop=(k == 7)),
                                  reads=[b_wgb[wi], b_xnT[tb]], writes=[b_pG[pi]])
                        for k in range(8):
                            kb.op("pe", lambda e, wi=wi, c=c, k=k, pi=pi, tb=tb: e.matmul(pU[pi][:], wub[wi][:, k, c * 128:(c + 1) * 128],
                                                                                          xnT[:, k, tb * 512:(tb + 1) * 512], start=(k == 0), stop=(k == 7)),
                                  reads=[b_wub[wi], b_xnT[tb]], writes=[b_pU[pi]])
                        kb.op("act", lambda e, pi=pi: e.activation(out=sg[pi][:], in_=pG[pi][:], func=AF.Silu), reads=[b_pG[pi]], writes=[b_sg[pi]])
                        kb.op("dve", lambda e, pi=pi, hi=hi, c=c: e.tensor_tensor(out=hm[hi][:, c, :], in0=sg[pi][:], in1=pU[pi][:], op=ALU.mult),
                              reads=[b_sg[pi], b_pU[pi]], writes=[b_hm[hi][c]])
                    for j in range(4):
                        t = tb * 4 + j
                        for dh in range(2):
                            yi = (j * 2 + dh) % 2
                            for c in range(nch):
                                kb.op("pe", lambda e, hi=hi, c=c, j=j, dh=dh, yi=yi, wi=wi, nch=nch: e.matmul(
                                    pY[yi][:], hm[hi][:, c, j * 128:(j + 1) * 128], wdb[wi][:, c, dh * 512:(dh + 1) * 512],
                                    start=(c == 0), stop=(c == nch - 1)),
                                    reads=[b_hm[hi][c], b_wdb[wi]], writes=[b_pY[yi]])
                            if moe:
                                kb.op("dve", lambda e, t=t, dh=dh, yi=yi, ex=ex: e.scalar_tensor_tensor(
                                    out=acc[:, t, dh * 512:(dh + 1) * 512], in0=pY[yi][:], scalar=gates[:, t, ex:ex + 1],
                                    in1=acc[:, t, dh * 512:(dh + 1) * 512], op0=ALU.mult, op1=ALU.add),
                                    reads=[b_pY[yi], b_gates, b_acc[t]], writes=[b_acc[t]])
                            else:
                                kb.op("dve", lambda e, t=t, dh=dh, yi=yi: e.tensor_tensor(
                                    out=acc[:, t, dh * 512:(dh + 1) * 512], in0=pY[yi][:], in1=acc[:, t, dh * 512:(dh + 1) * 512], op=ALU.add),
                                    reads=[b_pY[yi], b_acc[t]], writes=[b_acc[t]])
        if final_norm:
            for base in range(0, NT, NIF):
                run_lockstep([epi_gen(t, t - base, t0) for t in range(base, min(base + NIF, NT))])
        else:
            for t in range(NT):
                r0 = t0 + t * 128
                kb.op("sp", lambda e, t=t, r0=r0: e.dma_start(out=hout_d[r0:r0 + 128, :], in_=acc[:, t, :]), reads=[b_acc[t]], dma_sem=s_st[t % 4])
    kb.end_phase()


def phase_dn(kb, hin_d, hout_d, W):
    nc = kb.nc
    kb.begin_phase()
    sq = kb.phase_dma_sem
    NT = S // 128
    win_d = W["dn_w_in"][0]

    def P(eng, fn, r=(), w=()):
        return kb.op(eng, fn, reads=r, writes=w)

    def const(name, shape, src, eng="sp"):
        t = kb.sb(name, shape, F32); b = kb.buf()
        kb.op(eng, lambda e: e.dma_start(out=t[:], in_=src), writes=[b], dma_sem=sq())
        return t, b

    gbc, b_gbc = const("gbc", [128, 1024], W["g_mix1_bc"])
    idf, b_idf = const("idf", [128, 128], W["ident"])
    ngbc, b_ngbc = const("ngbc", [128, 1024], W["dn_norm_g_bc"])
    cw, b_cw = const("cw", [128, 24, 4], W["conv_wl"])
    alog, b_alog = const("alog", [128, 8], W["a_log_bc"])
    dtb, b_dtb = const("dtb", [128, 8], W["dt_bias_bc"])
    trit, b_trit = const("trit", [128, 128], W["trit"])
    bones, b_bones = const("bones", [128, 128], W["bones"])
    csel, b_csel = const("csel", [128, 2, 128], W["csel"])
    lvm, b_lvm = const("lvm", [128, 6, 128], W["lvm"])
    muin, b_muin = const("muin", [128, 128], W["muin"])
    id4, b_id4 = const("id4", [128, 4, 128], W["id4"])
    idb = kb.sb("idb", [128, 128], BF16); b_idb = kb.buf()
    kb.op("pool", lambda e: e.dma_start(out=idb[:], in_=W["ident"]), writes=[b_idb], dma_sem=sq())
    wout = kb.sb("wout", [128, 8, 1024], BF16); b_wout = kb.buf()
    kb.op("pool", lambda e: e.dma_start(out=wout[:], in_=W["dn_w_out"][0].rearrange("(k p) n -> p k n", p=128)), writes=[b_wout], dma_sem=sq())
    wba = kb.sb("wba", [128, 8, 16], BF16); b_wba = kb.buf()
    kb.op("pool", lambda e: e.dma_start(out=wba[:], in_=W["wba_l"]), writes=[b_wba], dma_sem=sq())
    nega = kb.sb("nega", [128, 8], F32); b_nega = kb.buf()
    P("act", lambda e: e.activation(out=nega[:], in_=alog[:], func=AF.Exp), [b_alog], [b_nega])
    P("dve", lambda e: e.tensor_scalar(out=nega[:], in0=nega[:], scalar1=-1.0, scalar2=None, op0=ALU.mult), [b_nega], [b_nega])

    xt = kb.sb("xt", [128, 1024], F32); b_xt = kb.buf(); s_xt = sq()
    hn = kb.sb("hn", [128, 1024], F32); b_hn = kb.buf()
    hnT = kb.sb("hnT", [128, 8, 128], BF16); b_hnT = kb.buf()
    ss = kb.sb("ss", [128, 1], F32); b_ss = kb.buf()
    rstd = kb.sb("rstd", [128, 1], F32); b_rstd = kb.buf()
    junk = kb.sb("junk", [128, 128], BF16); b_junk = kb.buf()
    junkD = kb.sb("junkD", [128, 128], BF16); b_junkD = kb.buf()
    NWB = 2
    wst = [kb.sb("wst%d" % i, [128, 8, 512], BF16) for i in range(NWB)]; b_wst = [kb.buf() for _ in range(NWB)]
    s_wst = [sq() for _ in range(NWB)]
    hist = kb.sb("hist", [128, 24, 3], F32); b_hist = [kb.buf() for _ in range(24)]
    xcw = [kb.sb("xcw%d" % i, [128, 131], F32) for i in range(2)]; b_xcw = [kb.buf() for _ in range(2)]
    cv = [kb.sb("cv%d" % i, [128, 128], F32) for i in range(2)]; b_cv = [kb.buf() for _ in range(2)]
    sv = [kb.sb("sv%d" % i, [128, 128], F32) for i in range(2)]; b_sv = [kb.buf() for _ in range(2)]
    toks = [kb.sb("tok%d" % i, [128, 3072], F32) for i in range(2)]; b_toks = [[kb.buf() for _ in range(6)] for _ in range(2)]
    zss = [kb.sb("zs%d" % i, [128, 1024], BF16) for i in range(3)]; b_zss = [kb.buf() for _ in range(3)]
    scs = [kb.sb("sc%d" % i, [128, 16, 8], F32) for i in range(2)]; b_scs = [kb.buf() for _ in range(2)]
    (I_EB, I_SB, I_X, I_G, I_GC, I_EGC, I_EDEC, I_RQ, I_RK, I_SKB, I_SKG, I_SKD, I_SQG, I_TMP, I_NG, I_RO) = range(16)
    qkss = kb.sb("qkss", [128, 16], F32); b_qkss = kb.buf()
    qkssD = kb.sb("qkssD", [128, 8], F32); b_qkssD = kb.buf()
    scD = kb.sb("scD", [128, 8], F32); b_scD = kb.buf()
    egls = [kb.sb("egl%d" % i, [128, 2, 8], F32) for i in range(2)]; b_egls = [kb.buf() for _ in range(2)]
    GB = []
    for gq_ in range(2):
        dct = {}
        for n_ in ['kbt', 'knt', 'kgt', 'kdt', 'qnt', 'qgt', 'kbT', 'knT', 'qnT', 'QG0', 'QG1', 'GT', 'NGT', 'Dm', 'Dn', 'L3', 'qkT', 'W0', 'W1', 'vn']:
            dct[n_] = (kb.sb("%s_%d" % (n_, gq_), [128, 4, 128], BF16 if n_ in ('kbt', 'knt', 'qnt', 'kbT', 'knT', 'qnT') else F32), kb.buf())
        GB.append(dct)
    BALL = [(kb.sb("Ball%d" % g_, [128, 6, 4, 128], BF16), kb.buf()) for g_ in range(2)]
    St = [[kb.sb("S%d_%d" % (g, i), [128, 4, 128], F32) for i in range(3)] for g in range(2)]
    b_St = [[kb.buf() for i in range(3)] for g in range(2)]
    otoks = [kb.sb("otok%d" % i, [128, 1024], F32) for i in range(2)]; b_otoks = [[kb.buf() for _ in range(2)] for _ in range(2)]
    ogT = kb.sb("ogT", [128, 8, 128], BF16); b_ogT = kb.buf()
    res = kb.sb("res", [128, 1024], F32); b_res = kb.buf(); s_res = sq(); s_out = sq()

    pX = kb.ps("pX", [128, 4, 128], F32); b_pX = kb.pbuf()
    pYk = kb.ps("pYk", [128, 4, 128], F32); b_pYk = kb.pbuf()
    pG = [kb.ps("pG%d" % i, [128, 4, 128], F32) for i in range(4)]; b_pG = [kb.pbuf() for _ in range(4)]
    pD1 = kb.ps("pD1", [128, 4, 128], F32); b_pD1 = kb.pbuf()
    pS = kb.ps("pS", [128, 64], F32); b_pS = kb.pbuf()

    for g in range(2):
        for i in range(3):
            P("pool", lambda e, g=g, i=i: e.memset(St[g][i][:], 0.0), [], [b_St[g][i]])
    P("pool", lambda e: e.memset(hist[:], 0.0), [], b_hist)
    for gq_ in range(2):
        for n_ in ("QG0", "QG1", "W0", "W1"):
            t_, b_ = GB[gq_][n_]
            P("pool", lambda e, t_=t_: e.memset(t_[:], 0.0), [], [b_])

    GSLOT = [[(pG[0][:], b_pG[0]), (pG[1][:], b_pG[1])], [(pG[2][:], b_pG[2]), (pG[3][:], b_pG[3])]]
    gctr = [0, 0]
    wcount = [0]
    sidx = [0, 0]

    def stageAB(t):
        p = t % 2
        tok = toks[p]; b_tok = b_toks[p]
        zs = zss[t % 3]; b_zs = b_zss[t % 3]
        sc = scs[p]; b_sc = b_scs[p]; egl = egls[p]; b_egl = b_egls[p]
        r0 = t * 128
        kb.op("sp", lambda e: e.dma_start(out=xt[:], in_=hin_d[r0:r0 + 128, :]), writes=[b_xt], dma_sem=s_xt)
        P("act", lambda e: e.activation(out=hn[:], in_=xt[:], func=AF.Square, accum_out=ss[:, 0:1]), [b_xt], [b_hn, b_ss])
        P("dve", lambda e: e.tensor_scalar(out=rstd[:], in0=ss[:], scalar1=1.0 / D, scalar2=EPS, op0=ALU.mult, op1=ALU.add), [b_ss], [b_rstd])
        P("act", lambda e: e.activation(out=rstd[:], in_=rstd[:], func=AF.Ln), [b_rstd], [b_rstd])
        P("act", lambda e: e.activation(out=rstd[:], in_=rstd[:], func=AF.Exp, scale=-0.5), [b_rstd], [b_rstd])
        yield
        P("dve", lambda e: e.scalar_tensor_tensor(out=hn[:], in0=xt[:], scalar=rstd[:, 0:1], in1=gbc[:], op0=ALU.mult, op1=ALU.mult),
          [b_xt, b_rstd, b_gbc], [b_hn])
        yield
        for half, (pt_, bpt_) in enumerate(((pX, b_pX), (pYk, b_pYk))):
            for k4 in range(4):
                k = half * 4 + k4
                P("pe", lambda e, k=k, k4=k4, pt_=pt_: e.transpose(pt_[:, k4, :], hn[:, k * 128:(k + 1) * 128], idf[:]), [b_hn, b_idf], [bpt_])
            P("act", lambda e, half=half, pt_=pt_: e.copy(out=hnT[:, half * 4:(half + 1) * 4, :], in_=pt_[:]), [bpt_], [b_hnT])
            yield
        wis = {}
        for step in range(24 + 2):
            cc = step
            if cc < 24:
                grp_ = cc // 4; c4 = cc % 4
                if c4 == 0:
                    wi = wcount[0] % NWB; wcount[0] += 1
                    wis[grp_] = wi
                    src = win_d[:, grp_ * 512:(grp_ + 1) * 512].rearrange("(k p) f -> p k f", p=128)
                    kb.op("pool", lambda e, wi=wi, src=src: e.dma_start(out=wst[wi][:], in_=src), writes=[b_wst[wi]], dma_sem=s_wst[wi])
                wi = wis[grp_]
                for k in range(8):
                    P("pe", lambda e, wi=wi, c4=c4, k=k: e.matmul(pX[:, c4, :], wst[wi][:, k, c4 * 128:(c4 + 1) * 128], hnT[:, k, :], start=(k == 0), stop=(k == 7)),
                      [b_wst[wi], b_hnT], [b_pX])
                xi = cc % 2
                xc = xcw[xi]; bxc = b_xcw[xi]
                P("pool", lambda e, xc=xc, cc=cc: e.tensor_copy(out=xc[:, 0:3], in_=hist[:, cc, :]), [b_hist[cc]], [bxc])
                P("act", lambda e, xc=xc, c4=c4: e.copy(out=xc[:, 3:131], in_=pX[:, c4, :]), [b_pX], [bxc])
            cc = step - 1
            if 0 <= cc < 24:
                xi = cc % 2
                xc = xcw[xi]; bxc = b_xcw[xi]
                P("pool", lambda e, xc=xc, cc=cc: e.tensor_copy(out=hist[:, cc, :], in_=xc[:, 128:131]), [bxc], [b_hist[cc]])
                cvb = cv[xi]; bcv = b_cv[xi]
                P("dve", lambda e, xc=xc, cc=cc, cvb=cvb: e.tensor_scalar(out=cvb[:], in0=xc[:, 0:128], scalar1=cw[:, cc, 0:1], scalar2=None, op0=ALU.mult),
                  [bxc, b_cw], [bcv])
                for jj in range(1, 4):
                    P("dve", lambda e, xc=xc, cc=cc, cvb=cvb, jj=jj: e.scalar_tensor_tensor(out=cvb[:], in0=xc[:, jj:jj + 128], scalar=cw[:, cc, jj:jj + 1], in1=cvb[:],
                                                                                             op0=ALU.mult, op1=ALU.add),
                      [bxc, b_cw, bcv], [bcv])
                svb = sv[xi]; bsv = b_sv[xi]
                P("act", lambda e, cvb=cvb, svb=svb: e.activation(out=svb[:], in_=cvb[:], func=AF.Silu), [bcv], [bsv])
            cc = step - 2
            if 0 <= cc < 24:
                xi = cc % 2; c4 = cc % 4; grp_ = cc // 4
                svb = sv[xi]; bsv = b_sv[xi]
                P("pe", lambda e, svb=svb, c4=c4: e.transpose(pYk[:, c4, :], svb[:], idf[:]), [bsv, b_idf], [b_pYk])
                if c4 == 3:
                    if grp_ % 2 == 0:
                        P("dve", lambda e, grp_=grp_: e.tensor_copy(out=tok[:, grp_ * 512:(grp_ + 1) * 512], in_=pYk[:].rearrange("p c d -> p (c d)")), [b_pYk], [b_tok[grp_]])
                    else:
                        P("act", lambda e, grp_=grp_: e.copy(out=tok[:, grp_ * 512:(grp_ + 1) * 512], in_=pYk[:].rearrange("p c d -> p (c d)")), [b_pYk], [b_tok[grp_]])
            yield
        for zh in range(2):
            wi = wcount[0] % NWB; wcount[0] += 1
            src = win_d[:, 3072 + zh * 512:3072 + (zh + 1) * 512].rearrange("(k p) f -> p k f", p=128)
            kb.op("pool", lambda e, wi=wi, src=src: e.dma_start(out=wst[wi][:], in_=src), writes=[b_wst[wi]], dma_sem=s_wst[wi])
            for k in range(8):
                P("pe", lambda e, wi=wi, k=k: e.matmul(pX[:].rearrange("p c d -> p (c d)"), hnT[:, k, :], wst[wi][:, k, :], start=(k == 0), stop=(k == 7)),
                  [b_wst[wi], b_hnT], [b_pX])
            P("act", lambda e, zh=zh: e.activation(out=zs[:, zh * 512:(zh + 1) * 512], in_=pX[:].rearrange("p c d -> p (c d)"), func=AF.Silu), [b_pX], [b_zs])
            yield
        for k in range(8):
            P("pe", lambda e, k=k: e.matmul(pS[:, 0:16], hnT[:, k, :], wba[:, k, :], start=(k == 0), stop=(k == 7)), [b_hnT, b_wba], [b_pS])
        R = [b_sc]
        P("act", lambda e: e.activation(out=sc[:, I_EB, :], in_=pS[:, 0:8], func=AF.Exp, scale=-1.0), [b_pS] + R, R)
        P("dve", lambda e: e.tensor_tensor(out=sc[:, I_X, :], in0=pS[:, 8:16], in1=dtb[:], op=ALU.add), [b_pS, b_dtb] + R, R)
        yield
        P("dve", lambda e: e.tensor_scalar(out=sc[:, I_EB, :], in0=sc[:, I_EB, :], scalar1=1.0, scalar2=None, op0=ALU.add), R, R)
        P("act", lambda e: e.activation(out=sc[:, I_SB, :], in_=sc[:, I_EB, :], func=AF.Ln), R, R)
        P("act", lambda e: e.activation(out=sc[:, I_SB, :], in_=sc[:, I_SB, :], func=AF.Exp, scale=-0.5), R, R)
        yield
        P("act", lambda e: e.activation(out=sc[:, I_X, :], in_=sc[:, I_X, :], func=AF.Exp), R, R)
        P("dve", lambda e: e.tensor_scalar(out=sc[:, I_X, :], in0=sc[:, I_X, :], scalar1=1.0, scalar2=None, op0=ALU.add), R, R)
        P("act", lambda e: e.activation(out=sc[:, I_X, :], in_=sc[:, I_X, :], func=AF.Ln), R, R)
        yield
        P("dve", lambda e: e.tensor_tensor(out=sc[:, I_G, :], in0=sc[:, I_X, :], in1=nega[:], op=ALU.mult), R + [b_nega], R)
        P("pe", lambda e: e.matmul(pS[:, 16:24], trit[:], sc[:, I_G, :], start=True, stop=True), R + [b_trit], [b_pS])
        P("pe", lambda e: e.matmul(pS[:, 24:32], bones[:], sc[:, I_G, :], start=True, stop=True), R + [b_bones], [b_pS])
        for c in range(2):
            P("pe", lambda e, c=c: e.matmul(pS[:, 32 + 8 * c:40 + 8 * c], csel[:, c, :], sc[:, I_G, :], start=True, stop=True), R + [b_csel], [b_pS])
        yield
        P("act", lambda e: e.copy(out=sc[:, I_GC, :], in_=pS[:, 16:24]), [b_pS] + R, R)
        P("act", lambda e: e.activation(out=sc[:, I_EGC, :], in_=pS[:, 16:24], func=AF.Exp), [b_pS] + R, R)
        P("dve", lambda e: e.tensor_tensor(out=sc[:, I_EDEC, :], in0=pS[:, 24:32], in1=sc[:, I_GC, :], op=ALU.subtract), [b_pS] + R, R)
        yield
        P("act", lambda e: e.activation(out=sc[:, I_EDEC, :], in_=sc[:, I_EDEC, :], func=AF.Exp), R, R)
        P("act", lambda e: e.activation(out=egl[:].rearrange("p c h -> p (c h)"), in_=pS[:, 32:48], func=AF.Exp), [b_pS], [b_egl])
        yield
        for hh in range(16):
            P("act", lambda e, hh=hh: e.activation(out=junk[:], in_=tok[:, hh * 128:(hh + 1) * 128], func=AF.Square, accum_out=qkss[:, hh:hh + 1]),
              [b_tok[hh // 4]], [b_junk, b_qkss])
            if hh % 4 == 3:
                yield
        P("dve", lambda e: e.tensor_scalar(out=sc[:, I_RQ, :], in0=qkss[:, 0:8], scalar1=EPS, scalar2=128.0, op0=ALU.add, op1=ALU.mult), [b_qkss] + R, R)
        P("dve", lambda e: e.tensor_scalar(out=sc[:, I_RK, :], in0=qkss[:, 8:16], scalar1=EPS, scalar2=None, op0=ALU.add), [b_qkss] + R, R)
        P("act", lambda e: e.activation(out=sc[:, I_RQ:I_RK + 1, :], in_=sc[:, I_RQ:I_RK + 1, :], func=AF.Ln), R, R)
        P("act", lambda e: e.activation(out=sc[:, I_RQ:I_RK + 1, :], in_=sc[:, I_RQ:I_RK + 1, :], func=AF.Exp, scale=-0.5), R, R)
        yield
        P("dve", lambda e: e.tensor_tensor(out=sc[:, I_SKB, :], in0=sc[:, I_RK, :], in1=sc[:, I_SB, :], op=ALU.mult), R, R)
        P("dve", lambda e: e.tensor_tensor(out=sc[:, I_SKG, :], in0=sc[:, I_RK, :], in1=sc[:, I_EGC, :], op=ALU.mult), R, R)
        P("dve", lambda e: e.tensor_tensor(out=sc[:, I_SKD, :], in0=sc[:, I_RK, :], in1=sc[:, I_EDEC, :], op=ALU.mult), R, R)
        P("dve", lambda e: e.tensor_tensor(out=sc[:, I_SQG, :], in0=sc[:, I_RQ, :], in1=sc[:, I_EGC, :], op=ALU.mult), R, R)
        P("dve", lambda e: e.tensor_scalar(out=sc[:, I_NG, :], in0=sc[:, I_G, :], scalar1=-1.0, scalar2=None, op0=ALU.mult), R, R)
        yield

    def grp(gq, t):
        p = t % 2
        sc = scs[p]; b_sc = b_scs[p]; egl = egls[p]; b_egl = b_egls[p]; otok = otoks[p]; b_otok = b_otoks[p]
        def bc4(col, gq, lo=0, hi=128):
            return sc[lo:hi, col, gq * 4:(gq + 1) * 4].unsqueeze(2).to_broadcast([hi - lo, 4, 128])
        def palloc():
            i_ = gctr[gq]; gctr[gq] += 1
            return GSLOT[gq][i_ % len(GSLOT[gq])]
        kbt, b_kbt = GB[gq]['kbt']
        knt, b_knt = GB[gq]['knt']
        kgt, b_kgt = GB[gq]['kgt']
        kdt, b_kdt = GB[gq]['kdt']
        qnt, b_qnt = GB[gq]['qnt']
        qgt, b_qgt = GB[gq]['qgt']
        kbT, b_kbT = GB[gq]['kbT']
        knT, b_knT = GB[gq]['knT']
        qnT, b_qnT = GB[gq]['qnT']
        QG0, b_QG0 = GB[gq]['QG0']
        QG1, b_QG1 = GB[gq]['QG1']
        GT, b_GT = GB[gq]['GT']
        NGT, b_NGT = GB[gq]['NGT']
        Dm, b_Dm = GB[gq]['Dm']
        Dn, b_Dn = GB[gq]['Dn']
        L3, b_L3 = GB[gq]['L3']
        qkT, b_qkT = GB[gq]['qkT']
        W0, b_W0 = GB[gq]['W0']
        W1, b_W1 = GB[gq]['W1']
        vn, b_vn = GB[gq]['vn']
        Ball, b_Ball = BALL[gq]
        Y, b_Y = GB[gq]['kbT']
        Qs, b_Qs = GB[gq]['kbt']
        YTs, b_YTs = GB[gq]['knt']
        Yb, b_Yb = GB[gq]['GT']
        tmpS, b_tmpS = GB[gq]['qgt']
        h0 = gq * 4
        kraw = toks[p][:, 1024 + h0 * 128:1024 + (h0 + 4) * 128].rearrange("p (h d) -> p h d", h=4)
        qraw = toks[p][:, h0 * 128:(h0 + 4) * 128].rearrange("p (h d) -> p h d", h=4)
        vraw = toks[p][:, 2048 + h0 * 128:2048 + (h0 + 4) * 128].rearrange("p (h d) -> p h d", h=4)
        bk = b_toks[p][2 + gq]; bq = b_toks[p][gq]; bv = b_toks[p][4 + gq]
        def scaled(out, bout, raw, braw, col, eng):
            in1 = bc4(col, gq)
            P(eng, lambda e: e.tensor_tensor(out=out[:], in0=raw, in1=in1, op=ALU.mult), [braw, b_sc], [bout])
        scaled(kbt, b_kbt, kraw, bk, I_SKB, "dve")
        scaled(knt, b_knt, kraw, bk, I_RK, "pool")
        scaled(qnt, b_qnt, qraw, bq, I_RQ, "dve")
        scaled(qgt, b_qgt, qraw, bq, I_SQG, "pool")
        P("pool", lambda e, gq=gq: e.tensor_tensor(out=GT[:], in0=trit[:].unsqueeze(1).to_broadcast([128, 4, 128]), in1=bc4(I_G, gq), op=ALU.mult),
          [b_trit, b_sc], [b_GT])
        P("pool", lambda e, gq=gq: e.tensor_tensor(out=NGT[:], in0=trit[:].unsqueeze(1).to_broadcast([128, 4, 128]), in1=bc4(I_NG, gq), op=ALU.mult),
          [b_trit, b_sc], [b_NGT])
        scaled(kgt, b_kgt, kraw, bk, I_SKG, "dve")
        scaled(kdt, b_kdt, kraw, bk, I_SKD, "pool")
        yield

        def tr4(src, bsrc, lowp=True):
            pb, bpb = palloc()
            for h in range(4):
                if lowp:
                    P("pe", lambda e, h=h, pb=pb: e.matmul(pb[:, h, :], src[:, h, :], idb[:], start=True, stop=True), [bsrc, b_idb], [bpb])
                else:
                    P("pe", lambda e, h=h, pb=pb: e.transpose(pb[:, h, :], src[:, h, :], idf[:]), [bsrc, b_idf], [bpb])
            return pb, bpb
        pb1, bpb1 = tr4(kbt, b_kbt)
        pb2, bpb2 = tr4(knt, b_knt)
        yield
        P("act", lambda e, pb=pb1: e.copy(out=kbT[:], in_=pb[:]), [bpb1], [b_kbT])
        P("dve", lambda e, pb=pb2: e.tensor_copy(out=knT[:], in_=pb[:]), [bpb2], [b_knT])
        yield
        pb1, bpb1 = tr4(qnt, b_qnt)
        pb2, bpb2 = tr4(qgt, b_qgt, lowp=False)
        yield
        P("act", lambda e, pb=pb1: e.copy(out=qnT[:], in_=pb[:]), [bpb1], [b_qnT])
        P("dve", lambda e, pb=pb2: e.tensor_copy(out=QG0[:, :, 0:64], in_=pb[:, :, 0:64]), [bpb2], [b_QG0])
        P("act", lambda e, pb=pb2: e.copy(out=QG1[:, :, 64:128], in_=pb[:, :, 64:128]), [bpb2], [b_QG1])
        yield
        pD, bpD = palloc()
        for h in range(4):
            P("pe", lambda e, h=h, pD=pD: e.matmul(pD[:, h, :], GT[:, h, :], bones[:], start=True, stop=False), [b_GT, b_bones], [bpD])
            P("pe", lambda e, h=h, pD=pD: e.matmul(pD[:, h, :], bones[:], NGT[:, h, :], start=False, stop=True), [b_NGT, b_bones], [bpD])
        pK, bpK = palloc()
        for h in range(4):
            P("pe", lambda e, h=h, pK=pK: e.matmul(pK[:, h, :], kbT[:, h, :], kbT[:, h, :], start=True, stop=True), [b_kbT], [bpK])
        yield
        P("dve", lambda e, pD=pD: e.tensor_scalar(out=Dm[:], in0=pD[:], scalar1=0.0, scalar2=None, op0=ALU.min), [bpD], [b_Dm])
        P("dve", lambda e, pD=pD: e.tensor_scalar(out=Dn[:], in0=pD[:], scalar1=-1.0, scalar2=0.0, op0=ALU.mult, op1=ALU.min), [bpD], [b_Dn])
        yield
        P("act", lambda e: e.activation(out=Dm[:], in_=Dm[:], func=AF.Exp), [b_Dm], [b_Dm])
        P("act", lambda e: e.activation(out=Dn[:], in_=Dn[:], func=AF.Exp), [b_Dn], [b_Dn])
        pQ, bpQ = palloc()
        for h in range(4):
            P("pe", lambda e, h=h, pQ=pQ: e.matmul(pQ[:, h, :], knT[:, h, :], qnT[:, h, :], start=True, stop=True), [b_knT, b_qnT], [bpQ])
        yield
        P("dve", lambda e, pK=pK: e.tensor_tensor(out=L3[:], in0=pK[:], in1=Dm[:], op=ALU.mult), [bpK, b_Dm], [b_L3])
        P("pool", lambda e: e.tensor_tensor(out=Dn[:], in0=Dn[:], in1=muin[:].unsqueeze(1).to_broadcast([128, 4, 128]), op=ALU.mult), [b_Dn, b_muin], [b_Dn])
        yield
        P("pool", lambda e: e.tensor_tensor(out=Ball[:], in0=L3[:].unsqueeze(1).to_broadcast([128, 6, 4, 128]),
                                            in1=lvm[:].unsqueeze(2).to_broadcast([128, 6, 4, 128]), op=ALU.mult), [b_L3, b_lvm], [b_Ball])
        P("dve", lambda e, pQ=pQ: e.tensor_tensor(out=qkT[:], in0=pQ[:], in1=Dn[:], op=ALU.mult), [bpQ, b_Dn], [b_qkT])
        yield
        for lv in range(6):
            pq, bpq = palloc()
            if lv == 0:
                for h in range(4):
                    P("pe", lambda e, h=h, pq=pq: e.matmul(pq[:, h, :], Ball[:, 0, h, :], idb[:], start=True, stop=True), [b_Ball, b_idb], [bpq])
                yield
                P("dve", lambda e, pq=pq: e.scalar_tensor_tensor(out=Y[:], in0=pq[:], scalar=-1.0, in1=id4[:], op0=ALU.mult, op1=ALU.add), [bpq, b_id4], [b_Y])
                yield
                continue
            for h in range(4):
                P("pe", lambda e, h=h, pq=pq, lv=lv: e.matmul(pq[:, h, :], Ball[:, lv, h, :], Y[:, h, :], start=True, stop=True), [b_Ball, b_Y], [bpq])
            pt, bpt = tr4(Y, b_Y)
            yield
            P("act", lambda e, pq=pq: e.copy(out=Qs[:], in_=pq[:]), [bpq], [b_Qs])
            P("dve", lambda e, pt=pt: e.tensor_copy(out=YTs[:], in_=pt[:]), [bpt], [b_YTs])
            yield
            py, bpy = palloc()
            for h in range(4):
                P("pe", lambda e, h=h, py=py: e.matmul(py[:, h, :], YTs[:, h, :], Qs[:, h, :], start=True, stop=True), [b_YTs, b_Qs], [bpy])
            yield
            P("dve", lambda e, py=py: e.tensor_tensor(out=Y[:], in0=Y[:], in1=py[:], op=ALU.subtract), [bpy, b_Y], [b_Y])
            yield
        P("dve", lambda e, gq=gq: e.tensor_tensor(out=Yb[:], in0=Y[:], in1=bc4(I_SB, gq), op=ALU.mult), [b_Y, b_sc], [b_Yb])
        yield
        pw, bpw = palloc()
        for h in range(4):
            P("pe", lambda e, h=h, pw=pw: e.matmul(pw[:, h, :], kgt[:, h, :], Yb[:, h, :], start=True, stop=True), [b_kgt, b_Yb], [bpw])
        yield
        P("dve", lambda e, pw=pw: e.tensor_scalar(out=W0[:, :, 0:64], in0=pw[:, :, 0:64], scalar1=-1.0, scalar2=None, op0=ALU.mult), [bpw], [b_W0])
        P("act", lambda e, pw=pw: e.activation(out=W1[:, :, 64:128], in_=pw[:, :, 64:128], func=AF.Identity, scale=-1.0), [bpw], [b_W1])
        yield
        si = sidx[gq]
        Sa, bSa = St[gq][si % 3], b_St[gq][si % 3]
        Sb, bSb = St[gq][(si + 1) % 3], b_St[gq][(si + 1) % 3]
        Sc_, bSc = St[gq][(si + 2) % 3], b_St[gq][(si + 2) % 3]
        sidx[gq] = si + 2
        for c, (Sin, bSin, Sout, bSout, Wc, bWc) in enumerate(((Sa, bSa, Sb, bSb, W0, b_W0), (Sb, bSb, Sc_, bSc, W1, b_W1))):
            lo, hi = c * 64, c * 64 + 64
            pv, bpv = palloc()
            for h in range(4):
                P("pe", lambda e, h=h, pv=pv, vraw=vraw: e.matmul(pv[:, h, :], Yb[:, h, :], vraw[:, h, :], start=True, stop=False), [b_Yb, bv], [bpv])
                P("pe", lambda e, h=h, pv=pv, Wc=Wc, Sin=Sin: e.matmul(pv[:, h, :], Wc[:, h, :], Sin[:, h, :], start=False, stop=True), [bWc, bSin], [bpv])
            yield
            P("dve", lambda e, pv=pv, lo=lo, hi=hi, gq=gq: e.tensor_tensor(out=vn[lo:hi], in0=pv[lo:hi], in1=bc4(I_SB, gq, lo, hi), op=ALU.mult),
              [bpv, b_sc], [b_vn])
            yield
            ps_, bps = palloc()
            for h in range(4):
                P("pe", lambda e, h=h, ps_=ps_, lo=lo, hi=hi: e.matmul(ps_[:, h, :], kdt[lo:hi, h, :], vn[lo:hi, h, :], start=True, stop=True), [b_kdt, b_vn], [bps])
            P("pool", lambda e, c=c, gq=gq, Sin=Sin: e.tensor_tensor(out=tmpS[:], in0=Sin[:], in1=egl[:, c, gq * 4:(gq + 1) * 4].unsqueeze(2).to_broadcast([128, 4, 128]), op=ALU.mult),
              [bSin, b_egl], [b_tmpS])
            yield
            P("dve", lambda e, ps_=ps_, Sout=Sout: e.tensor_tensor(out=Sout[:], in0=tmpS[:], in1=ps_[:], op=ALU.add), [b_tmpS, bps], [bSout])
            yield
        pO, bpO = palloc()
        for h in range(4):
            P("pe", lambda e, h=h, pO=pO, Sa=Sa: e.matmul(pO[:, h, :], QG0[:, h, :], Sa[:, h, :], start=True, stop=False), [b_QG0, bSa], [bpO])
            P("pe", lambda e, h=h, pO=pO, Sb=Sb: e.matmul(pO[:, h, :], QG1[:, h, :], Sb[:, h, :], start=False, stop=False), [b_QG1, bSb], [bpO])
            P("pe", lambda e, h=h, pO=pO: e.matmul(pO[:, h, :], qkT[:, h, :], vn[:, h, :], start=False, stop=True), [b_qkT, b_vn], [bpO])
        yield
        P("act", lambda e, pO=pO, gq=gq: e.copy(out=otok[:, gq * 512:(gq + 1) * 512], in_=pO.rearrange("p h d -> p (h d)")), [bpO], [b_otok[gq]])
        yield

    def stageD(t):
        p = t % 2
        otok = otoks[p]; b_otok = b_otoks[p]
        zs = zss[t % 3]; b_zs = b_zss[t % 3]
        for hh in range(8):
            P("act", lambda e, hh=hh: e.activation(out=junkD[:], in_=otok[:, hh * 128:(hh + 1) * 128], func=AF.Square, accum_out=qkssD[:, hh:hh + 1]),
              [b_otok[hh // 4]], [b_junkD, b_qkssD])
            if hh % 4 == 3:
                yield
        P("dve", lambda e: e.tensor_scalar(out=scD[:], in0=qkssD[:], scalar1=1.0 / 128, scalar2=EPS, op0=ALU.mult, op1=ALU.add), [b_qkssD, b_scD], [b_scD])
        P("act", lambda e: e.activation(out=scD[:], in_=scD[:], func=AF.Ln), [b_scD], [b_scD])
        P("act", lambda e: e.activation(out=scD[:], in_=scD[:], func=AF.Exp, scale=-0.5), [b_scD], [b_scD])
        yield
        P("dve", lambda e: e.tensor_tensor(out=otok[:].rearrange("p (h d) -> p h d", h=8), in0=otok[:].rearrange("p (h d) -> p h d", h=8),
                                           in1=scD[:].unsqueeze(2).to_broadcast([128, 8, 128]), op=ALU.mult), b_otok + [b_scD], b_otok)
        yield
        P("pool", lambda e: e.tensor_tensor(out=otok[:], in0=otok[:], in1=ngbc[:], op=ALU.mult), b_otok + [b_ngbc], b_otok)
        yield
        P("dve", lambda e: e.tensor_tensor(out=otok[:], in0=otok[:], in1=zs[:], op=ALU.mult), b_otok + [b_zs], b_otok)
        yield
        for half in range(2):
            for k4 in range(4):
                k = half * 4 + k4
                P("pe", lambda e, k=k, k4=k4: e.transpose(pD1[:, k4, :], otok[:, k * 128:(k + 1) * 128], idf[:]), b_otok + [b_idf], [b_pD1])
            P("act", lambda e, half=half: e.copy(out=ogT[:, half * 4:(half + 1) * 4, :], in_=pD1[:]), [b_pD1], [b_ogT])
            yield
        rr = t * 128
        kb.op("sp", lambda e: e.dma_start(out=res[:], in_=hin_d[rr:rr + 128, :]), writes=[b_res], dma_sem=s_res)
        for dh in range(2):
            for k in range(8):
                P("pe", lambda e, k=k, dh=dh: e.matmul(pD1[:].rearrange("p c d -> p (c d)"), ogT[:, k, :], wout[:, k, dh * 512:(dh + 1) * 512], start=(k == 0), stop=(k == 7)),
                  [b_ogT, b_wout], [b_pD1])
            yield
            P("dve", lambda e, dh=dh: e.tensor_tensor(out=res[:, dh * 512:(dh + 1) * 512], in0=res[:, dh * 512:(dh + 1) * 512], in1=pD1[:].rearrange("p c d -> p (c d)"), op=ALU.add),
              [b_pD1, b_res], [b_res])
            yield
        kb.op("sp", lambda e: e.dma_start(out=hout_d[rr:rr + 128, :], in_=res[:]), reads=[b_res], dma_sem=s_out)
        yield

    for it in range(-1, NT + 1):
        gens = []
        if 0 <= it < NT:
            gens.append(grp(0, it)); gens.append(grp(1, it))
        if 0 <= it + 1 < NT:
            gens.append(stageAB(it + 1))
        if 0 <= it - 1 < NT:
            gens.append(stageD(it - 1))
        while gens:
            for g_ in list(gens):
                try:
                    next(g_)
                except StopIteration:
                    gens.remove(g_)
    kb.end_phase()


def make_consts():
    c = {}
    c["ident"] = np.eye(128, dtype=np.float32)
    invc = np.zeros((128, 4, 16), np.float32)
    for g in range(4):
        w = 2 ** (g + 1)
        for t in range(16):
            invc[:, g, t] = 1.0 / min(t + 1, w)
    c["invc"] = invc
    i = np.arange(128)
    same = (i[:, None] // 64) == (i[None, :] // 64)
    c["trit"] = (same & (i[:, None] <= i[None, :])).astype(np.float32)
    c["bones"] = same.astype(np.float32)
    cs = np.zeros((128, 2, 128), np.float32)
    cs[:64, 0, :] = 1.0
    cs[64:, 1, :] = 1.0
    c["csel"] = cs
    lvm = np.zeros((128, 6, 128), np.float32)
    for lv in range(6):
        b = 2 ** lv
        m = ((i[:, None] // (2 * b)) == (i[None, :] // (2 * b))) & ((i[:, None] % (2 * b)) >= b) & ((i[None, :] % (2 * b)) < b)
        lvm[:, lv, :] = m
    c["lvm"] = lvm
    c["muin"] = (same & (i[None, :] >= i[:, None])).astype(np.float32)
    c["id4"] = np.ascontiguousarray(np.broadcast_to(np.eye(128, dtype=np.float32)[:, None, :], (128, 4, 128)))
    return c

def bc(v, n=128):
    return np.ascontiguousarray(np.broadcast_to(np.asarray(v, np.float32).reshape(1, -1), (n, np.asarray(v).size)))

def host_inputs(inp):
    d = {}
    for k in ["pool_w_in", "pool_w_group", "dn_w_in", "dn_w_out", "ffn_w_gate", "ffn_w_up", "ffn_w_down",
              "moe_w_gate", "moe_w_up", "moe_w_down"]:
        d[k] = np.ascontiguousarray(inp[k], dtype=np.float32)
    d["pool_scale_bc"] = bc(inp["pool_scale"][0])
    d["g_mix0_bc"] = bc(inp["norm_mix_g"][0])
    d["g_mix1_bc"] = bc(inp["norm_mix_g"][1])
    d["g_ffn0_bc"] = bc(inp["norm_ffn_g"][0])
    d["g_ffn1_bc"] = bc(inp["norm_ffn_g"][1])
    d["g_final_bc"] = bc(inp["final_norm_g"])
    d["router_b_bc"] = bc(inp["moe_router_b"][0])
    d["a_log_bc"] = bc(inp["dn_a_log"][0])
    d["dt_bias_bc"] = bc(inp["dn_dt_bias"][0])
    d["dn_norm_g_bc"] = bc(np.tile(inp["dn_norm_g"][0], 8))
    d["conv_wl"] = np.ascontiguousarray(inp["dn_conv_w"][0].T.reshape(24, 128, 4).transpose(1, 0, 2))
    d["router_w_l"] = np.ascontiguousarray(inp["moe_router_w"][0].reshape(8, 128, 8).transpose(1, 0, 2))
    d["wba_l"] = np.ascontiguousarray(inp["dn_w_in"][0][:, 4096:4112].reshape(8, 128, 16).transpose(1, 0, 2))
    d.update(make_consts())
    return d

def declare_inputs(nc, d):
    W = {}
    for k, v in d.items():
        W[k] = nc.dram_tensor(k, list(v.shape), F32, kind="ExternalInput").ap()
    return W


def build_program(d):
    nc = bass.Bass("TRN2", target_bir_lowering=False)
    W = declare_inputs(nc, d)
    x_d = nc.dram_tensor("x", [S, D], F32, kind="ExternalInput").ap()
    out_d = nc.dram_tensor("out", [S, D], F32, kind="ExternalOutput").ap()
    h1 = nc.dram_tensor("h1s", [S, D], F32).ap()
    h2 = nc.dram_tensor("h2s", [S, D], F32).ap()
    h3 = nc.dram_tensor("h3s", [S, D], F32).ap()
    kb = KB(nc)
    phase_pool(kb, x_d, h1, W)
    phase_ffn(kb, h1, h2, W, moe=False)
    phase_dn(kb, h2, h3, W)
    phase_ffn(kb, h3, out_d, W, moe=True, final_norm=True)
    kb.close()
    return nc


def kernel(**inputs):
    inp = {k: np.asarray(v) for k, v in inputs.items()}
    d = host_inputs(inp)
    nc = build_program(d)
    x = np.ascontiguousarray(inp["x"], dtype=np.float32)
    nb = x.shape[0]
    in_maps = []
    for b in range(nb):
        m = dict(d)
        m["x"] = x[b]
        in_maps.append(m)
    res = run_bass_kernel_spmd(nc, in_maps, core_ids=list(range(nb)))
    return np.stack([np.asarray(r["out"], dtype=np.float32) for r in res.results], axis=0)
```

```python
import numpy as np
from contextlib import ExitStack
import concourse.bass as bass
import concourse.mybir as mybir
from concourse.bass_utils import run_bass_kernel_spmd

F32 = mybir.dt.float32
BF16 = mybir.dt.bfloat16
I32 = mybir.dt.int32
ALU = mybir.AluOpType
AF = mybir.ActivationFunctionType
AX = mybir.AxisListType


class Buf:
    __slots__ = ("name", "lw", "rd", "sem", "excl")

    def __init__(self, name):
        self.name = name
        self.excl = False
        self.lw = None
        self.rd = []
        self.sem = None


class Op:
    __slots__ = ("eng", "fn", "deps", "sig", "dma", "semname", "used")

    def __init__(self, eng, fn, dma, semname):
        self.eng = eng
        self.fn = fn
        self.deps = []
        self.sig = None
        self.dma = dma
        self.semname = semname
        self.used = False


class KB:
    ENGS = ("pe", "act", "dve", "pool", "sp")

    def __init__(self, nc):
        self.nc = nc
        self.stack = ExitStack()
        self.sems = {}
        self.semval = {}
        self.free_dma_sems = {"hw": [], "sw": []}
        self.n_dma_sems = 0
        self.n_logical = 0
        for e in ("pe", "act", "dve", "pool"):
            self._mksem("c_" + e)
        self.ops = []
        self.last_dma_on_sem = {}
        self.phase_stack = None
        self.n_emitted = 0

    def _mksem(self, name):
        h = self.stack.enter_context(self.nc.semaphore(name))
        self.sems[name] = h
        self.semval[name] = 0
        return h

    def dma_sem(self, kind):
        if self.free_dma_sems[kind]:
            return self.free_dma_sems[kind].pop()
        name = "d%s%d" % (kind, self.n_dma_sems)
        self.n_dma_sems += 1
        self._mksem(name)
        return name

    def begin_phase(self):
        self.ops = []
        self.last_dma_on_sem = {}
        self.phase_stack = ExitStack()
        self.phase_sems = []
        self.sem_map = {}
        self.bufs = []

    def pbuf(self):
        b = self.buf()
        b.excl = True
        return b

    def buf(self, name="b"):
        b = Buf(name)
        self.bufs.append(b)
        return b

    def sb(self, name, shape, dtype):
        self.uid = getattr(self, "uid", 0) + 1
        return self.phase_stack.enter_context(self.nc.sbuf_tensor("sb%d_%s" % (self.uid, name), list(shape), dtype))

    def ps(self, name, shape, dtype=F32):
        self.uid = getattr(self, "uid", 0) + 1
        return self.phase_stack.enter_context(self.nc.psum_tensor("ps%d_%s" % (self.uid, name), list(shape), dtype))

    def phase_dma_sem(self):
        self.n_logical += 1
        return "L%d" % self.n_logical

    def op(self, eng, fn, reads=(), writes=(), dma_sem=None):
        if dma_sem is not None:
            kind = "sw" if eng == "pool" else "hw"
            key = (dma_sem, kind)
            if key not in self.sem_map:
                ph = self.dma_sem(kind)
                self.sem_map[key] = ph
                self.phase_sems.append((kind, ph))
            dma_sem = self.sem_map[key]
        o = Op(eng, fn, dma_sem is not None, dma_sem)
        deps = []
        for b in reads:
            if b.lw is not None:
                deps.append(b.lw)
            if b.excl:
                deps.extend(r for r in b.rd if r.eng != eng)
        for b in writes:
            if b.lw is not None:
                deps.append(b.lw)
            deps.extend(b.rd)
        if dma_sem is not None:
            p = self.last_dma_on_sem.get(dma_sem)
            if p is not None:
                deps.append(p)
            self.last_dma_on_sem[dma_sem] = o
        seen = set()
        for d in deps:
            if id(d) in seen or d is o:
                continue
            seen.add(id(d))
            if eng == "pe" and d.eng == "pe" and not d.dma and not o.dma:
                continue
            o.deps.append(d)
            d.used = True
        for b in reads:
            b.rd.append(o)
        for b in writes:
            b.lw = o
            b.rd = []
        self.ops.append(o)
        return o

    def end_phase(self, final_wait=True):
        nc = self.nc
        for o in self.ops:
            if o.dma:
                self.semval[o.semname] += 16
                o.sig = (o.semname, self.semval[o.semname])
            elif o.used:
                s = "c_" + o.eng
                self.semval[s] += 1
                o.sig = (s, self.semval[s])
        per = {e: [] for e in self.ENGS}
        for o in self.ops:
            per[o.eng].append(o)
        final = [(s, self.semval[s]) for s in set(o.semname for o in self.ops if o.dma)]
        sems = self.sems
        self.n_emitted += len(self.ops)

        def emit(engname, handle):
            waited = {}
            for o in per[engname]:
                for d in o.deps:
                    s, v = d.sig
                    if waited.get(s, 0) < v:
                        handle.wait_ge(sems[s], v)
                        waited[s] = v
                ins = o.fn(handle)
                if o.sig is not None:
                    ins.then_inc(sems[o.sig[0]], 16 if o.dma else 1)
            if engname == "sp" and final_wait:
                for s, v in final:
                    if waited.get(s, 0) < v:
                        handle.wait_ge(sems[s], v)

        with nc.Block() as block:
            @block.sync
            def _(e):
                emit("sp", e)

            @block.tensor
            def _(e):
                emit("pe", e)

            @block.scalar
            def _(e):
                emit("act", e)

            @block.vector
            def _(e):
                emit("dve", e)

            @block.gpsimd
            def _(e):
                emit("pool", e)
        for kind, s in self.phase_sems:
            self.free_dma_sems[kind].append(s)
        self.phase_stack.close()
        self.phase_stack = None
        self.ops = []

    def close(self):
        self.stack.close()


EPS = 1e-6
S = 4096
D = 1024


def norm_tiles(kb, nt, src, b_src, gbc, b_gbc, hn, b_hn, ss, b_ss, rstd, b_rstd, junk, b_junk):
    for j in range(nt):
        kb.op("act", lambda e, j=j: e.activation(out=junk[:], in_=src[:, j, :], func=AF.Square, accum_out=ss[:, j:j + 1]),
              reads=[b_src[j]], writes=[b_junk, b_ss])
    kb.op("dve", lambda e: e.tensor_scalar(out=rstd[:, 0:nt], in0=ss[:, 0:nt], scalar1=1.0 / D, scalar2=EPS, op0=ALU.mult, op1=ALU.add),
          reads=[b_ss], writes=[b_rstd])
    kb.op("act", lambda e: e.activation(out=rstd[:, 0:nt], in_=rstd[:, 0:nt], func=AF.Sqrt), reads=[b_rstd], writes=[b_rstd])
    kb.op("dve", lambda e: e.reciprocal(out=rstd[:, 0:nt], in_=rstd[:, 0:nt]), reads=[b_rstd], writes=[b_rstd])
    for j in range(nt):
        kb.op("dve", lambda e, j=j: e.scalar_tensor_tensor(out=hn[:, j, :], in0=src[:, j, :], scalar=rstd[:, j:j + 1], in1=gbc[:],
                                                             op0=ALU.mult, op1=ALU.mult),
              reads=[b_src[j], b_rstd, b_gbc], writes=[b_hn[j]])


def phase_pool(kb, x_d, h1_d, W):
    nc = kb.nc
    kb.begin_phase()
    NB = S // 512
    win = kb.sb("win", [128, 8, 1024], BF16); b_win = kb.buf()
    wgs = kb.sb("wgs", [128, 4, 2, 256], F32); b_wgs = kb.buf()
    wg = kb.sb("wg", [128, 4, 2, 256], BF16); b_wg = kb.buf()
    scbc = kb.sb("scbc", [128, 1024], F32); b_scbc = kb.buf()
    gbc = kb.sb("gbc", [128, 1024], F32); b_gbc = kb.buf()
    idf = kb.sb("idf", [128, 128], F32); b_idf = kb.buf()
    invc = kb.sb("invc", [128, 4, 16], F32); b_invc = kb.buf()
    sq = kb.phase_dma_sem
    kb.op("pool", lambda e: e.dma_start(out=win[:], in_=W["pool_w_in"][0].rearrange("(k p) n -> p k n", p=128)), writes=[b_win], dma_sem=sq())
    kb.op("sp", lambda e: e.dma_start(out=wgs[:], in_=W["pool_w_group"][0].rearrange("g (k p) e -> p g k e", p=128)), writes=[b_wgs], dma_sem=sq())
    kb.op("sp", lambda e: e.dma_start(out=scbc[:], in_=W["pool_scale_bc"]), writes=[b_scbc], dma_sem=sq())
    kb.op("sp", lambda e: e.dma_start(out=gbc[:], in_=W["g_mix0_bc"]), writes=[b_gbc], dma_sem=sq())
    kb.op("sp", lambda e: e.dma_start(out=idf[:], in_=W["ident"]), writes=[b_idf], dma_sem=sq())
    kb.op("sp", lambda e: e.dma_start(out=invc[:], in_=W["invc"]), writes=[b_invc], dma_sem=sq())
    for kk in range(2):
        kb.op("dve", lambda e, kk=kk: e.tensor_tensor(out=wg[:, :, kk, :], in0=wgs[:, :, kk, :],
                                                       in1=scbc[:].rearrange("p (g e) -> p g e", g=4), op=ALU.mult),
              reads=[b_wgs, b_scbc], writes=[b_wg])

    NBUF = 3
    xt = [kb.sb("xt%d" % i, [128, 4, 1024], F32) for i in range(NBUF)]
    b_xt = [[kb.buf() for _ in range(4)] for i in range(NBUF)]
    s_xt = [sq() for i in range(NBUF)]
    s_st = [sq() for i in range(NBUF)]
    hns = [kb.sb("hn%d" % i, [128, 4, 1024], F32) for i in range(2)]; b_hns = [[kb.buf() for _ in range(4)] for _ in range(2)]
    hnTs = [kb.sb("hnT%d" % i, [128, 8, 512], BF16) for i in range(2)]; b_hnTs = [kb.buf() for _ in range(2)]
    ss = kb.sb("ss", [128, 4], F32); b_ss = kb.buf()
    rstd = kb.sb("rstd", [128, 4], F32); b_rstd = kb.buf()
    junk = kb.sb("junk", [128, 1024], BF16); b_junk = kb.buf()
    U = [kb.sb("U%d" % c, [128, 528], F32) for c in range(8)]; b_U = [kb.buf() for _ in range(8)]
    NAB = 3
    A = [kb.sb("A%d" % i, [128, 528], F32) for i in range(NAB)]; b_A = [kb.buf() for _ in range(NAB)]
    B = [kb.sb("B%d" % i, [128, 528], F32) for i in range(NAB)]; b_B = [kb.buf() for _ in range(NAB)]
    mTs = [kb.sb("mT%d" % i, [128, 8, 512], BF16) for i in range(2)]; b_mTs = [[kb.buf() for _ in range(8)] for _ in range(2)]
    tmp16 = kb.sb("tmp16", [128, 16], F32); b_tmp16 = kb.buf()
    pT = [kb.ps("pT%d" % i, [128, 2, 512], F32) for i in range(2)]; b_pT = [kb.pbuf() for _ in range(2)]
    pU = [kb.ps("pU%d" % i, [128, 512], F32) for i in range(2)]; b_pU = [kb.pbuf() for _ in range(2)]
    pY = kb.ps("pY", [128, 2, 512], F32); b_pY = kb.pbuf()

    for c in range(8):
        kb.op("pool", lambda e, c=c: e.memset(U[c][:, 0:16], 0.0), writes=[b_U[c]])

    def stA(blk):
        xb = xt[blk % NBUF]; bx = b_xt[blk % NBUF]
        hn = hns[blk % 2]; b_hn = b_hns[blk % 2]; hnT = hnTs[blk % 2]; b_hnT = b_hnTs[blk % 2]
        r0 = blk * 512
        kb.op("sp", lambda e: e.dma_start(out=xb[:], in_=x_d[r0:r0 + 512, :].rearrange("(j p) d -> p j d", p=128)),
              writes=bx, dma_sem=s_xt[blk % NBUF])
        yield
        for j in range(4):
            kb.op("act", lambda e, j=j: e.activation(out=junk[:], in_=xb[:, j, :], func=AF.Square, accum_out=ss[:, j:j + 1]),
                  reads=[bx[j]], writes=[b_junk, b_ss])
        yield
        kb.op("dve", lambda e: e.tensor_scalar(out=rstd[:], in0=ss[:], scalar1=1.0 / D, scalar2=EPS, op0=ALU.mult, op1=ALU.add), reads=[b_ss], writes=[b_rstd])
        yield
        kb.op("act", lambda e: e.activation(out=rstd[:], in_=rstd[:], func=AF.Sqrt), reads=[b_rstd], writes=[b_rstd])
        yield
        kb.op("dve", lambda e: e.reciprocal(out=rstd[:], in_=rstd[:]), reads=[b_rstd], writes=[b_rstd])
        yield
        for j in range(4):
            kb.op("dve", lambda e, j=j: e.scalar_tensor_tensor(out=hn[:, j, :], in0=xb[:, j, :], scalar=rstd[:, j:j + 1], in1=gbc[:], op0=ALU.mult, op1=ALU.mult),
                  reads=[bx[j], b_rstd, b_gbc], writes=[b_hn[j]])
            yield
        for j in range(4):
            pt = pT[j % 2]; bp = b_pT[j % 2]
            for k in range(8):
                kb.op("pe", lambda e, j=j, k=k, pt=pt: e.transpose(pt[:, k // 4, (k % 4) * 128:(k % 4 + 1) * 128], hn[:, j, k * 128:(k + 1) * 128], idf[:]),
                      reads=[b_hn[j], b_idf], writes=[bp])
            yield
            kb.op("act", lambda e, j=j, pt=pt: e.copy(out=hnT[:, :, j * 128:(j + 1) * 128], in_=pt[:].rearrange("p a (b t) -> p (a b) t", t=128)),
                  reads=[bp], writes=[b_hnT])
            yield

    def stB(blk):
        hnT = hnTs[blk % 2]; b_hnT = b_hnTs[blk % 2]; mT = mTs[blk % 2]; b_mT = b_mTs[blk % 2]
        state = {}
        for step in range(8 + 2):
            c = step
            if c < 8:
                pu = pU[c % 2]; bpu = b_pU[c % 2]
                for k in range(8):
                    kb.op("pe", lambda e, c=c, k=k, pu=pu: e.matmul(pu[:], win[:, k, c * 128:(c + 1) * 128], hnT[:, k, :], start=(k == 0), stop=(k == 7)),
                          reads=[b_hnT, b_win], writes=[bpu])
                kb.op("act", lambda e, c=c, pu=pu: e.copy(out=U[c][:, 16:528], in_=pu[:]), reads=[bpu], writes=[b_U[c]])
            c = step - 1
            if 0 <= c < 8:
                g = c // 2
                w = 2 ** (g + 1)
                a = A[c % NAB]; ba = b_A[c % NAB]; b = B[c % NAB]; bb = b_B[c % NAB]
                kb.op("pool", lambda e, c=c, a=a: e.tensor_tensor(out=a[:, 1:528], in0=U[c][:, 1:528], in1=U[c][:, 0:527], op=ALU.add),
                      reads=[b_U[c]], writes=[ba])
                cur, bcur = a, ba
                if w >= 4:
                    kb.op("pool", lambda e, a=a, b=b: e.tensor_tensor(out=b[:, 3:528], in0=a[:, 3:528], in1=a[:, 1:526], op=ALU.add),
                          reads=[ba], writes=[bb])
                    cur, bcur = b, bb
                if w >= 8:
                    kb.op("pool", lambda e, a=a, b=b: e.tensor_tensor(out=a[:, 7:528], in0=b[:, 7:528], in1=b[:, 3:524], op=ALU.add),
                          reads=[bb], writes=[ba])
                    cur, bcur = a, ba
                if w >= 16:
                    kb.op("pool", lambda e, a=a, b=b: e.tensor_tensor(out=b[:, 15:528], in0=a[:, 15:528], in1=a[:, 7:520], op=ALU.add),
                          reads=[ba], writes=[bb])
                    cur, bcur = b, bb
                state[c] = (cur, bcur, w, g)
            c = step - 2
            if 0 <= c < 8:
                cur, bcur, w, g = state[c]
                kb.op("dve", lambda e, c=c, cur=cur, w=w: e.scalar_tensor_tensor(out=mT[:, c, :], in0=cur[:, 16:528], scalar=1.0 / w, in1=U[c][:, 16:528],
                                                                                op0=ALU.mult, op1=ALU.subtract),
                      reads=[bcur, b_U[c]], writes=[b_mT[c]])
                if blk == 0:
                    kb.op("dve", lambda e, cur=cur, g=g: e.tensor_tensor(out=tmp16[:], in0=cur[:, 16:32], in1=invc[:, g, :], op=ALU.mult),
                          reads=[bcur, b_invc], writes=[b_tmp16])
                    kb.op("dve", lambda e, c=c: e.tensor_tensor(out=mT[:, c, 0:16], in0=tmp16[:], in1=U[c][:, 16:32], op=ALU.subtract),
                          reads=[b_tmp16, b_U[c]], writes=[b_mT[c]])
                kb.op("pool", lambda e, c=c: e.tensor_copy(out=U[c][:, 0:16], in_=U[c][:, 512:528]), reads=[b_U[c]], writes=[b_U[c]])
            yield

    def stC(blk):
        xb = xt[blk % NBUF]; bx = b_xt[blk % NBUF]
        mT = mTs[blk % 2]; b_mT = b_mTs[blk % 2]
        r0 = blk * 512
        for j in range(4):
            for g in range(4):
                for kk in range(2):
                    kb.op("pe", lambda e, j=j, g=g, kk=kk: e.matmul(pY[:, g // 2, (g % 2) * 256:(g % 2 + 1) * 256],
                                                                   mT[:, 2 * g + kk, j * 128:(j + 1) * 128], wg[:, g, kk, :],
                                                                   start=(kk == 0), stop=(kk == 1)),
                          reads=[b_mT[2 * g + kk], b_wg], writes=[b_pY])
            yield
            for hh in range(2):
                kb.op("dve", lambda e, j=j, hh=hh: e.tensor_tensor(out=xb[:, j, hh * 512:(hh + 1) * 512], in0=xb[:, j, hh * 512:(hh + 1) * 512],
                                                                   in1=pY[:, hh, :], op=ALU.add),
                      reads=[b_pY, bx[j]], writes=[bx[j]])
            yield
        kb.op("sp", lambda e: e.dma_start(out=h1_d[r0:r0 + 512, :].rearrange("(j p) d -> p j d", p=128), in_=xb[:]),
              reads=bx, dma_sem=s_st[blk % NBUF])
        yield

    for it in range(-1, NB + 1):
        gens = []
        if 0 <= it < NB:
            gens.append(stB(it))
        if 0 <= it - 1 < NB:
            gens.append(stC(it - 1))
        if 0 <= it + 1 < NB:
            gens.append(stA(it + 1))
        while gens:
            for g_ in list(gens):
                try:
                    next(g_)
                except StopIteration:
                    gens.remove(g_)
    kb.end_phase()


def phase_ffn(kb, hin_d, hout_d, W, moe, final_norm=False):
    nc = kb.nc
    if moe:
        NE, FF = 8, 3584
        wg_d, wu_d, wd_d = W["moe_w_gate"][0], W["moe_w_up"][0], W["moe_w_down"][0]
        gname = "g_ffn1_bc"
    else:
        NE, FF = 1, 2816
        wg_d, wu_d, wd_d = W["ffn_w_gate"], W["ffn_w_up"], W["ffn_w_down"]
        gname = "g_ffn0_bc"
    T = 2048
    NT = T // 128
    FB = 512
    blocks = []
    f0 = 0
    while f0 < FF:
        blocks.append((f0, min(FB, FF - f0)))
        f0 += FB
    kb.begin_phase()
    sq = kb.phase_dma_sem
    gbc = kb.sb("gbc", [128, 1024], F32); b_gbc = kb.buf()
    idf = kb.sb("idf", [128, 128], F32); b_idf = kb.buf()
    kb.op("sp", lambda e: e.dma_start(out=gbc[:], in_=W[gname]), writes=[b_gbc], dma_sem=sq())
    kb.op("sp", lambda e: e.dma_start(out=idf[:], in_=W["ident"]), writes=[b_idf], dma_sem=sq())
    if final_norm:
        gfin = kb.sb("gfin", [128, 1024], F32); b_gfin = kb.buf()
        kb.op("sp", lambda e: e.dma_start(out=gfin[:], in_=W["g_final_bc"]), writes=[b_gfin], dma_sem=sq())
    if moe:
        wr = kb.sb("wr", [128, 8, 8], F32); b_wr = kb.buf()
        rb = kb.sb("rb", [128, 8], F32); b_rb = kb.buf()
        kb.op("sp", lambda e: e.dma_start(out=wr[:], in_=W["router_w_l"]), writes=[b_wr], dma_sem=sq())
        kb.op("sp", lambda e: e.dma_start(out=rb[:], in_=W["router_b_bc"]), writes=[b_rb], dma_sem=sq())
        xf = kb.sb("xf", [128, 8, 128], F32); b_xf = kb.buf()
        lg = kb.sb("lg", [128, NT, 8], F32); b_lg = kb.buf()
        gates = kb.sb("gates", [128, NT, 8], F32); b_gates = kb.buf()
        m1 = kb.sb("m1", [128, NT], F32); m2 = kb.sb("m2", [128, NT], F32)
        mk1 = kb.sb("mk1", [128, NT, 8], F32); mk2 = kb.sb("mk2", [128, NT, 8], F32); l2 = kb.sb("l2", [128, NT, 8], F32)
        w1 = kb.sb("w1", [128, NT], F32); w2 = kb.sb("w2", [128, NT], F32)
        b_gt = kb.buf()
    acc = kb.sb("acc", [128, NT, 1024], F32); b_acc = [kb.buf() for _ in range(NT)]
    xnT = kb.sb("xnT", [128, 8, T], BF16); b_xnT = [kb.buf() for _ in range(T // 512)]
    NIF = 3
    hns = [kb.sb("hn%d" % i, [128, 1024], F32) for i in range(NIF)]; b_hns = [kb.buf() for _ in range(NIF)]
    sss = [kb.sb("ss%d" % i, [128, 1], F32) for i in range(NIF)]; b_sss = [kb.buf() for _ in range(NIF)]
    rstds = [kb.sb("rstd%d" % i, [128, 1], F32) for i in range(NIF)]; b_rstds = [kb.buf() for _ in range(NIF)]
    junks = [kb.sb("junk%d" % i, [128, 1024], BF16) for i in range(NIF)]; b_junks = [kb.buf() for _ in range(NIF)]
    if moe:
        xfs = [xf] + [kb.sb("xf%d" % i, [128, 8, 128], F32) for i in range(1, NIF)]; b_xfs = [b_xf] + [kb.buf() for _ in range(1, NIF)]
    NW = 2
    wgb = [kb.sb("wgb%d" % i, [128, 8, FB], BF16) for i in range(NW)]
    wub = [kb.sb("wub%d" % i, [128, 8, FB], BF16) for i in range(NW)]
    wdb = [kb.sb("wdb%d" % i, [128, FB // 128, 1024], BF16) for i in range(NW)]
    b_wgb = [kb.buf() for _ in range(NW)]; b_wub = [kb.buf() for _ in range(NW)]; b_wdb = [kb.buf() for _ in range(NW)]
    s_wg = [sq() for _ in range(NW)]; s_wu = [sq() for _ in range(NW)]; s_wd = [sq() for _ in range(NW)]
    sg = [kb.sb("sg%d" % i, [128, 512], BF16) for i in range(2)]; b_sg = [kb.buf() for _ in range(2)]
    hm = [kb.sb("hm%d" % i, [128, FB // 128, 512], BF16) for i in range(2)]; b_hm = [[kb.buf() for _ in range(FB // 128)] for _ in range(2)]
    s_ld = [sq() for _ in range(4)]; s_st = [sq() for _ in range(4)]
    pG = [kb.ps("pG%d" % i, [128, 512], F32) for i in range(2)]; b_pG = [kb.pbuf() for _ in range(2)]
    pU = [kb.ps("pU%d" % i, [128, 512], F32) for i in range(2)]; b_pU = [kb.pbuf() for _ in range(2)]
    pY = [kb.ps("pY%d" % i, [128, 512], F32) for i in range(2)]; b_pY = [kb.pbuf() for _ in range(2)]
    pTa = kb.ps("pTa", [128, 512], F32); b_pTa = kb.pbuf()
    pTb = kb.ps("pTb", [128, 512], F32); b_pTb = kb.pbuf()
    SLOT_T = [((pTa, b_pTa), (pTb, b_pTb)), ((pG[0], b_pG[0]), (pG[1], b_pG[1])), ((pU[0], b_pU[0]), (pU[1], b_pU[1]))]
    SLOT_L = [(pY[0][:, 0:8], b_pY[0]), (pY[1][:, 0:8], b_pY[1]), (pY[0][:, 8:16], b_pY[0])]

    def run_lockstep(gens):
        while gens:
            for g_ in list(gens):
                try:
                    next(g_)
                except StopIteration:
                    gens.remove(g_)

    def norm_gen(src, b_src, gain, b_gain, i):
        hn_, ss_, rstd_, junk_ = hns[i], sss[i], rstds[i], junks[i]
        kb.op("act", lambda e: e.activation(out=junk_[:], in_=src, func=AF.Square, accum_out=ss_[:, 0:1]), reads=[b_src], writes=[b_junks[i], b_sss[i]])
        yield
        kb.op("dve", lambda e: e.tensor_scalar(out=rstd_[:], in0=ss_[:], scalar1=1.0 / D, scalar2=EPS, op0=ALU.mult, op1=ALU.add), reads=[b_sss[i]], writes=[b_rstds[i]])
        yield
        kb.op("act", lambda e: e.activation(out=rstd_[:], in_=rstd_[:], func=AF.Sqrt), reads=[b_rstds[i]], writes=[b_rstds[i]])
        yield
        kb.op("dve", lambda e: e.reciprocal(out=rstd_[:], in_=rstd_[:]), reads=[b_rstds[i]], writes=[b_rstds[i]])
        yield
        kb.op("dve", lambda e: e.scalar_tensor_tensor(out=hn_[:], in0=src, scalar=rstd_[:, 0:1], in1=gain[:], op0=ALU.mult, op1=ALU.mult),
              reads=[b_src, b_rstds[i], b_gain], writes=[b_hns[i]])
        yield

    def pro_gen(t, i, t0):
        r0 = t0 + t * 128
        kb.op("sp", lambda e: e.dma_start(out=acc[:, t, :], in_=hin_d[r0:r0 + 128, :]), writes=[b_acc[t]], dma_sem=s_ld[i])
        yield
        for _ in norm_gen(acc[:, t, :], b_acc[t], gbc, b_gbc, i):
            yield
        hn_ = hns[i]
        for half in range(2):
            pt_, bpt_ = SLOT_T[i][half]
            for k4 in range(4):
                k = half * 4 + k4
                kb.op("pe", lambda e, k=k, k4=k4, pt_=pt_: e.transpose(pt_[:, k4 * 128:(k4 + 1) * 128], hn_[:, k * 128:(k + 1) * 128], idf[:]),
                      reads=[b_hns[i], b_idf], writes=[bpt_])
        yield
        for half in range(2):
            pt_, bpt_ = SLOT_T[i][half]
            kb.op("act", lambda e, half=half, pt_=pt_: e.copy(out=xnT[:, half * 4:(half + 1) * 4, t * 128:(t + 1) * 128], in_=pt_[:].rearrange("p (b t) -> p b t", t=128)),
                  reads=[bpt_], writes=[b_xnT[t // 4]])
            if moe:
                kb.op("dve", lambda e, half=half, pt_=pt_: e.tensor_copy(out=xfs[i][:, half * 4:(half + 1) * 4, :], in_=pt_[:].rearrange("p (b t) -> p b t", t=128)),
                      reads=[bpt_], writes=[b_xfs[i]])
        yield
        if moe:
            pL_, bpL_ = SLOT_L[i]
            for k in range(8):
                kb.op("pe", lambda e, k=k: e.matmul(pL_, xfs[i][:, k, :], wr[:, k, :], start=(k == 0), stop=(k == 7)),
                      reads=[b_xfs[i], b_wr], writes=[bpL_])
            yield
            kb.op("dve", lambda e: e.tensor_tensor(out=lg[:, t, :], in0=pL_, in1=rb[:], op=ALU.add), reads=[bpL_, b_rb], writes=[b_lg])
            yield

    def epi_gen(t, i, t0):
        r0 = t0 + t * 128
        for _ in norm_gen(acc[:, t, :], b_acc[t], gfin, b_gfin, i):
            yield
        kb.op("sp", lambda e: e.dma_start(out=hout_d[r0:r0 + 128, :], in_=hns[i][:]), reads=[b_hns[i]], dma_sem=s_st[i])
        yield

    wcount = 0
    for half in range(S // T):
        t0 = half * T
        for base in range(0, NT, NIF):
            run_lockstep([pro_gen(t, t - base, t0) for t in range(base, min(base + NIF, NT))])
        if moe:
            R = [b_lg, b_gt]
            bc = lambda a: a[:].unsqueeze(2).to_broadcast([128, NT, 8])
            kb.op("dve", lambda e: e.tensor_reduce(out=m1[:], in_=lg[:], axis=AX.X, op=ALU.max), reads=R, writes=[b_gt])
            kb.op("dve", lambda e: e.tensor_tensor(out=mk1[:], in0=lg[:], in1=bc(m1), op=ALU.is_equal), reads=R, writes=[b_gt])
            kb.op("dve", lambda e: e.scalar_tensor_tensor(out=l2[:], in0=mk1[:], scalar=-1e30, in1=lg[:], op0=ALU.mult, op1=ALU.add), reads=R, writes=[b_gt])
            kb.op("dve", lambda e: e.tensor_reduce(out=m2[:], in_=l2[:], axis=AX.X, op=ALU.max), reads=R, writes=[b_gt])
            kb.op("dve", lambda e: e.tensor_tensor(out=mk2[:], in0=l2[:], in1=bc(m2), op=ALU.is_equal), reads=R, writes=[b_gt])
            kb.op("dve", lambda e: e.tensor_tensor(out=w2[:], in0=m2[:], in1=m1[:], op=ALU.subtract), reads=R, writes=[b_gt])
            kb.op("act", lambda e: e.activation(out=w2[:], in_=w2[:], func=AF.Exp), reads=R, writes=[b_gt])
            kb.op("dve", lambda e: e.tensor_scalar(out=w1[:], in0=w2[:], scalar1=1.0, scalar2=None, op0=ALU.add), reads=R, writes=[b_gt])
            kb.op("dve", lambda e: e.reciprocal(out=w1[:], in_=w1[:]), reads=R, writes=[b_gt])
            kb.op("dve", lambda e: e.tensor_tensor(out=w2[:], in0=w2[:], in1=w1[:], op=ALU.mult), reads=R, writes=[b_gt])
            kb.op("dve", lambda e: e.tensor_tensor(out=mk1[:], in0=mk1[:], in1=bc(w1), op=ALU.mult), reads=R, writes=[b_gt])
            kb.op("dve", lambda e: e.tensor_tensor(out=mk2[:], in0=mk2[:], in1=bc(w2), op=ALU.mult), reads=R, writes=[b_gt])
            kb.op("dve", lambda e: e.tensor_tensor(out=gates[:], in0=mk1[:], in1=mk2[:], op=ALU.add), reads=R, writes=[b_gates])
        for ex in range(NE):
            for (f0, fsz) in blocks:
                nch = fsz // 128
                wi = wcount % NW
                wcount += 1
                gsrc = wg_d[ex][:, f0:f0 + fsz].rearrange("(k p) f -> p k f", p=128)
                usrc = wu_d[ex][:, f0:f0 + fsz].rearrange("(k p) f -> p k f", p=128)
                dsrc = wd_d[ex][f0:f0 + fsz, :].rearrange("(c p) d -> p c d", p=128)
                kb.op("pool", lambda e, wi=wi, gsrc=gsrc, fsz=fsz: e.dma_start(out=wgb[wi][:, :, 0:fsz], in_=gsrc), writes=[b_wgb[wi]], dma_sem=s_wg[wi])
                kb.op("pool", lambda e, wi=wi, usrc=usrc, fsz=fsz: e.dma_start(out=wub[wi][:, :, 0:fsz], in_=usrc), writes=[b_wub[wi]], dma_sem=s_wu[wi])
                kb.op("pool", lambda e, wi=wi, dsrc=dsrc, nch=nch: e.dma_start(out=wdb[wi][:, 0:nch, :], in_=dsrc), writes=[b_wdb[wi]], dma_sem=s_wd[wi])
                for tb in range(T // 512):
                    hi = tb % 2
                    for c in range(nch):
                        pi = c % 2
                        for k in range(8):
                            kb.op("pe", lambda e, wi=wi, c=c, k=k, pi=pi, tb=tb: e.matmul(pG[pi][:], wgb[wi][:, k, c * 128:(c + 1) * 128],
                                                                                          xnT[:, k, tb * 512:(tb + 1) * 512], start=(k == 0), stop=(k == 7)),
                                  reads=[b_wgb[wi], b_xnT[tb]], writes=[b_pG[pi]])
                        for k in range(8):
                            kb.op("pe", lambda e, wi=wi, c=c, k=k, pi=pi, tb=tb: e.matmul(pU[pi][:], wub[wi][:, k, c * 128:(c + 1) * 128],
                                                                                          xnT[:, k, tb * 512:(tb + 1) * 512], start=(k == 0), stop=(k == 7)),
                                  reads=[b_wub[wi], b_xnT[tb]], writes=[b_pU[pi]])
                        kb.op("act", lambda e, pi=pi: e.activation(out=sg[pi][:], in_=pG[pi][:], func=AF.Silu), reads=[b_pG[pi]], writes=[b_sg[pi]])
                        kb.op("dve", lambda e, pi=pi, hi=hi, c=c: e.tensor_tensor(out=hm[hi][:, c, :], in0=sg[pi][:], in1=pU[pi][:], op=ALU.mult),
                              reads=[b_sg[pi], b_pU[pi]], writes=[b_hm[hi][c]])
                    for j in range(4):
                        t = tb * 4 + j
                        for dh in range(2):
                            yi = (j * 2 + dh) % 2
                            for c in range(nch):
                                kb.op("pe", lambda e, hi=hi, c=c, j=j, dh=dh, yi=yi, wi=wi, nch=nch: e.matmul(
                                    pY[yi][:], hm[hi][:, c, j * 128:(j + 1) * 128], wdb[wi][:, c, dh * 512:(dh + 1) * 512],
                                    start=(c == 0), stop=(c == nch - 1)),
                                    reads=[b_hm[hi][c], b_wdb[wi]], writes=[b_pY[yi]])
                            if moe:
                                kb.op("dve", lambda e, t=t, dh=dh, yi=yi, ex=ex: e.scalar_tensor_tensor(
                                    out=acc[:, t, dh * 512:(dh + 1) * 512], in0=pY[yi][:], scalar=gates[:, t, ex:ex + 1],
                                    in1=acc[:, t, dh * 512:(dh + 1) * 512], op0=ALU.mult, op1=ALU.add),
                                    reads=[b_pY[yi], b_gates, b_acc[t]], writes=[b_acc[t]])
                            else:
                                kb.op("dve", lambda e, t=t, dh=dh, yi=yi: e.tensor_tensor(
                                    out=acc[:, t, dh * 512:(dh + 1) * 512], in0=pY[yi][:], in1=acc[:, t, dh * 512:(dh + 1) * 512], op=ALU.add),
                                    reads=[b_pY[yi], b_acc[t]], writes=[b_acc[t]])
        if final_norm:
            for base in range(0, NT, NIF):
                run_lockstep([epi_gen(t, t - base, t0) for t in range(base, min(base + NIF, NT))])
        else:
            for t in range(NT):
                r0 = t0 + t * 128
                kb.op("sp", lambda e, t=t, r0=r0: e.dma_start(out=hout_d[r0:r0 + 128, :], in_=acc[:, t, :]), reads=[b_acc[t]], dma_sem=s_st[t % 4])
    kb.end_phase()


def phase_dn(kb, hin_d, hout_d, W):
    nc = kb.nc
    kb.begin_phase()
    sq = kb.phase_dma_sem
    NT = S // 128
    win_d = W["dn_w_in"][0]

    def P(eng, fn, r=(), w=()):
        return kb.op(eng, fn, reads=r, writes=w)

    def const(name, shape, src, eng="sp"):
        t = kb.sb(name, shape, F32); b = kb.buf()
        kb.op(eng, lambda e: e.dma_start(out=t[:], in_=src), writes=[b], dma_sem=sq())
        return t, b

    gbc, b_gbc = const("gbc", [128, 1024], W["g_mix1_bc"])
    idf, b_idf = const("idf", [128, 128], W["ident"])
    ngbc, b_ngbc = const("ngbc", [128, 1024], W["dn_norm_g_bc"])
    cw, b_cw = const("cw", [128, 24, 4], W["conv_wl"])
    alog, b_alog = const("alog", [128, 8], W["a_log_bc"])
    dtb, b_dtb = const("dtb", [128, 8], W["dt_bias_bc"])
    trit, b_trit = const("trit", [128, 128], W["trit"])
    bones, b_bones = const("bones", [128, 128], W["bones"])
    csel, b_csel = const("csel", [128, 2, 128], W["csel"])
    lvm, b_lvm = const("lvm", [128, 6, 128], W["lvm"])
    muin, b_muin = const("muin", [128, 128], W["muin"])
    id4, b_id4 = const("id4", [128, 4, 128], W["id4"])
    idb = kb.sb("idb", [128, 128], BF16); b_idb = kb.buf()
    kb.op("pool", lambda e: e.dma_start(out=idb[:], in_=W["ident"]), writes=[b_idb], dma_sem=sq())
    wout = kb.sb("wout", [128, 8, 1024], BF16); b_wout = kb.buf()
    kb.op("pool", lambda e: e.dma_start(out=wout[:], in_=W["dn_w_out"][0].rearrange("(k p) n -> p k n", p=128)), writes=[b_wout], dma_sem=sq())
    wba = kb.sb("wba", [128, 8, 16], BF16); b_wba = kb.buf()
    kb.op("pool", lambda e: e.dma_start(out=wba[:], in_=W["wba_l"]), writes=[b_wba], dma_sem=sq())
    nega = kb.sb("nega", [128, 8], F32); b_nega = kb.buf()
    P("act", lambda e: e.activation(out=nega[:], in_=alog[:], func=AF.Exp), [b_alog], [b_nega])
    P("dve", lambda e: e.tensor_scalar(out=nega[:], in0=nega[:], scalar1=-1.0, scalar2=None, op0=ALU.mult), [b_nega], [b_nega])

    xt = kb.sb("xt", [128, 1024], F32); b_xt = kb.buf(); s_xt = sq()
    hn = kb.sb("hn", [128, 1024], F32); b_hn = kb.buf()
    hnT = kb.sb("hnT", [128, 8, 128], BF16); b_hnT = kb.buf()
    ss = kb.sb("ss", [128, 1], F32); b_ss = kb.buf()
    rstd = kb.sb("rstd", [128, 1], F32); b_rstd = kb.buf()
    junk = kb.sb("junk", [128, 128], BF16); b_junk = kb.buf()
    junkD = kb.sb("junkD", [128, 128], BF16); b_junkD = kb.buf()
    NWB = 2
    wst = [kb.sb("wst%d" % i, [128, 8, 512], BF16) for i in range(NWB)]; b_wst = [kb.buf() for _ in range(NWB)]
    s_wst = [sq() for _ in range(NWB)]
    hist = kb.sb("hist", [128, 24, 3], F32); b_hist = [kb.buf() for _ in range(6)]
    xcw4 = [kb.sb("xcw4_%d" % i, [128, 4, 131], F32) for i in range(2)]; b_xcw4 = [kb.buf() for _ in range(2)]
    cv4 = kb.sb("cv4", [128, 4, 128], F32); b_cv4 = kb.buf()
    ct4 = kb.sb("ct4", [128, 4, 128], F32); b_ct4 = kb.buf()
    toks = [kb.sb("tok%d" % i, [128, 3072], F32) for i in range(2)]; b_toks = [[kb.buf() for _ in range(6)] for _ in range(2)]
    zss = [kb.sb("zs%d" % i, [128, 1024], BF16) for i in range(3)]; b_zss = [kb.buf() for _ in range(3)]
    scs = [kb.sb("sc%d" % i, [128, 16, 8], F32) for i in range(2)]; b_scs = [kb.buf() for _ in range(2)]
    (I_EB, I_SB, I_X, I_G, I_GC, I_EGC, I_EDEC, I_RQ, I_RK, I_SKB, I_SKG, I_SKD, I_SQG, I_TMP, I_NG, I_RO) = range(16)
    qkss = kb.sb("qkss", [128, 16], F32); b_qkss = kb.buf()
    qkssD = kb.sb("qkssD", [128, 8], F32); b_qkssD = kb.buf()
    scD = kb.sb("scD", [128, 8], F32); b_scD = kb.buf()
    egls = [kb.sb("egl%d" % i, [128, 2, 8], F32) for i in range(2)]; b_egls = [kb.buf() for _ in range(2)]
    GB = []
    for gq_ in range(2):
        dct = {}
        for n_ in ['kbt', 'knt', 'kgt', 'kdt', 'qnt', 'qgt', 'kbT', 'knT', 'qnT', 'QG0', 'QG1', 'GT', 'NGT', 'Dm', 'Dn', 'L3', 'qkT', 'W0', 'W1', 'vn']:
            dct[n_] = (kb.sb("%s_%d" % (n_, gq_), [128, 4, 128], BF16 if n_ in ('kbt', 'knt', 'qnt', 'kbT', 'knT', 'qnT') else F32), kb.buf())
        GB.append(dct)
    BALL = [(kb.sb("Ball%d" % g_, [128, 6, 4, 128], BF16), kb.buf()) for g_ in range(2)]
    St = [[kb.sb("S%d_%d" % (g, i), [128, 4, 128], F32) for i in range(3)] for g in range(2)]
    b_St = [[kb.buf() for i in range(3)] for g in range(2)]
    otoks = [kb.sb("otok%d" % i, [128, 1024], F32) for i in range(2)]; b_otoks = [[kb.buf() for _ in range(2)] for _ in range(2)]
    ogT = kb.sb("ogT", [128, 8, 128], BF16); b_ogT = kb.buf()
    res = kb.sb("res", [128, 1024], F32); b_res = kb.buf(); s_res = sq(); s_out = sq()

    pX = kb.ps("pX", [128, 4, 128], F32); b_pX = kb.pbuf()
    pYk = kb.ps("pYk", [128, 4, 128], F32); b_pYk = kb.pbuf()
    pG = [kb.ps("pG%d" % i, [128, 4, 128], F32) for i in range(4)]; b_pG = [kb.pbuf() for _ in range(4)]
    pD1 = kb.ps("pD1", [128, 4, 128], F32); b_pD1 = kb.pbuf()
    pS = kb.ps("pS", [128, 64], F32); b_pS = kb.pbuf()

    for g in range(2):
        for i in range(3):
            P("pool", lambda e, g=g, i=i: e.memset(St[g][i][:], 0.0), [], [b_St[g][i]])
    P("pool", lambda e: e.memset(hist[:], 0.0), [], b_hist)
    for gq_ in range(2):
        for n_ in ("QG0", "QG1", "W0", "W1"):
            t_, b_ = GB[gq_][n_]
            P("pool", lambda e, t_=t_: e.memset(t_[:], 0.0), [], [b_])

    GSLOT = [[(pG[0][:], b_pG[0]), (pG[1][:], b_pG[1])], [(pG[2][:], b_pG[2]), (pG[3][:], b_pG[3])]]
    gctr = [0, 0]
    wcount = [0]
    sidx = [0, 0]

    def stageAB(t):
        p = t % 2
        tok = toks[p]; b_tok = b_toks[p]
        zs = zss[t % 3]; b_zs = b_zss[t % 3]
        sc = scs[p]; b_sc = b_scs[p]; egl = egls[p]; b_egl = b_egls[p]
        r0 = t * 128
        kb.op("sp", lambda e: e.dma_start(out=xt[:], in_=hin_d[r0:r0 + 128, :]), writes=[b_xt], dma_sem=s_xt)
        P("act", lambda e: e.activation(out=hn[:], in_=xt[:], func=AF.Square, accum_out=ss[:, 0:1]), [b_xt], [b_hn, b_ss])
        P("dve", lambda e: e.tensor_scalar(out=rstd[:], in0=ss[:], scalar1=1.0 / D, scalar2=EPS, op0=ALU.mult, op1=ALU.add), [b_ss], [b_rstd])
        P("act", lambda e: e.activation(out=rstd[:], in_=rstd[:], func=AF.Ln), [b_rstd], [b_rstd])
        P("act", lambda e: e.activation(out=rstd[:], in_=rstd[:], func=AF.Exp, scale=-0.5), [b_rstd], [b_rstd])
        yield
        P("dve", lambda e: e.scalar_tensor_tensor(out=hn[:], in0=xt[:], scalar=rstd[:, 0:1], in1=gbc[:], op0=ALU.mult, op1=ALU.mult),
          [b_xt, b_rstd, b_gbc], [b_hn])
        yield
        for half, (pt_, bpt_) in enumerate(((pX, b_pX), (pYk, b_pYk))):
            for k4 in range(4):
                k = half * 4 + k4
                P("pe", lambda e, k=k, k4=k4, pt_=pt_: e.transpose(pt_[:, k4, :], hn[:, k * 128:(k + 1) * 128], idf[:]), [b_hn, b_idf], [bpt_])
            P("act", lambda e, half=half, pt_=pt_: e.copy(out=hnT[:, half * 4:(half + 1) * 4, :], in_=pt_[:]), [bpt_], [b_hnT])
            yield
        def stage0(g):
            wi = wcount[0] % NWB; wcount[0] += 1
            src = win_d[:, g * 512:(g + 1) * 512].rearrange("(k p) f -> p k f", p=128)
            kb.op("pool", lambda e, wi=wi, src=src: e.dma_start(out=wst[wi][:], in_=src), writes=[b_wst[wi]], dma_sem=s_wst[wi])
            for c4 in range(4):
                for k in range(8):
                    P("pe", lambda e, wi=wi, c4=c4, k=k: e.matmul(pX[:, c4, :], wst[wi][:, k, c4 * 128:(c4 + 1) * 128], hnT[:, k, :], start=(k == 0), stop=(k == 7)),
                      [b_wst[wi], b_hnT], [b_pX])
            xc = xcw4[g % 2]; bxc = b_xcw4[g % 2]
            P("pool", lambda e, xc=xc, g=g: e.tensor_copy(out=xc[:, :, 0:3], in_=hist[:, g * 4:(g + 1) * 4, :]), [b_hist[g]], [bxc])

        def stage1(g):
            xc = xcw4[g % 2]; bxc = b_xcw4[g % 2]
            P("act", lambda e, xc=xc: e.copy(out=xc[:, :, 3:131], in_=pX[:]), [b_pX], [bxc])

        def wbc(g, j):
            return cw[:, g * 4:(g + 1) * 4, j:j + 1].to_broadcast([128, 4, 128])

        stage0(0)
        yield
        stage1(0)
        yield
        for g in range(6):
            xc = xcw4[g % 2]; bxc = b_xcw4[g % 2]
            if g + 1 < 6:
                stage0(g + 1)
            P("pool", lambda e, xc=xc, g=g: e.tensor_copy(out=hist[:, g * 4:(g + 1) * 4, :], in_=xc[:, :, 128:131]), [bxc], [b_hist[g]])
            P("dve", lambda e, xc=xc, g=g: e.tensor_tensor(out=cv4[:], in0=xc[:, :, 0:128], in1=wbc(g, 0), op=ALU.mult), [bxc, b_cw], [b_cv4])
            P("dve", lambda e, xc=xc, g=g: e.tensor_tensor(out=ct4[:], in0=xc[:, :, 1:129], in1=wbc(g, 1), op=ALU.mult), [bxc, b_cw], [b_ct4])
            P("dve", lambda e: e.tensor_tensor(out=cv4[:], in0=cv4[:], in1=ct4[:], op=ALU.add), [b_ct4, b_cv4], [b_cv4])
            P("dve", lambda e, xc=xc, g=g: e.tensor_tensor(out=ct4[:], in0=xc[:, :, 2:130], in1=wbc(g, 2), op=ALU.mult), [bxc, b_cw], [b_ct4])
            yield
            if g + 1 < 6:
                stage1(g + 1)
            P("dve", lambda e: e.tensor_tensor(out=cv4[:], in0=cv4[:], in1=ct4[:], op=ALU.add), [b_ct4, b_cv4], [b_cv4])
            P("dve", lambda e, xc=xc, g=g: e.tensor_tensor(out=ct4[:], in0=xc[:, :, 3:131], in1=wbc(g, 3), op=ALU.mult), [bxc, b_cw], [b_ct4])
            P("dve", lambda e: e.tensor_tensor(out=cv4[:], in0=cv4[:], in1=ct4[:], op=ALU.add), [b_ct4, b_cv4], [b_cv4])
            P("act", lambda e: e.activation(out=cv4[:], in_=cv4[:], func=AF.Silu), [b_cv4], [b_cv4])
            yield
            for c4 in range(4):
                P("pe", lambda e, c4=c4: e.transpose(pYk[:, c4, :], cv4[:, c4, :], idf[:]), [b_cv4, b_idf], [b_pYk])
            if g % 2 == 0:
                P("dve", lambda e, g=g: e.tensor_copy(out=tok[:, g * 512:(g + 1) * 512], in_=pYk[:].rearrange("p c d -> p (c d)")), [b_pYk], [b_tok[g]])
            else:
                P("act", lambda e, g=g: e.copy(out=tok[:, g * 512:(g + 1) * 512], in_=pYk[:].rearrange("p c d -> p (c d)")), [b_pYk], [b_tok[g]])
            yield
        for zh in range(2):
            wi = wcount[0] % NWB; wcount[0] += 1
            src = win_d[:, 3072 + zh * 512:3072 + (zh + 1) * 512].rearrange("(k p) f -> p k f", p=128)
            kb.op("pool", lambda e, wi=wi, src=src: e.dma_start(out=wst[wi][:], in_=src), writes=[b_wst[wi]], dma_sem=s_wst[wi])
            for k in range(8):
                P("pe", lambda e, wi=wi, k=k: e.matmul(pX[:].rearrange("p c d -> p (c d)"), hnT[:, k, :], wst[wi][:, k, :], start=(k == 0), stop=(k == 7)),
                  [b_wst[wi], b_hnT], [b_pX])
            P("act", lambda e, zh=zh: e.activation(out=zs[:, zh * 512:(zh + 1) * 512], in_=pX[:].rearrange("p c d -> p (c d)"), func=AF.Silu), [b_pX], [b_zs])
            yield
        for k in range(8):
            P("pe", lambda e, k=k: e.matmul(pS[:, 0:16], hnT[:, k, :], wba[:, k, :], start=(k == 0), stop=(k == 7)), [b_hnT, b_wba], [b_pS])
        R = [b_sc]
        P("act", lambda e: e.activation(out=sc[:, I_EB, :], in_=pS[:, 0:8], func=AF.Exp, scale=-1.0), [b_pS] + R, R)
        P("dve", lambda e: e.tensor_tensor(out=sc[:, I_X, :], in0=pS[:, 8:16], in1=dtb[:], op=ALU.add), [b_pS, b_dtb] + R, R)
        yield
        P("dve", lambda e: e.tensor_scalar(out=sc[:, I_EB, :], in0=sc[:, I_EB, :], scalar1=1.0, scalar2=None, op0=ALU.add), R, R)
        P("act", lambda e: e.activation(out=sc[:, I_SB, :], in_=sc[:, I_EB, :], func=AF.Ln), R, R)
        P("act", lambda e: e.activation(out=sc[:, I_SB, :], in_=sc[:, I_SB, :], func=AF.Exp, scale=-0.5), R, R)
        yield
        P("act", lambda e: e.activation(out=sc[:, I_X, :], in_=sc[:, I_X, :], func=AF.Exp), R, R)
        P("dve", lambda e: e.tensor_scalar(out=sc[:, I_X, :], in0=sc[:, I_X, :], scalar1=1.0, scalar2=None, op0=ALU.add), R, R)
        P("act", lambda e: e.activation(out=sc[:, I_X, :], in_=sc[:, I_X, :], func=AF.Ln), R, R)
        yield
        P("dve", lambda e: e.tensor_tensor(out=sc[:, I_G, :], in0=sc[:, I_X, :], in1=nega[:], op=ALU.mult), R + [b_nega], R)
        P("pe", lambda e: e.matmul(pS[:, 16:24], trit[:], sc[:, I_G, :], start=True, stop=True), R + [b_trit], [b_pS])
        P("pe", lambda e: e.matmul(pS[:, 24:32], bones[:], sc[:, I_G, :], start=True, stop=True), R + [b_bones], [b_pS])
        for c in range(2):
            P("pe", lambda e, c=c: e.matmul(pS[:, 32 + 8 * c:40 + 8 * c], csel[:, c, :], sc[:, I_G, :], start=True, stop=True), R + [b_csel], [b_pS])
        yield
        P("act", lambda e: e.copy(out=sc[:, I_GC, :], in_=pS[:, 16:24]), [b_pS] + R, R)
        P("act", lambda e: e.activation(out=sc[:, I_EGC, :], in_=pS[:, 16:24], func=AF.Exp), [b_pS] + R, R)
        P("dve", lambda e: e.tensor_tensor(out=sc[:, I_EDEC, :], in0=pS[:, 24:32], in1=sc[:, I_GC, :], op=ALU.subtract), [b_pS] + R, R)
        yield
        P("act", lambda e: e.activation(out=sc[:, I_EDEC, :], in_=sc[:, I_EDEC, :], func=AF.Exp), R, R)
        P("act", lambda e: e.activation(out=egl[:].rearrange("p c h -> p (c h)"), in_=pS[:, 32:48], func=AF.Exp), [b_pS], [b_egl])
        yield
        for hh in range(16):
            P("act", lambda e, hh=hh: e.activation(out=junk[:], in_=tok[:, hh * 128:(hh + 1) * 128], func=AF.Square, accum_out=qkss[:, hh:hh + 1]),
              [b_tok[hh // 4]], [b_junk, b_qkss])
            if hh % 4 == 3:
                yield
        P("dve", lambda e: e.tensor_scalar(out=sc[:, I_RQ, :], in0=qkss[:, 0:8], scalar1=EPS, scalar2=128.0, op0=ALU.add, op1=ALU.mult), [b_qkss] + R, R)
        P("dve", lambda e: e.tensor_scalar(out=sc[:, I_RK, :], in0=qkss[:, 8:16], scalar1=EPS, scalar2=None, op0=ALU.add), [b_qkss] + R, R)
        P("act", lambda e: e.activation(out=sc[:, I_RQ:I_RK + 1, :], in_=sc[:, I_RQ:I_RK + 1, :], func=AF.Ln), R, R)
        P("act", lambda e: e.activation(out=sc[:, I_RQ:I_RK + 1, :], in_=sc[:, I_RQ:I_RK + 1, :], func=AF.Exp, scale=-0.5), R, R)
        yield
        P("dve", lambda e: e.tensor_tensor(out=sc[:, I_SKB, :], in0=sc[:, I_RK, :], in1=sc[:, I_SB, :], op=ALU.mult), R, R)
        P("dve", lambda e: e.tensor_tensor(out=sc[:, I_SKG, :], in0=sc[:, I_RK, :], in1=sc[:, I_EGC, :], op=ALU.mult), R, R)
        P("dve", lambda e: e.tensor_tensor(out=sc[:, I_SKD, :], in0=sc[:, I_RK, :], in1=sc[:, I_EDEC, :], op=ALU.mult), R, R)
        P("dve", lambda e: e.tensor_tensor(out=sc[:, I_SQG, :], in0=sc[:, I_RQ, :], in1=sc[:, I_EGC, :], op=ALU.mult), R, R)
        P("dve", lambda e: e.tensor_scalar(out=sc[:, I_NG, :], in0=sc[:, I_G, :], scalar1=-1.0, scalar2=None, op0=ALU.mult), R, R)
        yield

    def grp(gq, t):
        p = t % 2
        sc = scs[p]; b_sc = b_scs[p]; egl = egls[p]; b_egl = b_egls[p]; otok = otoks[p]; b_otok = b_otoks[p]
        def bc4(col, gq, lo=0, hi=128):
            return sc[lo:hi, col, gq * 4:(gq + 1) * 4].unsqueeze(2).to_broadcast([hi - lo, 4, 128])
        def palloc():
            i_ = gctr[gq]; gctr[gq] += 1
            return GSLOT[gq][i_ % len(GSLOT[gq])]
        kbt, b_kbt = GB[gq]['kbt']
        knt, b_knt = GB[gq]['knt']
        kgt, b_kgt = GB[gq]['kgt']
        kdt, b_kdt = GB[gq]['kdt']
        qnt, b_qnt = GB[gq]['qnt']
        qgt, b_qgt = GB[gq]['qgt']
        kbT, b_kbT = GB[gq]['kbT']
        knT, b_knT = GB[gq]['knT']
        qnT, b_qnT = GB[gq]['qnT']
        QG0, b_QG0 = GB[gq]['QG0']
        QG1, b_QG1 = GB[gq]['QG1']
        GT, b_GT = GB[gq]['GT']
        NGT, b_NGT = GB[gq]['NGT']
        Dm, b_Dm = GB[gq]['Dm']
        Dn, b_Dn = GB[gq]['Dn']
        L3, b_L3 = GB[gq]['L3']
        qkT, b_qkT = GB[gq]['qkT']
        W0, b_W0 = GB[gq]['W0']
        W1, b_W1 = GB[gq]['W1']
        vn, b_vn = GB[gq]['vn']
        Ball, b_Ball = BALL[gq]
        Y, b_Y = GB[gq]['kbT']
        Qs, b_Qs = GB[gq]['kbt']
        YTs, b_YTs = GB[gq]['knt']
        Yb, b_Yb = GB[gq]['GT']
        tmpS, b_tmpS = GB[gq]['qgt']
        h0 = gq * 4
        kraw = toks[p][:, 1024 + h0 * 128:1024 + (h0 + 4) * 128].rearrange("p (h d) -> p h d", h=4)
        qraw = toks[p][:, h0 * 128:(h0 + 4) * 128].rearrange("p (h d) -> p h d", h=4)
        vraw = toks[p][:, 2048 + h0 * 128:2048 + (h0 + 4) * 128].rearrange("p (h d) -> p h d", h=4)
        bk = b_toks[p][2 + gq]; bq = b_toks[p][gq]; bv = b_toks[p][4 + gq]
        def scaled(out, bout, raw, braw, col, eng):
            in1 = bc4(col, gq)
            P(eng, lambda e: e.tensor_tensor(out=out[:], in0=raw, in1=in1, op=ALU.mult), [braw, b_sc], [bout])
        scaled(kbt, b_kbt, kraw, bk, I_SKB, "dve")
        scaled(knt, b_knt, kraw, bk, I_RK, "pool")
        scaled(qnt, b_qnt, qraw, bq, I_RQ, "dve")
        scaled(qgt, b_qgt, qraw, bq, I_SQG, "pool")
        P("pool", lambda e, gq=gq: e.tensor_tensor(out=GT[:], in0=trit[:].unsqueeze(1).to_broadcast([128, 4, 128]), in1=bc4(I_G, gq), op=ALU.mult),
          [b_trit, b_sc], [b_GT])
        P("pool", lambda e, gq=gq: e.tensor_tensor(out=NGT[:], in0=trit[:].unsqueeze(1).to_broadcast([128, 4, 128]), in1=bc4(I_NG, gq), op=ALU.mult),
          [b_trit, b_sc], [b_NGT])
        scaled(kgt, b_kgt, kraw, bk, I_SKG, "dve")
        scaled(kdt, b_kdt, kraw, bk, I_SKD, "pool")
        yield

        def tr4(src, bsrc, lowp=True):
            pb, bpb = palloc()
            for h in range(4):
                if lowp:
                    P("pe", lambda e, h=h, pb=pb: e.matmul(pb[:, h, :], src[:, h, :], idb[:], start=True, stop=True), [bsrc, b_idb], [bpb])
                else:
                    P("pe", lambda e, h=h, pb=pb: e.transpose(pb[:, h, :], src[:, h, :], idf[:]), [bsrc, b_idf], [bpb])
            return pb, bpb
        pb1, bpb1 = tr4(kbt, b_kbt)
        pb2, bpb2 = tr4(knt, b_knt)
        yield
        P("act", lambda e, pb=pb1: e.copy(out=kbT[:], in_=pb[:]), [bpb1], [b_kbT])
        P("dve", lambda e, pb=pb2: e.tensor_copy(out=knT[:], in_=pb[:]), [bpb2], [b_knT])
        yield
        pb1, bpb1 = tr4(qnt, b_qnt)
        pb2, bpb2 = tr4(qgt, b_qgt, lowp=False)
        yield
        P("act", lambda e, pb=pb1: e.copy(out=qnT[:], in_=pb[:]), [bpb1], [b_qnT])
        P("dve", lambda e, pb=pb2: e.tensor_copy(out=QG0[:, :, 0:64], in_=pb[:, :, 0:64]), [bpb2], [b_QG0])
        P("act", lambda e, pb=pb2: e.copy(out=QG1[:, :, 64:128], in_=pb[:, :, 64:128]), [bpb2], [b_QG1])
        yield
        pD, bpD = palloc()
        for h in range(4):
            P("pe", lambda e, h=h, pD=pD: e.matmul(pD[:, h, :], GT[:, h, :], bones[:], start=True, stop=False), [b_GT, b_bones], [bpD])
            P("pe", lambda e, h=h, pD=pD: e.matmul(pD[:, h, :], bones[:], NGT[:, h, :], start=False, stop=True), [b_NGT, b_bones], [bpD])
        pK, bpK = palloc()
        for h in range(4):
            P("pe", lambda e, h=h, pK=pK: e.matmul(pK[:, h, :], kbT[:, h, :], kbT[:, h, :], start=True, stop=True), [b_kbT], [bpK])
        yield
        P("dve", lambda e, pD=pD: e.tensor_scalar(out=Dm[:], in0=pD[:], scalar1=0.0, scalar2=None, op0=ALU.min), [bpD], [b_Dm])
        P("dve", lambda e, pD=pD: e.tensor_scalar(out=Dn[:], in0=pD[:], scalar1=-1.0, scalar2=0.0, op0=ALU.mult, op1=ALU.min), [bpD], [b_Dn])
        yield
        P("act", lambda e: e.activation(out=Dm[:], in_=Dm[:], func=AF.Exp), [b_Dm], [b_Dm])
        P("act", lambda e: e.activation(out=Dn[:], in_=Dn[:], func=AF.Exp), [b_Dn], [b_Dn])
        pQ, bpQ = palloc()
        for h in range(4):
            P("pe", lambda e, h=h, pQ=pQ: e.matmul(pQ[:, h, :], knT[:, h, :], qnT[:, h, :], start=True, stop=True), [b_knT, b_qnT], [bpQ])
        yield
        P("dve", lambda e, pK=pK: e.tensor_tensor(out=L3[:], in0=pK[:], in1=Dm[:], op=ALU.mult), [bpK, b_Dm], [b_L3])
        P("pool", lambda e: e.tensor_tensor(out=Dn[:], in0=Dn[:], in1=muin[:].unsqueeze(1).to_broadcast([128, 4, 128]), op=ALU.mult), [b_Dn, b_muin], [b_Dn])
        yield
        P("pool", lambda e: e.tensor_tensor(out=Ball[:], in0=L3[:].unsqueeze(1).to_broadcast([128, 6, 4, 128]),
                                            in1=lvm[:].unsqueeze(2).to_broadcast([128, 6, 4, 128]), op=ALU.mult), [b_L3, b_lvm], [b_Ball])
        P("dve", lambda e, pQ=pQ: e.tensor_tensor(out=qkT[:], in0=pQ[:], in1=Dn[:], op=ALU.mult), [bpQ, b_Dn], [b_qkT])
        yield
        for lv in range(6):
            pq, bpq = palloc()
            if lv == 0:
                for h in range(4):
                    P("pe", lambda e, h=h, pq=pq: e.matmul(pq[:, h, :], Ball[:, 0, h, :], idb[:], start=True, stop=True), [b_Ball, b_idb], [bpq])
                yield
                P("dve", lambda e, pq=pq: e.scalar_tensor_tensor(out=Y[:], in0=pq[:], scalar=-1.0, in1=id4[:], op0=ALU.mult, op1=ALU.add), [bpq, b_id4], [b_Y])
                yield
                continue
            for h in range(4):
                P("pe", lambda e, h=h, pq=pq, lv=lv: e.matmul(pq[:, h, :], Ball[:, lv, h, :], Y[:, h, :], start=True, stop=True), [b_Ball, b_Y], [bpq])
            pt, bpt = tr4(Y, b_Y)
            yield
            P("act", lambda e, pq=pq: e.copy(out=Qs[:], in_=pq[:]), [bpq], [b_Qs])
            P("dve", lambda e, pt=pt: e.tensor_copy(out=YTs[:], in_=pt[:]), [bpt], [b_YTs])
            yield
            py, bpy = palloc()
            for h in range(4):
                P("pe", lambda e, h=h, py=py: e.matmul(py[:, h, :], YTs[:, h, :], Qs[:, h, :], start=True, stop=True), [b_YTs, b_Qs], [bpy])
            yield
            P("dve", lambda e, py=py: e.tensor_tensor(out=Y[:], in0=Y[:], in1=py[:], op=ALU.subtract), [bpy, b_Y], [b_Y])
            yield
        P("dve", lambda e, gq=gq: e.tensor_tensor(out=Yb[:], in0=Y[:], in1=bc4(I_SB, gq), op=ALU.mult), [b_Y, b_sc], [b_Yb])
        yield
        pw, bpw = palloc()
        for h in range(4):
            P("pe", lambda e, h=h, pw=pw: e.matmul(pw[:, h, :], kgt[:, h, :], Yb[:, h, :], start=True, stop=True), [b_kgt, b_Yb], [bpw])
        yield
        P("dve", lambda e, pw=pw: e.tensor_scalar(out=W0[:, :, 0:64], in0=pw[:, :, 0:64], scalar1=-1.0, scalar2=None, op0=ALU.mult), [bpw], [b_W0])
        P("act", lambda e, pw=pw: e.activation(out=W1[:, :, 64:128], in_=pw[:, :, 64:128], func=AF.Identity, scale=-1.0), [bpw], [b_W1])
        yield
        si = sidx[gq]
        Sa, bSa = St[gq][si % 3], b_St[gq][si % 3]
        Sb, bSb = St[gq][(si + 1) % 3], b_St[gq][(si + 1) % 3]
        Sc_, bSc = St[gq][(si + 2) % 3], b_St[gq][(si + 2) % 3]
        sidx[gq] = si + 2
        for c, (Sin, bSin, Sout, bSout, Wc, bWc) in enumerate(((Sa, bSa, Sb, bSb, W0, b_W0), (Sb, bSb, Sc_, bSc, W1, b_W1))):
            lo, hi = c * 64, c * 64 + 64
            pv, bpv = palloc()
            for h in range(4):
                P("pe", lambda e, h=h, pv=pv, vraw=vraw: e.matmul(pv[:, h, :], Yb[:, h, :], vraw[:, h, :], start=True, stop=False), [b_Yb, bv], [bpv])
                P("pe", lambda e, h=h, pv=pv, Wc=Wc, Sin=Sin: e.matmul(pv[:, h, :], Wc[:, h, :], Sin[:, h, :], start=False, stop=True), [bWc, bSin], [bpv])
            yield
            P("dve", lambda e, pv=pv, lo=lo, hi=hi, gq=gq: e.tensor_tensor(out=vn[lo:hi], in0=pv[lo:hi], in1=bc4(I_SB, gq, lo, hi), op=ALU.mult),
              [bpv, b_sc], [b_vn])
            yield
            ps_, bps = palloc()
            for h in range(4):
                P("pe", lambda e, h=h, ps_=ps_, lo=lo, hi=hi: e.matmul(ps_[:, h, :], kdt[lo:hi, h, :], vn[lo:hi, h, :], start=True, stop=True), [b_kdt, b_vn], [bps])
            P("pool", lambda e, c=c, gq=gq, Sin=Sin: e.tensor_tensor(out=tmpS[:], in0=Sin[:], in1=egl[:, c, gq * 4:(gq + 1) * 4].unsqueeze(2).to_broadcast([128, 4, 128]), op=ALU.mult),
              [bSin, b_egl], [b_tmpS])
            yield
            P("dve", lambda e, ps_=ps_, Sout=Sout: e.tensor_tensor(out=Sout[:], in0=tmpS[:], in1=ps_[:], op=ALU.add), [b_tmpS, bps], [bSout])
            yield
        pO, bpO = palloc()
        for h in range(4):
            P("pe", lambda e, h=h, pO=pO, Sa=Sa: e.matmul(pO[:, h, :], QG0[:, h, :], Sa[:, h, :], start=True, stop=False), [b_QG0, bSa], [bpO])
            P("pe", lambda e, h=h, pO=pO, Sb=Sb: e.matmul(pO[:, h, :], QG1[:, h, :], Sb[:, h, :], start=False, stop=False), [b_QG1, bSb], [bpO])
            P("pe", lambda e, h=h, pO=pO: e.matmul(pO[:, h, :], qkT[:, h, :], vn[:, h, :], start=False, stop=True), [b_qkT, b_vn], [bpO])
        yield
        P("act", lambda e, pO=pO, gq=gq: e.copy(out=otok[:, gq * 512:(gq + 1) * 512], in_=pO.rearrange("p h d -> p (h d)")), [bpO], [b_otok[gq]])
        yield

    def stageD(t):
        p = t % 2
        otok = otoks[p]; b_otok = b_otoks[p]
        zs = zss[t % 3]; b_zs = b_zss[t % 3]
        for hh in range(8):
            P("act", lambda e, hh=hh: e.activation(out=junkD[:], in_=otok[:, hh * 128:(hh + 1) * 128], func=AF.Square, accum_out=qkssD[:, hh:hh + 1]),
              [b_otok[hh // 4]], [b_junkD, b_qkssD])
            if hh % 4 == 3:
                yield
        P("dve", lambda e: e.tensor_scalar(out=scD[:], in0=qkssD[:], scalar1=1.0 / 128, scalar2=EPS, op0=ALU.mult, op1=ALU.add), [b_qkssD, b_scD], [b_scD])
        P("act", lambda e: e.activation(out=scD[:], in_=scD[:], func=AF.Ln), [b_scD], [b_scD])
        P("act", lambda e: e.activation(out=scD[:], in_=scD[:], func=AF.Exp, scale=-0.5), [b_scD], [b_scD])
        yield
        P("dve", lambda e: e.tensor_tensor(out=otok[:].rearrange("p (h d) -> p h d", h=8), in0=otok[:].rearrange("p (h d) -> p h d", h=8),
                                           in1=scD[:].unsqueeze(2).to_broadcast([128, 8, 128]), op=ALU.mult), b_otok + [b_scD], b_otok)
        yield
        P("pool", lambda e: e.tensor_tensor(out=otok[:], in0=otok[:], in1=ngbc[:], op=ALU.mult), b_otok + [b_ngbc], b_otok)
        yield
        P("dve", lambda e: e.tensor_tensor(out=otok[:], in0=otok[:], in1=zs[:], op=ALU.mult), b_otok + [b_zs], b_otok)
        yield
        for half in range(2):
            for k4 in range(4):
                k = half * 4 + k4
                P("pe", lambda e, k=k, k4=k4: e.transpose(pD1[:, k4, :], otok[:, k * 128:(k + 1) * 128], idf[:]), b_otok + [b_idf], [b_pD1])
            P("act", lambda e, half=half: e.copy(out=ogT[:, half * 4:(half + 1) * 4, :], in_=pD1[:]), [b_pD1], [b_ogT])
            yield
        rr = t * 128
        kb.op("sp", lambda e: e.dma_start(out=res[:], in_=hin_d[rr:rr + 128, :]), writes=[b_res], dma_sem=s_res)
        for dh in range(2):
            for k in range(8):
                P("pe", lambda e, k=k, dh=dh: e.matmul(pD1[:].rearrange("p c d -> p (c d)"), ogT[:, k, :], wout[:, k, dh * 512:(dh + 1) * 512], start=(k == 0), stop=(k == 7)),
                  [b_ogT, b_wout], [b_pD1])
            yield
            P("dve", lambda e, dh=dh: e.tensor_tensor(out=res[:, dh * 512:(dh + 1) * 512], in0=res[:, dh * 512:(dh + 1) * 512], in1=pD1[:].rearrange("p c d -> p (c d)"), op=ALU.add),
              [b_pD1, b_res], [b_res])
            yield
        kb.op("sp", lambda e: e.dma_start(out=hout_d[rr:rr + 128, :], in_=res[:]), reads=[b_res], dma_sem=s_out)
        yield

    for it in range(-1, NT + 1):
        gens = []
        if 0 <= it < NT:
            gens.append(grp(0, it)); gens.append(grp(1, it))
        if 0 <= it + 1 < NT:
            gens.append(stageAB(it + 1))
        if 0 <= it - 1 < NT:
            gens.append(stageD(it - 1))
        while gens:
            for g_ in list(gens):
                try:
                    next(g_)
                except StopIteration:
                    gens.remove(g_)
    kb.end_phase()


def make_consts():
    c = {}
    c["ident"] = np.eye(128, dtype=np.float32)
    invc = np.zeros((128, 4, 16), np.float32)
    for g in range(4):
        w = 2 ** (g + 1)
        for t in range(16):
            invc[:, g, t] = 1.0 / min(t + 1, w)
    c["invc"] = invc
    i = np.arange(128)
    same = (i[:, None] // 64) == (i[None, :] // 64)
    c["trit"] = (same & (i[:, None] <= i[None, :])).astype(np.float32)
    c["bones"] = same.astype(np.float32)
    cs = np.zeros((128, 2, 128), np.float32)
    cs[:64, 0, :] = 1.0
    cs[64:, 1, :] = 1.0
    c["csel"] = cs
    lvm = np.zeros((128, 6, 128), np.float32)
    for lv in range(6):
        b = 2 ** lv
        m = ((i[:, None] // (2 * b)) == (i[None, :] // (2 * b))) & ((i[:, None] % (2 * b)) >= b) & ((i[None, :] % (2 * b)) < b)
        lvm[:, lv, :] = m
    c["lvm"] = lvm
    c["muin"] = (same & (i[None, :] >= i[:, None])).astype(np.float32)
    c["id4"] = np.ascontiguousarray(np.broadcast_to(np.eye(128, dtype=np.float32)[:, None, :], (128, 4, 128)))
    return c

def bc(v, n=128):
    return np.ascontiguousarray(np.broadcast_to(np.asarray(v, np.float32).reshape(1, -1), (n, np.asarray(v).size)))

def host_inputs(inp):
    d = {}
    for k in ["pool_w_in", "pool_w_group", "dn_w_in", "dn_w_out", "ffn_w_gate", "ffn_w_up", "ffn_w_down",
              "moe_w_gate", "moe_w_up", "moe_w_down"]:
        d[k] = np.ascontiguousarray(inp[k], dtype=np.float32)
    d["pool_scale_bc"] = bc(inp["pool_scale"][0])
    d["g_mix0_bc"] = bc(inp["norm_mix_g"][0])
    d["g_mix1_bc"] = bc(inp["norm_mix_g"][1])
    d["g_ffn0_bc"] = bc(inp["norm_ffn_g"][0])
    d["g_ffn1_bc"] = bc(inp["norm_ffn_g"][1])
    d["g_final_bc"] = bc(inp["final_norm_g"])
    d["router_b_bc"] = bc(inp["moe_router_b"][0])
    d["a_log_bc"] = bc(inp["dn_a_log"][0])
    d["dt_bias_bc"] = bc(inp["dn_dt_bias"][0])
    d["dn_norm_g_bc"] = bc(np.tile(inp["dn_norm_g"][0], 8))
    d["conv_wl"] = np.ascontiguousarray(inp["dn_conv_w"][0].T.reshape(24, 128, 4).transpose(1, 0, 2))
    d["router_w_l"] = np.ascontiguousarray(inp["moe_router_w"][0].reshape(8, 128, 8).transpose(1, 0, 2))
    d["wba_l"] = np.ascontiguousarray(inp["dn_w_in"][0][:, 4096:4112].reshape(8, 128, 16).transpose(1, 0, 2))
    d.update(make_consts())
    return d

def declare_inputs(nc, d):
    W = {}
    for k, v in d.items():
        W[k] = nc.dram_tensor(k, list(v.shape), F32, kind="ExternalInput").ap()
    return W


def build_program(d):
    nc = bass.Bass("TRN2", target_bir_lowering=False)
    W = declare_inputs(nc, d)
    x_d = nc.dram_tensor("x", [S, D], F32, kind="ExternalInput").ap()
    out_d = nc.dram_tensor("out", [S, D], F32, kind="ExternalOutput").ap()
    h1 = nc.dram_tensor("h1s", [S, D], F32).ap()
    h2 = nc.dram_tensor("h2s", [S, D], F32).ap()
    h3 = nc.dram_tensor("h3s", [S, D], F32).ap()
    kb = KB(nc)
    phase_pool(kb, x_d, h1, W)
    phase_ffn(kb, h1, h2, W, moe=False)
    phase_dn(kb, h2, h3, W)
    phase_ffn(kb, h3, out_d, W, moe=True, final_norm=True)
    kb.close()
    return nc


def kernel(**inputs):
    inp = {k: np.asarray(v) for k, v in inputs.items()}
    d = host_inputs(inp)
    nc = build_program(d)
    x = np.ascontiguousarray(inp["x"], dtype=np.float32)
    nb = x.shape[0]
    in_maps = []
    for b in range(nb):
        m = dict(d)
        m["x"] = x[b]
        in_maps.append(m)
    res = run_bass_kernel_spmd(nc, in_maps, core_ids=list(range(nb)))
    return np.stack([np.asarray(r["out"], dtype=np.float32) for r in res.results], axis=0)
```

```python
import numpy as np
from contextlib import ExitStack
import concourse.bass as bass
import concourse.mybir as mybir
from concourse.bass_utils import run_bass_kernel_spmd

F32 = mybir.dt.float32
BF16 = mybir.dt.bfloat16
I32 = mybir.dt.int32
ALU = mybir.AluOpType
AF = mybir.ActivationFunctionType
AX = mybir.AxisListType


ATTACH_WAITS = True


class Buf:
    __slots__ = ("name", "lw", "rd", "sem", "excl")

    def __init__(self, name):
        self.name = name
        self.excl = False
        self.lw = None
        self.rd = []
        self.sem = None


class Op:
    __slots__ = ("eng", "fn", "deps", "sig", "dma", "semname", "used")

    def __init__(self, eng, fn, dma, semname):
        self.eng = eng
        self.fn = fn
        self.deps = []
        self.sig = None
        self.dma = dma
        self.semname = semname
        self.used = False


class KB:
    ENGS = ("pe", "act", "dve", "pool", "sp")

    def __init__(self, nc):
        self.nc = nc
        self.stack = ExitStack()
        self.sems = {}
        self.semval = {}
        self.free_dma_sems = {"hw": [], "sw": []}
        self.n_dma_sems = 0
        self.n_logical = 0
        for e in ("pe", "act", "dve", "pool"):
            self._mksem("c_" + e)
        self.ops = []
        self.last_dma_on_sem = {}
        self.phase_stack = None
        self.n_emitted = 0

    def _mksem(self, name):
        h = self.stack.enter_context(self.nc.semaphore(name))
        self.sems[name] = h
        self.semval[name] = 0
        return h

    def dma_sem(self, kind):
        if self.free_dma_sems[kind]:
            return self.free_dma_sems[kind].pop()
        name = "d%s%d" % (kind, self.n_dma_sems)
        self.n_dma_sems += 1
        self._mksem(name)
        return name

    def begin_phase(self):
        self.ops = []
        self.last_dma_on_sem = {}
        self.phase_stack = ExitStack()
        self.phase_sems = []
        self.sem_map = {}
        self.bufs = []

    def pbuf(self):
        b = self.buf()
        b.excl = True
        return b

    def buf(self, name="b"):
        b = Buf(name)
        self.bufs.append(b)
        return b

    def sb(self, name, shape, dtype):
        self.uid = getattr(self, "uid", 0) + 1
        return self.phase_stack.enter_context(self.nc.sbuf_tensor("sb%d_%s" % (self.uid, name), list(shape), dtype))

    def ps(self, name, shape, dtype=F32):
        self.uid = getattr(self, "uid", 0) + 1
        return self.phase_stack.enter_context(self.nc.psum_tensor("ps%d_%s" % (self.uid, name), list(shape), dtype))

    def phase_dma_sem(self):
        self.n_logical += 1
        return "L%d" % self.n_logical

    def op(self, eng, fn, reads=(), writes=(), dma_sem=None):
        if dma_sem is not None:
            kind = "sw" if eng == "pool" else "hw"
            key = (dma_sem, kind)
            if key not in self.sem_map:
                ph = self.dma_sem(kind)
                self.sem_map[key] = ph
                self.phase_sems.append((kind, ph))
            dma_sem = self.sem_map[key]
        o = Op(eng, fn, dma_sem is not None, dma_sem)
        deps = []
        for b in reads:
            if b.lw is not None:
                deps.append(b.lw)
            if b.excl:
                deps.extend(r for r in b.rd if r.eng != eng)
        for b in writes:
            if b.lw is not None:
                deps.append(b.lw)
            deps.extend(b.rd)
        if dma_sem is not None:
            p = self.last_dma_on_sem.get(dma_sem)
            if p is not None:
                deps.append(p)
            self.last_dma_on_sem[dma_sem] = o
        seen = set()
        for d in deps:
            if id(d) in seen or d is o:
                continue
            seen.add(id(d))
            if eng == "pe" and d.eng == "pe" and not d.dma and not o.dma:
                continue
            o.deps.append(d)
            d.used = True
        for b in reads:
            b.rd.append(o)
        for b in writes:
            b.lw = o
            b.rd = []
        self.ops.append(o)
        return o

    def end_phase(self, final_wait=True):
        nc = self.nc
        for o in self.ops:
            if o.dma:
                self.semval[o.semname] += 16
                o.sig = (o.semname, self.semval[o.semname])
            elif o.used:
                s = "c_" + o.eng
                self.semval[s] += 1
                o.sig = (s, self.semval[s])
        per = {e: [] for e in self.ENGS}
        for o in self.ops:
            per[o.eng].append(o)
        final = [(s, self.semval[s]) for s in set(o.semname for o in self.ops if o.dma)]
        sems = self.sems
        self.n_emitted += len(self.ops)

        def emit(engname, handle):
            waited = {}
            for o in per[engname]:
                need = {}
                for d in o.deps:
                    s, v = d.sig
                    if waited.get(s, 0) < v and need.get(s, 0) < v:
                        need[s] = v
                need = list(need.items())
                attach = None
                if ATTACH_WAITS and need and not o.dma:
                    attach = need.pop()
                for s, v in need:
                    handle.wait_ge(sems[s], v)
                    waited[s] = v
                ins = o.fn(handle)
                if attach is not None:
                    ins._wait_ge(sems[attach[0]], attach[1])
                    waited[attach[0]] = attach[1]
                if o.sig is not None:
                    ins.then_inc(sems[o.sig[0]], 16 if o.dma else 1)
            if engname == "sp" and final_wait:
                for s, v in final:
                    if waited.get(s, 0) < v:
                        handle.wait_ge(sems[s], v)

        with nc.Block() as block:
            @block.sync
            def _(e):
                emit("sp", e)

            @block.tensor
            def _(e):
                emit("pe", e)

            @block.scalar
            def _(e):
                emit("act", e)

            @block.vector
            def _(e):
                emit("dve", e)

            @block.gpsimd
            def _(e):
                emit("pool", e)
        for kind, s in self.phase_sems:
            self.free_dma_sems[kind].append(s)
        self.phase_stack.close()
        self.phase_stack = None
        self.ops = []

    def close(self):
        self.stack.close()


EPS = 1e-6
S = 4096
D = 1024


def norm_tiles(kb, nt, src, b_src, gbc, b_gbc, hn, b_hn, ss, b_ss, rstd, b_rstd, junk, b_junk):
    for j in range(nt):
        kb.op("act", lambda e, j=j: e.activation(out=junk[:], in_=src[:, j, :], func=AF.Square, accum_out=ss[:, j:j + 1]),
              reads=[b_src[j]], writes=[b_junk, b_ss])
    kb.op("dve", lambda e: e.tensor_scalar(out=rstd[:, 0:nt], in0=ss[:, 0:nt], scalar1=1.0 / D, scalar2=EPS, op0=ALU.mult, op1=ALU.add),
          reads=[b_ss], writes=[b_rstd])
    kb.op("act", lambda e: e.activation(out=rstd[:, 0:nt], in_=rstd[:, 0:nt], func=AF.Sqrt), reads=[b_rstd], writes=[b_rstd])
    kb.op("dve", lambda e: e.reciprocal(out=rstd[:, 0:nt], in_=rstd[:, 0:nt]), reads=[b_rstd], writes=[b_rstd])
    for j in range(nt):
        kb.op("dve", lambda e, j=j: e.scalar_tensor_tensor(out=hn[:, j, :], in0=src[:, j, :], scalar=rstd[:, j:j + 1], in1=gbc[:],
                                                             op0=ALU.mult, op1=ALU.mult),
              reads=[b_src[j], b_rstd, b_gbc], writes=[b_hn[j]])


def phase_pool(kb, x_d, h1_d, W):
    nc = kb.nc
    kb.begin_phase()
    NB = S // 512
    win = kb.sb("win", [128, 8, 1024], BF16); b_win = kb.buf()
    wgs = kb.sb("wgs", [128, 4, 2, 256], F32); b_wgs = kb.buf()
    wg = kb.sb("wg", [128, 4, 2, 256], BF16); b_wg = kb.buf()
    scbc = kb.sb("scbc", [128, 1024], F32); b_scbc = kb.buf()
    gbc = kb.sb("gbc", [128, 1024], F32); b_gbc = kb.buf()
    idf = kb.sb("idf", [128, 128], F32); b_idf = kb.buf()
    invc = kb.sb("invc", [128, 4, 16], F32); b_invc = kb.buf()
    sq = kb.phase_dma_sem
    kb.op("pool", lambda e: e.dma_start(out=win[:], in_=W["pool_w_in"][0].rearrange("(k p) n -> p k n", p=128)), writes=[b_win], dma_sem=sq())
    kb.op("sp", lambda e: e.dma_start(out=wgs[:], in_=W["pool_w_group"][0].rearrange("g (k p) e -> p g k e", p=128)), writes=[b_wgs], dma_sem=sq())
    kb.op("sp", lambda e: e.dma_start(out=scbc[:], in_=W["pool_scale_bc"]), writes=[b_scbc], dma_sem=sq())
    kb.op("sp", lambda e: e.dma_start(out=gbc[:], in_=W["g_mix0_bc"]), writes=[b_gbc], dma_sem=sq())
    kb.op("sp", lambda e: e.dma_start(out=idf[:], in_=W["ident"]), writes=[b_idf], dma_sem=sq())
    kb.op("sp", lambda e: e.dma_start(out=invc[:], in_=W["invc"]), writes=[b_invc], dma_sem=sq())
    for kk in range(2):
        kb.op("dve", lambda e, kk=kk: e.tensor_tensor(out=wg[:, :, kk, :], in0=wgs[:, :, kk, :],
                                                       in1=scbc[:].rearrange("p (g e) -> p g e", g=4), op=ALU.mult),
              reads=[b_wgs, b_scbc], writes=[b_wg])

    NBUF = 3
    xt = [kb.sb("xt%d" % i, [128, 4, 1024], F32) for i in range(NBUF)]
    b_xt = [[kb.buf() for _ in range(4)] for i in range(NBUF)]
    s_xt = [sq() for i in range(NBUF)]
    s_st = [sq() for i in range(NBUF)]
    hns = [kb.sb("hn%d" % i, [128, 4, 1024], F32) for i in range(2)]; b_hns = [[kb.buf() for _ in range(4)] for _ in range(2)]
    hnTs = [kb.sb("hnT%d" % i, [128, 8, 512], BF16) for i in range(2)]; b_hnTs = [kb.buf() for _ in range(2)]
    ss = kb.sb("ss", [128, 4], F32); b_ss = kb.buf()
    rstd = kb.sb("rstd", [128, 4], F32); b_rstd = kb.buf()
    junk = kb.sb("junk", [128, 1024], BF16); b_junk = kb.buf()
    U = [kb.sb("U%d" % c, [128, 528], F32) for c in range(8)]; b_U = [kb.buf() for _ in range(8)]
    NAB = 3
    A = [kb.sb("A%d" % i, [128, 528], F32) for i in range(NAB)]; b_A = [kb.buf() for _ in range(NAB)]
    B = [kb.sb("B%d" % i, [128, 528], F32) for i in range(NAB)]; b_B = [kb.buf() for _ in range(NAB)]
    mTs = [kb.sb("mT%d" % i, [128, 8, 512], BF16) for i in range(2)]; b_mTs = [[kb.buf() for _ in range(8)] for _ in range(2)]
    tmp16 = kb.sb("tmp16", [128, 16], F32); b_tmp16 = kb.buf()
    pT = [kb.ps("pT%d" % i, [128, 2, 512], F32) for i in range(2)]; b_pT = [kb.pbuf() for _ in range(2)]
    pU = [kb.ps("pU%d" % i, [128, 512], F32) for i in range(2)]; b_pU = [kb.pbuf() for _ in range(2)]
    pY = kb.ps("pY", [128, 2, 512], F32); b_pY = kb.pbuf()

    for c in range(8):
        kb.op("pool", lambda e, c=c: e.memset(U[c][:, 0:16], 0.0), writes=[b_U[c]])

    def stA(blk):
        xb = xt[blk % NBUF]; bx = b_xt[blk % NBUF]
        hn = hns[blk % 2]; b_hn = b_hns[blk % 2]; hnT = hnTs[blk % 2]; b_hnT = b_hnTs[blk % 2]
        r0 = blk * 512
        kb.op("sp", lambda e: e.dma_start(out=xb[:], in_=x_d[r0:r0 + 512, :].rearrange("(j p) d -> p j d", p=128)),
              writes=bx, dma_sem=s_xt[blk % NBUF])
        yield
        for j in range(4):
            kb.op("act", lambda e, j=j: e.activation(out=junk[:], in_=xb[:, j, :], func=AF.Square, accum_out=ss[:, j:j + 1]),
                  reads=[bx[j]], writes=[b_junk, b_ss])
        yield
        kb.op("dve", lambda e: e.tensor_scalar(out=rstd[:], in0=ss[:], scalar1=1.0 / D, scalar2=EPS, op0=ALU.mult, op1=ALU.add), reads=[b_ss], writes=[b_rstd])
        yield
        kb.op("act", lambda e: e.activation(out=rstd[:], in_=rstd[:], func=AF.Sqrt), reads=[b_rstd], writes=[b_rstd])
        yield
        kb.op("dve", lambda e: e.reciprocal(out=rstd[:], in_=rstd[:]), reads=[b_rstd], writes=[b_rstd])
        yield
        for j in range(4):
            kb.op("dve", lambda e, j=j: e.scalar_tensor_tensor(out=hn[:, j, :], in0=xb[:, j, :], scalar=rstd[:, j:j + 1], in1=gbc[:], op0=ALU.mult, op1=ALU.mult),
                  reads=[bx[j], b_rstd, b_gbc], writes=[b_hn[j]])
            yield
        for j in range(4):
            pt = pT[j % 2]; bp = b_pT[j % 2]
            for k in range(8):
                kb.op("pe", lambda e, j=j, k=k, pt=pt: e.transpose(pt[:, k // 4, (k % 4) * 128:(k % 4 + 1) * 128], hn[:, j, k * 128:(k + 1) * 128], idf[:]),
                      reads=[b_hn[j], b_idf], writes=[bp])
            yield
            kb.op("act", lambda e, j=j, pt=pt: e.copy(out=hnT[:, :, j * 128:(j + 1) * 128], in_=pt[:].rearrange("p a (b t) -> p (a b) t", t=128)),
                  reads=[bp], writes=[b_hnT])
            yield

    def stB(blk):
        hnT = hnTs[blk % 2]; b_hnT = b_hnTs[blk % 2]; mT = mTs[blk % 2]; b_mT = b_mTs[blk % 2]
        state = {}
        for step in range(8 + 2):
            c = step
            if c < 8:
                pu = pU[c % 2]; bpu = b_pU[c % 2]
                for k in range(8):
                    kb.op("pe", lambda e, c=c, k=k, pu=pu: e.matmul(pu[:], win[:, k, c * 128:(c + 1) * 128], hnT[:, k, :], start=(k == 0), stop=(k == 7)),
                          reads=[b_hnT, b_win], writes=[bpu])
                kb.op("act", lambda e, c=c, pu=pu: e.copy(out=U[c][:, 16:528], in_=pu[:]), reads=[bpu], writes=[b_U[c]])
            c = step - 1
            if 0 <= c < 8:
                g = c // 2
                w = 2 ** (g + 1)
                a = A[c % NAB]; ba = b_A[c % NAB]; b = B[c % NAB]; bb = b_B[c % NAB]
                kb.op("pool", lambda e, c=c, a=a: e.tensor_tensor(out=a[:, 1:528], in0=U[c][:, 1:528], in1=U[c][:, 0:527], op=ALU.add),
                      reads=[b_U[c]], writes=[ba])
                cur, bcur = a, ba
                if w >= 4:
                    kb.op("pool", lambda e, a=a, b=b: e.tensor_tensor(out=b[:, 3:528], in0=a[:, 3:528], in1=a[:, 1:526], op=ALU.add),
                          reads=[ba], writes=[bb])
                    cur, bcur = b, bb
                if w >= 8:
                    kb.op("pool", lambda e, a=a, b=b: e.tensor_tensor(out=a[:, 7:528], in0=b[:, 7:528], in1=b[:, 3:524], op=ALU.add),
                          reads=[bb], writes=[ba])
                    cur, bcur = a, ba
                if w >= 16:
                    kb.op("pool", lambda e, a=a, b=b: e.tensor_tensor(out=b[:, 15:528], in0=a[:, 15:528], in1=a[:, 7:520], op=ALU.add),
                          reads=[ba], writes=[bb])
                    cur, bcur = b, bb
                state[c] = (cur, bcur, w, g)
            c = step - 2
            if 0 <= c < 8:
                cur, bcur, w, g = state[c]
                kb.op("dve", lambda e, c=c, cur=cur, w=w: e.scalar_tensor_tensor(out=mT[:, c, :], in0=cur[:, 16:528], scalar=1.0 / w, in1=U[c][:, 16:528],
                                                                                op0=ALU.mult, op1=ALU.subtract),
                      reads=[bcur, b_U[c]], writes=[b_mT[c]])
                if blk == 0:
                    kb.op("dve", lambda e, cur=cur, g=g: e.tensor_tensor(out=tmp16[:], in0=cur[:, 16:32], in1=invc[:, g, :], op=ALU.mult),
                          reads=[bcur, b_invc], writes=[b_tmp16])
                    kb.op("dve", lambda e, c=c: e.tensor_tensor(out=mT[:, c, 0:16], in0=tmp16[:], in1=U[c][:, 16:32], op=ALU.subtract),
                          reads=[b_tmp16, b_U[c]], writes=[b_mT[c]])
                kb.op("pool", lambda e, c=c: e.tensor_copy(out=U[c][:, 0:16], in_=U[c][:, 512:528]), reads=[b_U[c]], writes=[b_U[c]])
            yield

    def stC(blk):
        xb = xt[blk % NBUF]; bx = b_xt[blk % NBUF]
        mT = mTs[blk % 2]; b_mT = b_mTs[blk % 2]
        r0 = blk * 512
        for j in range(4):
            for g in range(4):
                for kk in range(2):
                    kb.op("pe", lambda e, j=j, g=g, kk=kk: e.matmul(pY[:, g // 2, (g % 2) * 256:(g % 2 + 1) * 256],
                                                                   mT[:, 2 * g + kk, j * 128:(j + 1) * 128], wg[:, g, kk, :],
                                                                   start=(kk == 0), stop=(kk == 1)),
                          reads=[b_mT[2 * g + kk], b_wg], writes=[b_pY])
            yield
            for hh in range(2):
                kb.op("dve", lambda e, j=j, hh=hh: e.tensor_tensor(out=xb[:, j, hh * 512:(hh + 1) * 512], in0=xb[:, j, hh * 512:(hh + 1) * 512],
                                                                   in1=pY[:, hh, :], op=ALU.add),
                      reads=[b_pY, bx[j]], writes=[bx[j]])
            yield
        kb.op("sp", lambda e: e.dma_start(out=h1_d[r0:r0 + 512, :].rearrange("(j p) d -> p j d", p=128), in_=xb[:]),
              reads=bx, dma_sem=s_st[blk % NBUF])
        yield

    for it in range(-1, NB + 1):
        gens = []
        if 0 <= it < NB:
            gens.append(stB(it))
        if 0 <= it - 1 < NB:
            gens.append(stC(it - 1))
        if 0 <= it + 1 < NB:
            gens.append(stA(it + 1))
        while gens:
            for g_ in list(gens):
                try:
                    next(g_)
                except StopIteration:
                    gens.remove(g_)
    kb.end_phase()


def phase_ffn(kb, hin_d, hout_d, W, moe, final_norm=False):
    nc = kb.nc
    if moe:
        NE, FF = 8, 3584
        wg_d, wu_d, wd_d = W["moe_w_gate"][0], W["moe_w_up"][0], W["moe_w_down"][0]
        gname = "g_ffn1_bc"
    else:
        NE, FF = 1, 2816
        wg_d, wu_d, wd_d = W["ffn_w_gate"], W["ffn_w_up"], W["ffn_w_down"]
        gname = "g_ffn0_bc"
    T = 2048
    NT = T // 128
    FB = 512
    blocks = []
    f0 = 0
    while f0 < FF:
        blocks.append((f0, min(FB, FF - f0)))
        f0 += FB
    kb.begin_phase()
    sq = kb.phase_dma_sem
    gbc = kb.sb("gbc", [128, 1024], F32); b_gbc = kb.buf()
    idf = kb.sb("idf", [128, 128], F32); b_idf = kb.buf()
    kb.op("sp", lambda e: e.dma_start(out=gbc[:], in_=W[gname]), writes=[b_gbc], dma_sem=sq())
    kb.op("sp", lambda e: e.dma_start(out=idf[:], in_=W["ident"]), writes=[b_idf], dma_sem=sq())
    if final_norm:
        gfin = kb.sb("gfin", [128, 1024], F32); b_gfin = kb.buf()
        kb.op("sp", lambda e: e.dma_start(out=gfin[:], in_=W["g_final_bc"]), writes=[b_gfin], dma_sem=sq())
    if moe:
        wr = kb.sb("wr", [128, 8, 8], F32); b_wr = kb.buf()
        rb = kb.sb("rb", [128, 8], F32); b_rb = kb.buf()
        kb.op("sp", lambda e: e.dma_start(out=wr[:], in_=W["router_w_l"]), writes=[b_wr], dma_sem=sq())
        kb.op("sp", lambda e: e.dma_start(out=rb[:], in_=W["router_b_bc"]), writes=[b_rb], dma_sem=sq())
        xf = kb.sb("xf", [128, 8, 128], F32); b_xf = kb.buf()
        lg = kb.sb("lg", [128, NT, 8], F32); b_lg = kb.buf()
        gates = kb.sb("gates", [128, NT, 8], F32); b_gates = kb.buf()
        m1 = kb.sb("m1", [128, NT], F32); m2 = kb.sb("m2", [128, NT], F32)
        mk1 = kb.sb("mk1", [128, NT, 8], F32); mk2 = kb.sb("mk2", [128, NT, 8], F32); l2 = kb.sb("l2", [128, NT, 8], F32)
        w1 = kb.sb("w1", [128, NT], F32); w2 = kb.sb("w2", [128, NT], F32)
        b_gt = kb.buf()
    acc = kb.sb("acc", [128, NT, 1024], F32); b_acc = [kb.buf() for _ in range(NT)]
    xnT = kb.sb("xnT", [128, 8, T], BF16); b_xnT = [kb.buf() for _ in range(T // 512)]
    NIF = 3
    hns = [kb.sb("hn%d" % i, [128, 1024], F32) for i in range(NIF)]; b_hns = [kb.buf() for _ in range(NIF)]
    sss = [kb.sb("ss%d" % i, [128, 1], F32) for i in range(NIF)]; b_sss = [kb.buf() for _ in range(NIF)]
    rstds = [kb.sb("rstd%d" % i, [128, 1], F32) for i in range(NIF)]; b_rstds = [kb.buf() for _ in range(NIF)]
    junks = [kb.sb("junk%d" % i, [128, 1024], BF16) for i in range(NIF)]; b_junks = [kb.buf() for _ in range(NIF)]
    if moe:
        xfs = [xf] + [kb.sb("xf%d" % i, [128, 8, 128], F32) for i in range(1, NIF)]; b_xfs = [b_xf] + [kb.buf() for _ in range(1, NIF)]
    NW = 2
    wgb = [kb.sb("wgb%d" % i, [128, 8, FB], BF16) for i in range(NW)]
    wub = [kb.sb("wub%d" % i, [128, 8, FB], BF16) for i in range(NW)]
    wdb = [kb.sb("wdb%d" % i, [128, FB // 128, 1024], BF16) for i in range(NW)]
    b_wgb = [kb.buf() for _ in range(NW)]; b_wub = [kb.buf() for _ in range(NW)]; b_wdb = [kb.buf() for _ in range(NW)]
    s_wg = [sq() for _ in range(NW)]; s_wu = [sq() for _ in range(NW)]; s_wd = [sq() for _ in range(NW)]
    sg = [kb.sb("sg%d" % i, [128, 512], BF16) for i in range(2)]; b_sg = [kb.buf() for _ in range(2)]
    hm = [kb.sb("hm%d" % i, [128, FB // 128, 512], BF16) for i in range(2)]; b_hm = [[kb.buf() for _ in range(FB // 128)] for _ in range(2)]
    s_ld = [sq() for _ in range(4)]; s_st = [sq() for _ in range(4)]
    pG = [kb.ps("pG%d" % i, [128, 512], F32) for i in range(2)]; b_pG = [kb.pbuf() for _ in range(2)]
    pU = [kb.ps("pU%d" % i, [128, 512], F32) for i in range(2)]; b_pU = [kb.pbuf() for _ in range(2)]
    pY = [kb.ps("pY%d" % i, [128, 512], F32) for i in range(2)]; b_pY = [kb.pbuf() for _ in range(2)]
    pTa = kb.ps("pTa", [128, 512], F32); b_pTa = kb.pbuf()
    pTb = kb.ps("pTb", [128, 512], F32); b_pTb = kb.pbuf()
    SLOT_T = [((pTa, b_pTa), (pTb, b_pTb)), ((pG[0], b_pG[0]), (pG[1], b_pG[1])), ((pU[0], b_pU[0]), (pU[1], b_pU[1]))]
    SLOT_L = [(pY[0][:, 0:8], b_pY[0]), (pY[1][:, 0:8], b_pY[1]), (pY[0][:, 8:16], b_pY[0])]

    def run_lockstep(gens):
        while gens:
            for g_ in list(gens):
                try:
                    next(g_)
                except StopIteration:
                    gens.remove(g_)

    def norm_gen(src, b_src, gain, b_gain, i):
        hn_, ss_, rstd_, junk_ = hns[i], sss[i], rstds[i], junks[i]
        kb.op("act", lambda e: e.activation(out=junk_[:], in_=src, func=AF.Square, accum_out=ss_[:, 0:1]), reads=[b_src], writes=[b_junks[i], b_sss[i]])
        yield
        kb.op("dve", lambda e: e.tensor_scalar(out=rstd_[:], in0=ss_[:], scalar1=1.0 / D, scalar2=EPS, op0=ALU.mult, op1=ALU.add), reads=[b_sss[i]], writes=[b_rstds[i]])
        yield
        kb.op("act", lambda e: e.activation(out=rstd_[:], in_=rstd_[:], func=AF.Sqrt), reads=[b_rstds[i]], writes=[b_rstds[i]])
        yield
        kb.op("dve", lambda e: e.reciprocal(out=rstd_[:], in_=rstd_[:]), reads=[b_rstds[i]], writes=[b_rstds[i]])
        yield
        kb.op("dve", lambda e: e.scalar_tensor_tensor(out=hn_[:], in0=src, scalar=rstd_[:, 0:1], in1=gain[:], op0=ALU.mult, op1=ALU.mult),
              reads=[b_src, b_rstds[i], b_gain], writes=[b_hns[i]])
        yield

    def pro_gen(t, i, t0):
        r0 = t0 + t * 128
        kb.op("sp", lambda e: e.dma_start(out=acc[:, t, :], in_=hin_d[r0:r0 + 128, :]), writes=[b_acc[t]], dma_sem=s_ld[i])
        yield
        for _ in norm_gen(acc[:, t, :], b_acc[t], gbc, b_gbc, i):
            yield
        hn_ = hns[i]
        for half in range(2):
            pt_, bpt_ = SLOT_T[i][half]
            for k4 in range(4):
                k = half * 4 + k4
                kb.op("pe", lambda e, k=k, k4=k4, pt_=pt_: e.transpose(pt_[:, k4 * 128:(k4 + 1) * 128], hn_[:, k * 128:(k + 1) * 128], idf[:]),
                      reads=[b_hns[i], b_idf], writes=[bpt_])
        yield
        for half in range(2):
            pt_, bpt_ = SLOT_T[i][half]
            kb.op("act", lambda e, half=half, pt_=pt_: e.copy(out=xnT[:, half * 4:(half + 1) * 4, t * 128:(t + 1) * 128], in_=pt_[:].rearrange("p (b t) -> p b t", t=128)),
                  reads=[bpt_], writes=[b_xnT[t // 4]])
            if moe:
                kb.op("dve", lambda e, half=half, pt_=pt_: e.tensor_copy(out=xfs[i][:, half * 4:(half + 1) * 4, :], in_=pt_[:].rearrange("p (b t) -> p b t", t=128)),
                      reads=[bpt_], writes=[b_xfs[i]])
        yield
        if moe:
            pL_, bpL_ = SLOT_L[i]
            for k in range(8):
                kb.op("pe", lambda e, k=k: e.matmul(pL_, xfs[i][:, k, :], wr[:, k, :], start=(k == 0), stop=(k == 7)),
                      reads=[b_xfs[i], b_wr], writes=[bpL_])
            yield
            kb.op("dve", lambda e: e.tensor_tensor(out=lg[:, t, :], in0=pL_, in1=rb[:], op=ALU.add), reads=[bpL_, b_rb], writes=[b_lg])
            yield

    def epi_gen(t, i, t0):
        r0 = t0 + t * 128
        for _ in norm_gen(acc[:, t, :], b_acc[t], gfin, b_gfin, i):
            yield
        kb.op("sp", lambda e: e.dma_start(out=hout_d[r0:r0 + 128, :], in_=hns[i][:]), reads=[b_hns[i]], dma_sem=s_st[i])
        yield

    wcount = 0
    for half in range(S // T):
        t0 = half * T
        for base in range(0, NT, NIF):
            run_lockstep([pro_gen(t, t - base, t0) for t in range(base, min(base + NIF, NT))])
        if moe:
            R = [b_lg, b_gt]
            bc = lambda a: a[:].unsqueeze(2).to_broadcast([128, NT, 8])
            kb.op("dve", lambda e: e.tensor_reduce(out=m1[:], in_=lg[:], axis=AX.X, op=ALU.max), reads=R, writes=[b_gt])
            kb.op("dve", lambda e: e.tensor_tensor(out=mk1[:], in0=lg[:], in1=bc(m1), op=ALU.is_equal), reads=R, writes=[b_gt])
            kb.op("dve", lambda e: e.scalar_tensor_tensor(out=l2[:], in0=mk1[:], scalar=-1e30, in1=lg[:], op0=ALU.mult, op1=ALU.add), reads=R, writes=[b_gt])
            kb.op("dve", lambda e: e.tensor_reduce(out=m2[:], in_=l2[:], axis=AX.X, op=ALU.max), reads=R, writes=[b_gt])
            kb.op("dve", lambda e: e.tensor_tensor(out=mk2[:], in0=l2[:], in1=bc(m2), op=ALU.is_equal), reads=R, writes=[b_gt])
            kb.op("dve", lambda e: e.tensor_tensor(out=w2[:], in0=m2[:], in1=m1[:], op=ALU.subtract), reads=R, writes=[b_gt])
            kb.op("act", lambda e: e.activation(out=w2[:], in_=w2[:], func=AF.Exp), reads=R, writes=[b_gt])
            kb.op("dve", lambda e: e.tensor_scalar(out=w1[:], in0=w2[:], scalar1=1.0, scalar2=None, op0=ALU.add), reads=R, writes=[b_gt])
            kb.op("dve", lambda e: e.reciprocal(out=w1[:], in_=w1[:]), reads=R, writes=[b_gt])
            kb.op("dve", lambda e: e.tensor_tensor(out=w2[:], in0=w2[:], in1=w1[:], op=ALU.mult), reads=R, writes=[b_gt])
            kb.op("dve", lambda e: e.tensor_tensor(out=mk1[:], in0=mk1[:], in1=bc(w1), op=ALU.mult), reads=R, writes=[b_gt])
            kb.op("dve", lambda e: e.tensor_tensor(out=mk2[:], in0=mk2[:], in1=bc(w2), op=ALU.mult), reads=R, writes=[b_gt])
            kb.op("dve", lambda e: e.tensor_tensor(out=gates[:], in0=mk1[:], in1=mk2[:], op=ALU.add), reads=R, writes=[b_gates])
        for ex in range(NE):
            for (f0, fsz) in blocks:
                nch = fsz // 128
                wi = wcount % NW
                wcount += 1
                gsrc = wg_d[ex][:, f0:f0 + fsz].rearrange("(k p) f -> p k f", p=128)
                usrc = wu_d[ex][:, f0:f0 + fsz].rearrange("(k p) f -> p k f", p=128)
                dsrc = wd_d[ex][f0:f0 + fsz, :].rearrange("(c p) d -> p c d", p=128)
                kb.op("pool", lambda e, wi=wi, gsrc=gsrc, fsz=fsz: e.dma_start(out=wgb[wi][:, :, 0:fsz], in_=gsrc), writes=[b_wgb[wi]], dma_sem=s_wg[wi])
                kb.op("pool", lambda e, wi=wi, usrc=usrc, fsz=fsz: e.dma_start(out=wub[wi][:, :, 0:fsz], in_=usrc), writes=[b_wub[wi]], dma_sem=s_wu[wi])
                kb.op("pool", lambda e, wi=wi, dsrc=dsrc, nch=nch: e.dma_start(out=wdb[wi][:, 0:nch, :], in_=dsrc), writes=[b_wdb[wi]], dma_sem=s_wd[wi])
                for tb in range(T // 512):
                    hi = tb % 2
                    for c in range(nch):
                        pi = c % 2
                        for k in range(8):
                            kb.op("pe", lambda e, wi=wi, c=c, k=k, pi=pi, tb=tb: e.matmul(pG[pi][:], wgb[wi][:, k, c * 128:(c + 1) * 128],
                                                                                          xnT[:, k, tb * 512:(tb + 1) * 512], start=(k == 0), stop=(k == 7)),
                                  reads=[b_wgb[wi], b_xnT[tb]], writes=[b_pG[pi]])
                        for k in range(8):
                            kb.op("pe", lambda e, wi=wi, c=c, k=k, pi=pi, tb=tb: e.matmul(pU[pi][:], wub[wi][:, k, c * 128:(c + 1) * 128],
                                                                                          xnT[:, k, tb * 512:(tb + 1) * 512], start=(k == 0), stop=(k == 7)),
                                  reads=[b_wub[wi], b_xnT[tb]], writes=[b_pU[pi]])
                        kb.op("act", lambda e, pi=pi: e.activation(out=sg[pi][:], in_=pG[pi][:], func=AF.Silu), reads=[b_pG[pi]], writes=[b_sg[pi]])
                        kb.op("dve", lambda e, pi=pi, hi=hi, c=c: e.tensor_tensor(out=hm[hi][:, c, :], in0=sg[pi][:], in1=pU[pi][:], op=ALU.mult),
                              reads=[b_sg[pi], b_pU[pi]], writes=[b_hm[hi][c]])
                    for j in range(4):
                        t = tb * 4 + j
                        for dh in range(2):
                            yi = (j * 2 + dh) % 2
                            for c in range(nch):
                                kb.op("pe", lambda e, hi=hi, c=c, j=j, dh=dh, yi=yi, wi=wi, nch=nch: e.matmul(
                                    pY[yi][:], hm[hi][:, c, j * 128:(j + 1) * 128], wdb[wi][:, c, dh * 512:(dh + 1) * 512],
                                    start=(c == 0), stop=(c == nch - 1)),
                                    reads=[b_hm[hi][c], b_wdb[wi]], writes=[b_pY[yi]])
                            if moe:
                                kb.op("dve", lambda e, t=t, dh=dh, yi=yi, ex=ex: e.scalar_tensor_tensor(
                                    out=acc[:, t, dh * 512:(dh + 1) * 512], in0=pY[yi][:], scalar=gates[:, t, ex:ex + 1],
                                    in1=acc[:, t, dh * 512:(dh + 1) * 512], op0=ALU.mult, op1=ALU.add),
                                    reads=[b_pY[yi], b_gates, b_acc[t]], writes=[b_acc[t]])
                            else:
                                kb.op("dve", lambda e, t=t, dh=dh, yi=yi: e.tensor_tensor(
                                    out=acc[:, t, dh * 512:(dh + 1) * 512], in0=pY[yi][:], in1=acc[:, t, dh * 512:(dh + 1) * 512], op=ALU.add),
                                    reads=[b_pY[yi], b_acc[t]], writes=[b_acc[t]])
        if final_norm:
            for base in range(0, NT, NIF):
                run_lockstep([epi_gen(t, t - base, t0) for t in range(base, min(base + NIF, NT))])
        else:
            for t in range(NT):
                r0 = t0 + t * 128
                kb.op("sp", lambda e, t=t, r0=r0: e.dma_start(out=hout_d[r0:r0 + 128, :], in_=acc[:, t, :]), reads=[b_acc[t]], dma_sem=s_st[t % 4])
    kb.end_phase()


def phase_dn(kb, hin_d, hout_d, W):
    nc = kb.nc
    kb.begin_phase()
    sq = kb.phase_dma_sem
    NT = S // 128
    win_d = W["dn_w_in"][0]

    def P(eng, fn, r=(), w=()):
        return kb.op(eng, fn, reads=r, writes=w)

    def const(name, shape, src, eng="sp"):
        t = kb.sb(name, shape, F32); b = kb.buf()
        kb.op(eng, lambda e: e.dma_start(out=t[:], in_=src), writes=[b], dma_sem=sq())
        return t, b

    gbc, b_gbc = const("gbc", [128, 1024], W["g_mix1_bc"])
    idf, b_idf = const("idf", [128, 128], W["ident"])
    ngbc, b_ngbc = const("ngbc", [128, 1024], W["dn_norm_g_bc"])
    cw, b_cw = const("cw", [128, 24, 4], W["conv_wl"])
    alog, b_alog = const("alog", [128, 8], W["a_log_bc"])
    dtb, b_dtb = const("dtb", [128, 8], W["dt_bias_bc"])
    trit, b_trit = const("trit", [128, 128], W["trit"])
    bones, b_bones = const("bones", [128, 128], W["bones"])
    csel, b_csel = const("csel", [128, 2, 128], W["csel"])
    lvm, b_lvm = const("lvm", [128, 6, 128], W["lvm"])
    muin, b_muin = const("muin", [128, 128], W["muin"])
    id4, b_id4 = const("id4", [128, 4, 128], W["id4"])
    idb = kb.sb("idb", [128, 128], BF16); b_idb = kb.buf()
    kb.op("pool", lambda e: e.dma_start(out=idb[:], in_=W["ident"]), writes=[b_idb], dma_sem=sq())
    wout = kb.sb("wout", [128, 8, 1024], BF16); b_wout = kb.buf()
    kb.op("pool", lambda e: e.dma_start(out=wout[:], in_=W["dn_w_out"][0].rearrange("(k p) n -> p k n", p=128)), writes=[b_wout], dma_sem=sq())
    wba = kb.sb("wba", [128, 8, 16], BF16); b_wba = kb.buf()
    kb.op("pool", lambda e: e.dma_start(out=wba[:], in_=W["wba_l"]), writes=[b_wba], dma_sem=sq())
    nega = kb.sb("nega", [128, 8], F32); b_nega = kb.buf()
    P("act", lambda e: e.activation(out=nega[:], in_=alog[:], func=AF.Exp), [b_alog], [b_nega])
    P("dve", lambda e: e.tensor_scalar(out=nega[:], in0=nega[:], scalar1=-1.0, scalar2=None, op0=ALU.mult), [b_nega], [b_nega])

    xt = kb.sb("xt", [128, 1024], F32); b_xt = kb.buf(); s_xt = sq()
    hn = kb.sb("hn", [128, 1024], F32); b_hn = kb.buf()
    hnT = kb.sb("hnT", [128, 8, 128], BF16); b_hnT = kb.buf()
    ss = kb.sb("ss", [128, 1], F32); b_ss = kb.buf()
    rstd = kb.sb("rstd", [128, 1], F32); b_rstd = kb.buf()
    junk = kb.sb("junk", [128, 128], BF16); b_junk = kb.buf()
    junkD = kb.sb("junkD", [128, 128], BF16); b_junkD = kb.buf()
    NWB = 2
    wst = [kb.sb("wst%d" % i, [128, 8, 512], BF16) for i in range(NWB)]; b_wst = [kb.buf() for _ in range(NWB)]
    s_wst = [sq() for _ in range(NWB)]
    hist = kb.sb("hist", [128, 24, 3], F32); b_hist = [kb.buf() for _ in range(6)]
    xcw4 = [kb.sb("xcw4_%d" % i, [128, 4, 131], F32) for i in range(2)]; b_xcw4 = [kb.buf() for _ in range(2)]
    cv4 = kb.sb("cv4", [128, 4, 128], F32); b_cv4 = kb.buf()
    ct4 = kb.sb("ct4", [128, 4, 128], F32); b_ct4 = kb.buf()
    toks = [kb.sb("tok%d" % i, [128, 3072], F32) for i in range(2)]; b_toks = [[kb.buf() for _ in range(6)] for _ in range(2)]
    zss = [kb.sb("zs%d" % i, [128, 1024], BF16) for i in range(3)]; b_zss = [kb.buf() for _ in range(3)]
    scs = [kb.sb("sc%d" % i, [128, 16, 8], F32) for i in range(2)]; b_scs = [kb.buf() for _ in range(2)]
    (I_EB, I_SB, I_X, I_G, I_GC, I_EGC, I_EDEC, I_RQ, I_RK, I_SKB, I_SKG, I_SKD, I_SQG, I_TMP, I_NG, I_RO) = range(16)
    qkss = kb.sb("qkss", [128, 16], F32); b_qkss = kb.buf()
    qkssD = kb.sb("qkssD", [128, 8], F32); b_qkssD = kb.buf()
    scD = kb.sb("scD", [128, 8], F32); b_scD = kb.buf()
    egls = [kb.sb("egl%d" % i, [128, 2, 8], F32) for i in range(2)]; b_egls = [kb.buf() for _ in range(2)]
    GB = []
    for gq_ in range(2):
        dct = {}
        for n_ in ['kbt', 'knt', 'kgt', 'kdt', 'qnt', 'qgt', 'kbT', 'knT', 'qnT', 'QG0', 'QG1', 'GT', 'NGT', 'Dm', 'Dn', 'L3', 'qkT', 'W0', 'W1', 'vn']:
            dct[n_] = (kb.sb("%s_%d" % (n_, gq_), [128, 4, 128], BF16 if n_ in ('kbt', 'knt', 'qnt', 'kbT', 'knT', 'qnT') else F32), kb.buf())
        GB.append(dct)
    BALL = [(kb.sb("Ball%d" % g_, [128, 6, 4, 128], BF16), kb.buf()) for g_ in range(2)]
    St = [[kb.sb("S%d_%d" % (g, i), [128, 4, 128], F32) for i in range(3)] for g in range(2)]
    b_St = [[kb.buf() for i in range(3)] for g in range(2)]
    otoks = [kb.sb("otok%d" % i, [128, 1024], F32) for i in range(2)]; b_otoks = [[kb.buf() for _ in range(2)] for _ in range(2)]
    ogT = kb.sb("ogT", [128, 8, 128], BF16); b_ogT = kb.buf()
    res = kb.sb("res", [128, 1024], F32); b_res = kb.buf(); s_res = sq(); s_out = sq()

    pX = kb.ps("pX", [128, 4, 128], F32); b_pX = kb.pbuf()
    pYk = kb.ps("pYk", [128, 4, 128], F32); b_pYk = kb.pbuf()
    pG = [kb.ps("pG%d" % i, [128, 4, 128], F32) for i in range(4)]; b_pG = [kb.pbuf() for _ in range(4)]
    pD1 = kb.ps("pD1", [128, 4, 128], F32); b_pD1 = kb.pbuf()
    pS = kb.ps("pS", [128, 64], F32); b_pS = kb.pbuf()

    for g in range(2):
        for i in range(3):
            P("pool", lambda e, g=g, i=i: e.memset(St[g][i][:], 0.0), [], [b_St[g][i]])
    P("pool", lambda e: e.memset(hist[:], 0.0), [], b_hist)
    for gq_ in range(2):
        for n_ in ("QG0", "QG1", "W0", "W1"):
            t_, b_ = GB[gq_][n_]
            P("pool", lambda e, t_=t_: e.memset(t_[:], 0.0), [], [b_])

    GSLOT = [[(pG[0][:], b_pG[0]), (pG[1][:], b_pG[1])], [(pG[2][:], b_pG[2]), (pG[3][:], b_pG[3])]]
    gctr = [0, 0]
    wcount = [0]
    sidx = [0, 0]

    def stageAB(t):
        p = t % 2
        tok = toks[p]; b_tok = b_toks[p]
        zs = zss[t % 3]; b_zs = b_zss[t % 3]
        sc = scs[p]; b_sc = b_scs[p]; egl = egls[p]; b_egl = b_egls[p]
        r0 = t * 128
        kb.op("sp", lambda e: e.dma_start(out=xt[:], in_=hin_d[r0:r0 + 128, :]), writes=[b_xt], dma_sem=s_xt)
        P("act", lambda e: e.activation(out=hn[:], in_=xt[:], func=AF.Square, accum_out=ss[:, 0:1]), [b_xt], [b_hn, b_ss])
        P("dve", lambda e: e.tensor_scalar(out=rstd[:], in0=ss[:], scalar1=1.0 / D, scalar2=EPS, op0=ALU.mult, op1=ALU.add), [b_ss], [b_rstd])
        P("act", lambda e: e.activation(out=rstd[:], in_=rstd[:], func=AF.Ln), [b_rstd], [b_rstd])
        P("act", lambda e: e.activation(out=rstd[:], in_=rstd[:], func=AF.Exp, scale=-0.5), [b_rstd], [b_rstd])
        yield
        P("dve", lambda e: e.scalar_tensor_tensor(out=hn[:], in0=xt[:], scalar=rstd[:, 0:1], in1=gbc[:], op0=ALU.mult, op1=ALU.mult),
          [b_xt, b_rstd, b_gbc], [b_hn])
        yield
        for half, (pt_, bpt_) in enumerate(((pX, b_pX), (pYk, b_pYk))):
            for k4 in range(4):
                k = half * 4 + k4
                P("pe", lambda e, k=k, k4=k4, pt_=pt_: e.transpose(pt_[:, k4, :], hn[:, k * 128:(k + 1) * 128], idf[:]), [b_hn, b_idf], [bpt_])
            P("act", lambda e, half=half, pt_=pt_: e.copy(out=hnT[:, half * 4:(half + 1) * 4, :], in_=pt_[:]), [bpt_], [b_hnT])
            yield
        def stage0(g):
            wi = wcount[0] % NWB; wcount[0] += 1
            src = win_d[:, g * 512:(g + 1) * 512].rearrange("(k p) f -> p k f", p=128)
            kb.op("pool", lambda e, wi=wi, src=src: e.dma_start(out=wst[wi][:], in_=src), writes=[b_wst[wi]], dma_sem=s_wst[wi])
            for c4 in range(4):
                for k in range(8):
                    P("pe", lambda e, wi=wi, c4=c4, k=k: e.matmul(pX[:, c4, :], wst[wi][:, k, c4 * 128:(c4 + 1) * 128], hnT[:, k, :], start=(k == 0), stop=(k == 7)),
                      [b_wst[wi], b_hnT], [b_pX])
            xc = xcw4[g % 2]; bxc = b_xcw4[g % 2]
            P("pool", lambda e, xc=xc, g=g: e.tensor_copy(out=xc[:, :, 0:3], in_=hist[:, g * 4:(g + 1) * 4, :]), [b_hist[g]], [bxc])

        def stage1(g):
            xc = xcw4[g % 2]; bxc = b_xcw4[g % 2]
            P("act", lambda e, xc=xc: e.copy(out=xc[:, :, 3:131], in_=pX[:]), [b_pX], [bxc])

        def wbc(g, j):
            return cw[:, g * 4:(g + 1) * 4, j:j + 1].to_broadcast([128, 4, 128])

        stage0(0)
        yield
        stage1(0)
        yield
        for g in range(6):
            xc = xcw4[g % 2]; bxc = b_xcw4[g % 2]
            if g + 1 < 6:
                stage0(g + 1)
            P("pool", lambda e, xc=xc, g=g: e.tensor_copy(out=hist[:, g * 4:(g + 1) * 4, :], in_=xc[:, :, 128:131]), [bxc], [b_hist[g]])
            P("dve", lambda e, xc=xc, g=g: e.tensor_tensor(out=cv4[:], in0=xc[:, :, 0:128], in1=wbc(g, 0), op=ALU.mult), [bxc, b_cw], [b_cv4])
            P("dve", lambda e, xc=xc, g=g: e.tensor_tensor(out=ct4[:], in0=xc[:, :, 1:129], in1=wbc(g, 1), op=ALU.mult), [bxc, b_cw], [b_ct4])
            P("dve", lambda e: e.tensor_tensor(out=cv4[:], in0=cv4[:], in1=ct4[:], op=ALU.add), [b_ct4, b_cv4], [b_cv4])
            P("dve", lambda e, xc=xc, g=g: e.tensor_tensor(out=ct4[:], in0=xc[:, :, 2:130], in1=wbc(g, 2), op=ALU.mult), [bxc, b_cw], [b_ct4])
            yield
            if g + 1 < 6:
                stage1(g + 1)
            P("dve", lambda e: e.tensor_tensor(out=cv4[:], in0=cv4[:], in1=ct4[:], op=ALU.add), [b_ct4, b_cv4], [b_cv4])
            P("dve", lambda e, xc=xc, g=g: e.tensor_tensor(out=ct4[:], in0=xc[:, :, 3:131], in1=wbc(g, 3), op=ALU.mult), [bxc, b_cw], [b_ct4])
            P("dve", lambda e: e.tensor_tensor(out=cv4[:], in0=cv4[:], in1=ct4[:], op=ALU.add), [b_ct4, b_cv4], [b_cv4])
            P("act", lambda e: e.activation(out=cv4[:], in_=cv4[:], func=AF.Silu), [b_cv4], [b_cv4])
            yield
            for c4 in range(4):
                P("pe", lambda e, c4=c4: e.transpose(pYk[:, c4, :], cv4[:, c4, :], idf[:]), [b_cv4, b_idf], [b_pYk])
            if g % 2 == 0:
                P("dve", lambda e, g=g: e.tensor_copy(out=tok[:, g * 512:(g + 1) * 512], in_=pYk[:].rearrange("p c d -> p (c d)")), [b_pYk], [b_tok[g]])
            else:
                P("act", lambda e, g=g: e.copy(out=tok[:, g * 512:(g + 1) * 512], in_=pYk[:].rearrange("p c d -> p (c d)")), [b_pYk], [b_tok[g]])
            yield
        for zh in range(2):
            wi = wcount[0] % NWB; wcount[0] += 1
            src = win_d[:, 3072 + zh * 512:3072 + (zh + 1) * 512].rearrange("(k p) f -> p k f", p=128)
            kb.op("pool", lambda e, wi=wi, src=src: e.dma_start(out=wst[wi][:], in_=src), writes=[b_wst[wi]], dma_sem=s_wst[wi])
            for k in range(8):
                P("pe", lambda e, wi=wi, k=k: e.matmul(pX[:].rearrange("p c d -> p (c d)"), hnT[:, k, :], wst[wi][:, k, :], start=(k == 0), stop=(k == 7)),
                  [b_wst[wi], b_hnT], [b_pX])
            P("act", lambda e, zh=zh: e.activation(out=zs[:, zh * 512:(zh + 1) * 512], in_=pX[:].rearrange("p c d -> p (c d)"), func=AF.Silu), [b_pX], [b_zs])
            yield
        for k in range(8):
            P("pe", lambda e, k=k: e.matmul(pS[:, 0:16], hnT[:, k, :], wba[:, k, :], start=(k == 0), stop=(k == 7)), [b_hnT, b_wba], [b_pS])
        R = [b_sc]
        P("act", lambda e: e.activation(out=sc[:, I_EB, :], in_=pS[:, 0:8], func=AF.Exp, scale=-1.0), [b_pS] + R, R)
        P("dve", lambda e: e.tensor_tensor(out=sc[:, I_X, :], in0=pS[:, 8:16], in1=dtb[:], op=ALU.add), [b_pS, b_dtb] + R, R)
        yield
        P("dve", lambda e: e.tensor_scalar(out=sc[:, I_EB, :], in0=sc[:, I_EB, :], scalar1=1.0, scalar2=None, op0=ALU.add), R, R)
        P("act", lambda e: e.activation(out=sc[:, I_SB, :], in_=sc[:, I_EB, :], func=AF.Ln), R, R)
        P("act", lambda e: e.activation(out=sc[:, I_SB, :], in_=sc[:, I_SB, :], func=AF.Exp, scale=-0.5), R, R)
        yield
        P("act", lambda e: e.activation(out=sc[:, I_X, :], in_=sc[:, I_X, :], func=AF.Exp), R, R)
        P("dve", lambda e: e.tensor_scalar(out=sc[:, I_X, :], in0=sc[:, I_X, :], scalar1=1.0, scalar2=None, op0=ALU.add), R, R)
        P("act", lambda e: e.activation(out=sc[:, I_X, :], in_=sc[:, I_X, :], func=AF.Ln), R, R)
        yield
        P("dve", lambda e: e.tensor_tensor(out=sc[:, I_G, :], in0=sc[:, I_X, :], in1=nega[:], op=ALU.mult), R + [b_nega], R)
        P("pe", lambda e: e.matmul(pS[:, 16:24], trit[:], sc[:, I_G, :], start=True, stop=True), R + [b_trit], [b_pS])
        P("pe", lambda e: e.matmul(pS[:, 24:32], bones[:], sc[:, I_G, :], start=True, stop=True), R + [b_bones], [b_pS])
        for c in range(2):
            P("pe", lambda e, c=c: e.matmul(pS[:, 32 + 8 * c:40 + 8 * c], csel[:, c, :], sc[:, I_G, :], start=True, stop=True), R + [b_csel], [b_pS])
        yield
        P("act", lambda e: e.copy(out=sc[:, I_GC, :], in_=pS[:, 16:24]), [b_pS] + R, R)
        P("act", lambda e: e.activation(out=sc[:, I_EGC, :], in_=pS[:, 16:24], func=AF.Exp), [b_pS] + R, R)
        P("dve", lambda e: e.tensor_tensor(out=sc[:, I_EDEC, :], in0=pS[:, 24:32], in1=sc[:, I_GC, :], op=ALU.subtract), [b_pS] + R, R)
        yield
        P("act", lambda e: e.activation(out=sc[:, I_EDEC, :], in_=sc[:, I_EDEC, :], func=AF.Exp), R, R)
        P("act", lambda e: e.activation(out=egl[:].rearrange("p c h -> p (c h)"), in_=pS[:, 32:48], func=AF.Exp), [b_pS], [b_egl])
        yield
        for hh in range(16):
            P("act", lambda e, hh=hh: e.activation(out=junk[:], in_=tok[:, hh * 128:(hh + 1) * 128], func=AF.Square, accum_out=qkss[:, hh:hh + 1]),
              [b_tok[hh // 4]], [b_junk, b_qkss])
            if hh % 4 == 3:
                yield
        P("dve", lambda e: e.tensor_scalar(out=sc[:, I_RQ, :], in0=qkss[:, 0:8], scalar1=EPS, scalar2=128.0, op0=ALU.add, op1=ALU.mult), [b_qkss] + R, R)
        P("dve", lambda e: e.tensor_scalar(out=sc[:, I_RK, :], in0=qkss[:, 8:16], scalar1=EPS, scalar2=None, op0=ALU.add), [b_qkss] + R, R)
        P("act", lambda e: e.activation(out=sc[:, I_RQ:I_RK + 1, :], in_=sc[:, I_RQ:I_RK + 1, :], func=AF.Ln), R, R)
        P("act", lambda e: e.activation(out=sc[:, I_RQ:I_RK + 1, :], in_=sc[:, I_RQ:I_RK + 1, :], func=AF.Exp, scale=-0.5), R, R)
        yield
        P("dve", lambda e: e.tensor_tensor(out=sc[:, I_SKB, :], in0=sc[:, I_RK, :], in1=sc[:, I_SB, :], op=ALU.mult), R, R)
        P("dve", lambda e: e.tensor_tensor(out=sc[:, I_SKG, :], in0=sc[:, I_RK, :], in1=sc[:, I_EGC, :], op=ALU.mult), R, R)
        P("dve", lambda e: e.tensor_tensor(out=sc[:, I_SKD, :], in0=sc[:, I_RK, :], in1=sc[:, I_EDEC, :], op=ALU.mult), R, R)
        P("dve", lambda e: e.tensor_tensor(out=sc[:, I_SQG, :], in0=sc[:, I_RQ, :], in1=sc[:, I_EGC, :], op=ALU.mult), R, R)
        P("dve", lambda e: e.tensor_scalar(out=sc[:, I_NG, :], in0=sc[:, I_G, :], scalar1=-1.0, scalar2=None, op0=ALU.mult), R, R)
        yield

    def grp(gq, t):
        p = t % 2
        sc = scs[p]; b_sc = b_scs[p]; egl = egls[p]; b_egl = b_egls[p]; otok = otoks[p]; b_otok = b_otoks[p]
        def bc4(col, gq, lo=0, hi=128):
            return sc[lo:hi, col, gq * 4:(gq + 1) * 4].unsqueeze(2).to_broadcast([hi - lo, 4, 128])
        def palloc():
            i_ = gctr[gq]; gctr[gq] += 1
            return GSLOT[gq][i_ % len(GSLOT[gq])]
        kbt, b_kbt = GB[gq]['kbt']
        knt, b_knt = GB[gq]['knt']
        kgt, b_kgt = GB[gq]['kgt']
        kdt, b_kdt = GB[gq]['kdt']
        qnt, b_qnt = GB[gq]['qnt']
        qgt, b_qgt = GB[gq]['qgt']
        kbT, b_kbT = GB[gq]['kbT']
        knT, b_knT = GB[gq]['knT']
        qnT, b_qnT = GB[gq]['qnT']
        QG0, b_QG0 = GB[gq]['QG0']
        QG1, b_QG1 = GB[gq]['QG1']
        GT, b_GT = GB[gq]['GT']
        NGT, b_NGT = GB[gq]['NGT']
        Dm, b_Dm = GB[gq]['Dm']
        Dn, b_Dn = GB[gq]['Dn']
        L3, b_L3 = GB[gq]['L3']
        qkT, b_qkT = GB[gq]['qkT']
        W0, b_W0 = GB[gq]['W0']
        W1, b_W1 = GB[gq]['W1']
        vn, b_vn = GB[gq]['vn']
        Ball, b_Ball = BALL[gq]
        Y, b_Y = GB[gq]['kbT']
        Qs, b_Qs = GB[gq]['kbt']
        YTs, b_YTs = GB[gq]['knt']
        Yb, b_Yb = GB[gq]['GT']
        tmpS, b_tmpS = GB[gq]['qgt']
        h0 = gq * 4
        kraw = toks[p][:, 1024 + h0 * 128:1024 + (h0 + 4) * 128].rearrange("p (h d) -> p h d", h=4)
        qraw = toks[p][:, h0 * 128:(h0 + 4) * 128].rearrange("p (h d) -> p h d", h=4)
        vraw = toks[p][:, 2048 + h0 * 128:2048 + (h0 + 4) * 128].rearrange("p (h d) -> p h d", h=4)
        bk = b_toks[p][2 + gq]; bq = b_toks[p][gq]; bv = b_toks[p][4 + gq]
        def scaled(out, bout, raw, braw, col, eng):
            in1 = bc4(col, gq)
            P(eng, lambda e: e.tensor_tensor(out=out[:], in0=raw, in1=in1, op=ALU.mult), [braw, b_sc], [bout])
        scaled(kbt, b_kbt, kraw, bk, I_SKB, "dve")
        scaled(knt, b_knt, kraw, bk, I_RK, "pool")
        scaled(qnt, b_qnt, qraw, bq, I_RQ, "dve")
        scaled(qgt, b_qgt, qraw, bq, I_SQG, "pool")
        P("pool", lambda e, gq=gq: e.tensor_tensor(out=GT[:], in0=trit[:].unsqueeze(1).to_broadcast([128, 4, 128]), in1=bc4(I_G, gq), op=ALU.mult),
          [b_trit, b_sc], [b_GT])
        P("pool", lambda e, gq=gq: e.tensor_tensor(out=NGT[:], in0=trit[:].unsqueeze(1).to_broadcast([128, 4, 128]), in1=bc4(I_NG, gq), op=ALU.mult),
          [b_trit, b_sc], [b_NGT])
        scaled(kgt, b_kgt, kraw, bk, I_SKG, "dve")
        scaled(kdt, b_kdt, kraw, bk, I_SKD, "pool")
        yield

        def tr4(src, bsrc, lowp=True):
            pb, bpb = palloc()
            for h in range(4):
                if lowp:
                    P("pe", lambda e, h=h, pb=pb: e.matmul(pb[:, h, :], src[:, h, :], idb[:], start=True, stop=True), [bsrc, b_idb], [bpb])
                else:
                    P("pe", lambda e, h=h, pb=pb: e.transpose(pb[:, h, :], src[:, h, :], idf[:]), [bsrc, b_idf], [bpb])
            return pb, bpb
        pb1, bpb1 = tr4(kbt, b_kbt)
        pb2, bpb2 = tr4(knt, b_knt)
        yield
        P("act", lambda e, pb=pb1: e.copy(out=kbT[:], in_=pb[:]), [bpb1], [b_kbT])
        P("dve", lambda e, pb=pb2: e.tensor_copy(out=knT[:], in_=pb[:]), [bpb2], [b_knT])
        yield
        pb1, bpb1 = tr4(qnt, b_qnt)
        pb2, bpb2 = tr4(qgt, b_qgt, lowp=False)
        yield
        P("act", lambda e, pb=pb1: e.copy(out=qnT[:], in_=pb[:]), [bpb1], [b_qnT])
        P("dve", lambda e, pb=pb2: e.tensor_copy(out=QG0[:, :, 0:64], in_=pb[:, :, 0:64]), [bpb2], [b_QG0])
        P("act", lambda e, pb=pb2: e.copy(out=QG1[:, :, 64:128], in_=pb[:, :, 64:128]), [bpb2], [b_QG1])
        yield
        pD, bpD = palloc()
        for h in range(4):
            P("pe", lambda e, h=h, pD=pD: e.matmul(pD[:, h, :], GT[:, h, :], bones[:], start=True, stop=False), [b_GT, b_bones], [bpD])
            P("pe", lambda e, h=h, pD=pD: e.matmul(pD[:, h, :], bones[:], NGT[:, h, :], start=False, stop=True), [b_NGT, b_bones], [bpD])
        pK, bpK = palloc()
        for h in range(4):
            P("pe", lambda e, h=h, pK=pK: e.matmul(pK[:, h, :], kbT[:, h, :], kbT[:, h, :], start=True, stop=True), [b_kbT], [bpK])
        yield
        P("dve", lambda e, pD=pD: e.tensor_scalar(out=Dm[:], in0=pD[:], scalar1=0.0, scalar2=None, op0=ALU.min), [bpD], [b_Dm])
        P("dve", lambda e, pD=pD: e.tensor_scalar(out=Dn[:], in0=pD[:], scalar1=-1.0, scalar2=0.0, op0=ALU.mult, op1=ALU.min), [bpD], [b_Dn])
        yield
        P("act", lambda e: e.activation(out=Dm[:], in_=Dm[:], func=AF.Exp), [b_Dm], [b_Dm])
        P("act", lambda e: e.activation(out=Dn[:], in_=Dn[:], func=AF.Exp), [b_Dn], [b_Dn])
        pQ, bpQ = palloc()
        for h in range(4):
            P("pe", lambda e, h=h, pQ=pQ: e.matmul(pQ[:, h, :], knT[:, h, :], qnT[:, h, :], start=True, stop=True), [b_knT, b_qnT], [bpQ])
        yield
        P("dve", lambda e, pK=pK: e.tensor_tensor(out=L3[:], in0=pK[:], in1=Dm[:], op=ALU.mult), [bpK, b_Dm], [b_L3])
        P("pool", lambda e: e.tensor_tensor(out=Dn[:], in0=Dn[:], in1=muin[:].unsqueeze(1).to_broadcast([128, 4, 128]), op=ALU.mult), [b_Dn, b_muin], [b_Dn])
        yield
        P("pool", lambda e: e.tensor_tensor(out=Ball[:], in0=L3[:].unsqueeze(1).to_broadcast([128, 6, 4, 128]),
                                            in1=lvm[:].unsqueeze(2).to_broadcast([128, 6, 4, 128]), op=ALU.mult), [b_L3, b_lvm], [b_Ball])
        P("dve", lambda e, pQ=pQ: e.tensor_tensor(out=qkT[:], in0=pQ[:], in1=Dn[:], op=ALU.mult), [bpQ, b_Dn], [b_qkT])
        yield
        for lv in range(6):
            pq, bpq = palloc()
            if lv == 0:
                for h in range(4):
                    P("pe", lambda e, h=h, pq=pq: e.matmul(pq[:, h, :], Ball[:, 0, h, :], idb[:], start=True, stop=True), [b_Ball, b_idb], [bpq])
                yield
                P("dve", lambda e, pq=pq: e.scalar_tensor_tensor(out=Y[:], in0=pq[:], scalar=-1.0, in1=id4[:], op0=ALU.mult, op1=ALU.add), [bpq, b_id4], [b_Y])
                yield
                continue
            for h in range(4):
                P("pe", lambda e, h=h, pq=pq, lv=lv: e.matmul(pq[:, h, :], Ball[:, lv, h, :], Y[:, h, :], start=True, stop=True), [b_Ball, b_Y], [bpq])
            pt, bpt = tr4(Y, b_Y)
            yield
            P("act", lambda e, pq=pq: e.copy(out=Qs[:], in_=pq[:]), [bpq], [b_Qs])
            P("dve", lambda e, pt=pt: e.tensor_copy(out=YTs[:], in_=pt[:]), [bpt], [b_YTs])
            yield
            py, bpy = palloc()
            for h in range(4):
                P("pe", lambda e, h=h, py=py: e.matmul(py[:, h, :], YTs[:, h, :], Qs[:, h, :], start=True, stop=True), [b_YTs, b_Qs], [bpy])
            yield
            P("dve", lambda e, py=py: e.tensor_tensor(out=Y[:], in0=Y[:], in1=py[:], op=ALU.subtract), [bpy, b_Y], [b_Y])
            yield
        P("dve", lambda e, gq=gq: e.tensor_tensor(out=Yb[:], in0=Y[:], in1=bc4(I_SB, gq), op=ALU.mult), [b_Y, b_sc], [b_Yb])
        yield
        pw, bpw = palloc()
        for h in range(4):
            P("pe", lambda e, h=h, pw=pw: e.matmul(pw[:, h, :], kgt[:, h, :], Yb[:, h, :], start=True, stop=True), [b_kgt, b_Yb], [bpw])
        yield
        P("dve", lambda e, pw=pw: e.tensor_scalar(out=W0[:, :, 0:64], in0=pw[:, :, 0:64], scalar1=-1.0, scalar2=None, op0=ALU.mult), [bpw], [b_W0])
        P("act", lambda e, pw=pw: e.activation(out=W1[:, :, 64:128], in_=pw[:, :, 64:128], func=AF.Identity, scale=-1.0), [bpw], [b_W1])
        yield
        si = sidx[gq]
        Sa, bSa = St[gq][si % 3], b_St[gq][si % 3]
        Sb, bSb = St[gq][(si + 1) % 3], b_St[gq][(si + 1) % 3]
        Sc_, bSc = St[gq][(si + 2) % 3], b_St[gq][(si + 2) % 3]
        sidx[gq] = si + 2
        for c, (Sin, bSin, Sout, bSout, Wc, bWc) in enumerate(((Sa, bSa, Sb, bSb, W0, b_W0), (Sb, bSb, Sc_, bSc, W1, b_W1))):
            lo, hi = c * 64, c * 64 + 64
            pv, bpv = palloc()
            for h in range(4):
                P("pe", lambda e, h=h, pv=pv, vraw=vraw: e.matmul(pv[:, h, :], Yb[:, h, :], vraw[:, h, :], start=True, stop=False), [b_Yb, bv], [bpv])
                P("pe", lambda e, h=h, pv=pv, Wc=Wc, Sin=Sin: e.matmul(pv[:, h, :], Wc[:, h, :], Sin[:, h, :], start=False, stop=True), [bWc, bSin], [bpv])
            yield
            P("dve", lambda e, pv=pv, lo=lo, hi=hi, gq=gq: e.tensor_tensor(out=vn[lo:hi], in0=pv[lo:hi], in1=bc4(I_SB, gq, lo, hi), op=ALU.mult),
              [bpv, b_sc], [b_vn])
            yield
            ps_, bps = palloc()
            for h in range(4):
                P("pe", lambda e, h=h, ps_=ps_, lo=lo, hi=hi: e.matmul(ps_[:, h, :], kdt[lo:hi, h, :], vn[lo:hi, h, :], start=True, stop=True), [b_kdt, b_vn], [bps])
            P("pool", lambda e, c=c, gq=gq, Sin=Sin: e.tensor_tensor(out=tmpS[:], in0=Sin[:], in1=egl[:, c, gq * 4:(gq + 1) * 4].unsqueeze(2).to_broadcast([128, 4, 128]), op=ALU.mult),
              [bSin, b_egl], [b_tmpS])
            yield
            P("dve", lambda e, ps_=ps_, Sout=Sout: e.tensor_tensor(out=Sout[:], in0=tmpS[:], in1=ps_[:], op=ALU.add), [b_tmpS, bps], [bSout])
            yield
        pO, bpO = palloc()
        for h in range(4):
            P("pe", lambda e, h=h, pO=pO, Sa=Sa: e.matmul(pO[:, h, :], QG0[:, h, :], Sa[:, h, :], start=True, stop=False), [b_QG0, bSa], [bpO])
            P("pe", lambda e, h=h, pO=pO, Sb=Sb: e.matmul(pO[:, h, :], QG1[:, h, :], Sb[:, h, :], start=False, stop=False), [b_QG1, bSb], [bpO])
            P("pe", lambda e, h=h, pO=pO: e.matmul(pO[:, h, :], qkT[:, h, :], vn[:, h, :], start=False, stop=True), [b_qkT, b_vn], [bpO])
        yield
        P("act", lambda e, pO=pO, gq=gq: e.copy(out=otok[:, gq * 512:(gq + 1) * 512], in_=pO.rearrange("p h d -> p (h d)")), [bpO], [b_otok[gq]])
        yield

    def stageD(t):
        p = t % 2
        otok = otoks[p]; b_otok = b_otoks[p]
        zs = zss[t % 3]; b_zs = b_zss[t % 3]
        for hh in range(8):
            P("act", lambda e, hh=hh: e.activation(out=junkD[:], in_=otok[:, hh * 128:(hh + 1) * 128], func=AF.Square, accum_out=qkssD[:, hh:hh + 1]),
              [b_otok[hh // 4]], [b_junkD, b_qkssD])
            if hh % 4 == 3:
                yield
        P("dve", lambda e: e.tensor_scalar(out=scD[:], in0=qkssD[:], scalar1=1.0 / 128, scalar2=EPS, op0=ALU.mult, op1=ALU.add), [b_qkssD, b_scD], [b_scD])
        P("act", lambda e: e.activation(out=scD[:], in_=scD[:], func=AF.Ln), [b_scD], [b_scD])
        P("act", lambda e: e.activation(out=scD[:], in_=scD[:], func=AF.Exp, scale=-0.5), [b_scD], [b_scD])
        yield
        P("dve", lambda e: e.tensor_tensor(out=otok[:].rearrange("p (h d) -> p h d", h=8), in0=otok[:].rearrange("p (h d) -> p h d", h=8),
                                           in1=scD[:].unsqueeze(2).to_broadcast([128, 8, 128]), op=ALU.mult), b_otok + [b_scD], b_otok)
        yield
        P("pool", lambda e: e.tensor_tensor(out=otok[:], in0=otok[:], in1=ngbc[:], op=ALU.mult), b_otok + [b_ngbc], b_otok)
        yield
        P("dve", lambda e: e.tensor_tensor(out=otok[:], in0=otok[:], in1=zs[:], op=ALU.mult), b_otok + [b_zs], b_otok)
        yield
        for half in range(2):
            for k4 in range(4):
                k = half * 4 + k4
                P("pe", lambda e, k=k, k4=k4: e.transpose(pD1[:, k4, :], otok[:, k * 128:(k + 1) * 128], idf[:]), b_otok + [b_idf], [b_pD1])
            P("act", lambda e, half=half: e.copy(out=ogT[:, half * 4:(half + 1) * 4, :], in_=pD1[:]), [b_pD1], [b_ogT])
            yield
        rr = t * 128
        kb.op("sp", lambda e: e.dma_start(out=res[:], in_=hin_d[rr:rr + 128, :]), writes=[b_res], dma_sem=s_res)
        for dh in range(2):
            for k in range(8):
                P("pe", lambda e, k=k, dh=dh: e.matmul(pD1[:].rearrange("p c d -> p (c d)"), ogT[:, k, :], wout[:, k, dh * 512:(dh + 1) * 512], start=(k == 0), stop=(k == 7)),
                  [b_ogT, b_wout], [b_pD1])
            yield
            P("dve", lambda e, dh=dh: e.tensor_tensor(out=res[:, dh * 512:(dh + 1) * 512], in0=res[:, dh * 512:(dh + 1) * 512], in1=pD1[:].rearrange("p c d -> p (c d)"), op=ALU.add),
              [b_pD1, b_res], [b_res])
            yield
        kb.op("sp", lambda e: e.dma_start(out=hout_d[rr:rr + 128, :], in_=res[:]), reads=[b_res], dma_sem=s_out)
        yield

    for it in range(-1, NT + 1):
        gens = []
        if 0 <= it < NT:
            gens.append(grp(0, it)); gens.append(grp(1, it))
        if 0 <= it + 1 < NT:
            gens.append(stageAB(it + 1))
        if 0 <= it - 1 < NT:
            gens.append(stageD(it - 1))
        while gens:
            for g_ in list(gens):
                try:
                    next(g_)
                except StopIteration:
                    gens.remove(g_)
    kb.end_phase()


def make_consts():
    c = {}
    c["ident"] = np.eye(128, dtype=np.float32)
    invc = np.zeros((128, 4, 16), np.float32)
    for g in range(4):
        w = 2 ** (g + 1)
        for t in range(16):
            invc[:, g, t] = 1.0 / min(t + 1, w)
    c["invc"] = invc
    i = np.arange(128)
    same = (i[:, None] // 64) == (i[None, :] // 64)
    c["trit"] = (same & (i[:, None] <= i[None, :])).astype(np.float32)
    c["bones"] = same.astype(np.float32)
    cs = np.zeros((128, 2, 128), np.float32)
    cs[:64, 0, :] = 1.0
    cs[64:, 1, :] = 1.0
    c["csel"] = cs
    lvm = np.zeros((128, 6, 128), np.float32)
    for lv in range(6):
        b = 2 ** lv
        m = ((i[:, None] // (2 * b)) == (i[None, :] // (2 * b))) & ((i[:, None] % (2 * b)) >= b) & ((i[None, :] % (2 * b)) < b)
        lvm[:, lv, :] = m
    c["lvm"] = lvm
    c["muin"] = (same & (i[None, :] >= i[:, None])).astype(np.float32)
    c["id4"] = np.ascontiguousarray(np.broadcast_to(np.eye(128, dtype=np.float32)[:, None, :], (128, 4, 128)))
    return c

def bc(v, n=128):
    return np.ascontiguousarray(np.broadcast_to(np.asarray(v, np.float32).reshape(1, -1), (n, np.asarray(v).size)))

def host_inputs(inp):
    d = {}
    for k in ["pool_w_in", "pool_w_group", "dn_w_in", "dn_w_out", "ffn_w_gate", "ffn_w_up", "ffn_w_down",
              "moe_w_gate", "moe_w_up", "moe_w_down"]:
        d[k] = np.ascontiguousarray(inp[k], dtype=np.float32)
    d["pool_scale_bc"] = bc(inp["pool_scale"][0])
    d["g_mix0_bc"] = bc(inp["norm_mix_g"][0])
    d["g_mix1_bc"] = bc(inp["norm_mix_g"][1])
    d["g_ffn0_bc"] = bc(inp["norm_ffn_g"][0])
    d["g_ffn1_bc"] = bc(inp["norm_ffn_g"][1])
    d["g_final_bc"] = bc(inp["final_norm_g"])
    d["router_b_bc"] = bc(inp["moe_router_b"][0])
    d["a_log_bc"] = bc(inp["dn_a_log"][0])
    d["dt_bias_bc"] = bc(inp["dn_dt_bias"][0])
    d["dn_norm_g_bc"] = bc(np.tile(inp["dn_norm_g"][0], 8))
    d["conv_wl"] = np.ascontiguousarray(inp["dn_conv_w"][0].T.reshape(24, 128, 4).transpose(1, 0, 2))
    d["router_w_l"] = np.ascontiguousarray(inp["moe_router_w"][0].reshape(8, 128, 8).transpose(1, 0, 2))
    d["wba_l"] = np.ascontiguousarray(inp["dn_w_in"][0][:, 4096:4112].reshape(8, 128, 16).transpose(1, 0, 2))
    d.update(make_consts())
    return d

def declare_inputs(nc, d):
    W = {}
    for k, v in d.items():
        W[k] = nc.dram_tensor(k, list(v.shape), F32, kind="ExternalInput").ap()
    return W


def build_program(d):
    nc = bass.Bass("TRN2", target_bir_lowering=False)
    W = declare_inputs(nc, d)
    x_d = nc.dram_tensor("x", [S, D], F32, kind="ExternalInput").ap()
    out_d = nc.dram_tensor("out", [S, D], F32, kind="ExternalOutput").ap()
    h1 = nc.dram_tensor("h1s", [S, D], F32).ap()
    h2 = nc.dram_tensor("h2s", [S, D], F32).ap()
    h3 = nc.dram_tensor("h3s", [S, D], F32).ap()
    kb = KB(nc)
    phase_pool(kb, x_d, h1, W)
    phase_ffn(kb, h1, h2, W, moe=False)
    phase_dn(kb, h2, h3, W)
    phase_ffn(kb, h3, out_d, W, moe=True, final_norm=True)
    kb.close()
    return nc


def kernel(**inputs):
    inp = {k: np.asarray(v) for k, v in inputs.items()}
    d = host_inputs(inp)
    nc = build_program(d)
    x = np.ascontiguousarray(inp["x"], dtype=np.float32)
    nb = x.shape[0]
    in_maps = []
    for b in range(nb):
        m = dict(d)
        m["x"] = x[b]
        in_maps.append(m)
    res = run_bass_kernel_spmd(nc, in_maps, core_ids=list(range(nb)))
    return np.stack([np.asarray(r["out"], dtype=np.float32) for r in res.results], axis=0)
```
